# Optimizing a Trainium2 kernel written in Bass

```python
import jax, jax.numpy as jnp
from jax import lax
import numpy as np

D_MODEL = 1024
BATCH = 4
SEQ = 8192
DEPTH = 2

CHUNK = 64
N_META = 16
EPS = 1e-6

POOL_GROUPS = 4
POOL_WINDOWS = (2, 4, 8, 16)
POOL_WIDTH = D_MODEL // 4
POOL_GDIM = POOL_WIDTH // POOL_GROUPS
CONV_WIDTH = D_MODEL // 4
CONV_KSIZE = 31
FOX_HEADS = 8
FOX_HEAD_DIM = 64
FOX_WIDTH = FOX_HEADS * FOX_HEAD_DIM
Q_BLOCK = 128
N_BRANCH = 3
IN_SPLITS = (POOL_WIDTH, CONV_WIDTH, CONV_WIDTH, FOX_WIDTH, FOX_WIDTH, FOX_WIDTH, FOX_HEADS, N_BRANCH * D_MODEL)
N_IN = sum(IN_SPLITS)
N_GROUPS = 4
EXPERTS_PER_GROUP = 4
N_EXPERTS = N_GROUPS * EXPERTS_PER_GROUP
TOP_K = 2
EXPERT_HIDDEN = D_MODEL // 4

kernel_name = "hybrid_pool_conv_fox_hmoe_meta"


def rms_norm(x, g):
    xf = x.astype(jnp.float32)
    y = xf * lax.rsqrt(jnp.mean(xf * xf, axis=-1, keepdims=True) + EPS)
    return (y * g.astype(jnp.float32)).astype(x.dtype)


def layer_norm(x, g, b):
    xf = x.astype(jnp.float32)
    mu = jnp.mean(xf, axis=-1, keepdims=True)
    var = jnp.mean(jnp.square(xf - mu), axis=-1, keepdims=True)
    y = (xf - mu) * lax.rsqrt(var + EPS)
    return (y * g.astype(jnp.float32) + b.astype(jnp.float32)).astype(x.dtype)


def pool_mixer(a, pool_w, pool_b, pool_scale):
    B_, L, _ = a.shape
    af = a.astype(jnp.float32).reshape(B_, L, POOL_GROUPS, POOL_GDIM)
    cs = jnp.concatenate([jnp.zeros_like(af[:, :1]), jnp.cumsum(af, axis=1)], axis=1)
    t = jnp.arange(L, dtype=jnp.int32)[:, None]
    win = jnp.array(POOL_WINDOWS, dtype=jnp.int32)[None, :]
    lo = jnp.maximum(t + 1 - win, 0)
    cnt = (t + 1 - lo).astype(jnp.float32)
    grp = jnp.arange(POOL_GROUPS, dtype=jnp.int32)[None, :]
    mean = (cs[:, 1:] - cs[:, lo, grp]) / cnt[None, :, :, None]
    diff = mean - af
    y = jnp.einsum('blgc,gcd->blgd', diff, pool_w.astype(jnp.float32)) + pool_b.astype(jnp.float32)
    y = y.reshape(B_, L, POOL_WIDTH) * pool_scale.astype(jnp.float32)
    return y.astype(a.dtype)


def conv_module(bv, bg, conv_w, conv_b, ln_g, ln_b):
    glu = bv * jax.nn.sigmoid(bg)
    y = lax.conv_general_dilated(
        glu, conv_w.astype(glu.dtype)[:, None, :], window_strides=(1,),
        padding=[(CONV_KSIZE - 1, 0)], dimension_numbers=('NWC', 'WIO', 'NWC'),
        feature_group_count=CONV_WIDTH)
    y = y + conv_b.astype(y.dtype)
    y = layer_norm(y, ln_g, ln_b)
    return jax.nn.silu(y)


def forgetting_attention(q, k, v, logf):
    B_, L, H, dh = q.shape
    n_blk = -(-L // Q_BLOCK)
    Lp = n_blk * Q_BLOCK
    pad = Lp - L
    qh = jnp.pad(q.transpose(0, 2, 1, 3), ((0, 0), (0, 0), (0, pad), (0, 0)))
    kh = jnp.pad(k.transpose(0, 2, 1, 3), ((0, 0), (0, 0), (0, pad), (0, 0)))
    vh = jnp.pad(v.transpose(0, 2, 1, 3), ((0, 0), (0, 0), (0, pad), (0, 0)))
    F = jnp.cumsum(jnp.pad(logf.transpose(0, 2, 1), ((0, 0), (0, 0), (0, pad))), axis=-1)
    qb = qh.reshape(B_, H, n_blk, Q_BLOCK, dh).transpose(2, 0, 1, 3, 4)
    Fq = F.reshape(B_, H, n_blk, Q_BLOCK).transpose(2, 0, 1, 3)
    qpos = jnp.arange(Lp, dtype=jnp.int32).reshape(n_blk, Q_BLOCK)
    kpos = jnp.arange(Lp, dtype=jnp.int32)
    scale = FOX_HEAD_DIM ** -0.5

    def block(args):
        qi, Fi, pi = args
        s = jnp.einsum('bhqd,bhkd->bhqk', qi, kh, preferred_element_type=jnp.float32) * scale
        s = s + Fi[..., None] - F[:, :, None, :]
        s = jnp.where(kpos[None, :] <= pi[:, None], s, -jnp.inf)
        p = jax.nn.softmax(s, axis=-1)
        return jnp.einsum('bhqk,bhkd->bhqd', p.astype(vh.dtype), vh)

    o = lax.map(block, (qb, Fq, qpos))
    o = o.transpose(1, 0, 3, 2, 4).reshape(B_, Lp, H * dh)
    return o[:, :L]


def mixer_layer(u, w_in, b_forget, pool_w, pool_b, pool_scale, conv_w, conv_b, conv_ln_g, conv_ln_b,
                w_out_a, w_out_b, w_out_c, w_o):
    B_, L, D = u.shape
    p = u @ w_in
    a, bv, bg, q, k, v, fg, gates = jnp.split(p, [int(i) for i in np.cumsum(IN_SPLITS)[:-1]], axis=-1)
    ya = pool_mixer(a, pool_w, pool_b, pool_scale) @ w_out_a
    yb = conv_module(bv, bg, conv_w, conv_b, conv_ln_g, conv_ln_b) @ w_out_b
    logf = jax.nn.log_sigmoid((fg + b_forget).astype(jnp.float32))
    heads = lambda z: z.reshape(B_, L, FOX_HEADS, FOX_HEAD_DIM)
    yc = forgetting_attention(heads(q), heads(k), heads(v), logf) @ w_out_c
    g = jax.nn.sigmoid(gates.reshape(B_, L, N_BRANCH, D))
    merged = g[:, :, 0] * ya + g[:, :, 1] * yb + g[:, :, 2] * yc
    return merged @ w_o


def hier_moe(h, router_g, router_g_b, router_e, router_e_b, exp_w1, exp_w3, exp_w2):
    B_, L, D = h.shape
    t = h.reshape(B_ * L, D)
    T = t.shape[0]
    glog = (t @ router_g).astype(jnp.float32) + router_g_b.astype(jnp.float32)
    gprob = jax.nn.softmax(glog, axis=-1)
    _, gsel = lax.top_k(glog, 1)
    gw = jnp.take_along_axis(gprob, gsel, axis=-1)
    elog = ((t @ router_e).astype(jnp.float32) + router_e_b.astype(jnp.float32)).reshape(T, N_GROUPS, EXPERTS_PER_GROUP)
    elog_sel = jnp.take_along_axis(elog, gsel[:, :, None], axis=1)[:, 0]
    eprob = jax.nn.softmax(elog_sel, axis=-1)
    top_p, top_i = lax.top_k(eprob, TOP_K)
    top_p = top_p / jnp.sum(top_p, axis=-1, keepdims=True)
    w_grp = jnp.sum(jax.nn.one_hot(top_i, EXPERTS_PER_GROUP, dtype=jnp.float32) * top_p[..., None], axis=1)
    comb = (jax.nn.one_hot(gsel[:, 0], N_GROUPS, dtype=jnp.float32)[:, :, None] * w_grp[:, None, :] * gw[:, :, None])
    comb = comb.reshape(T, N_EXPERTS).astype(t.dtype)
    y = jnp.zeros_like(t)
    for e in range(N_EXPERTS):
        he = jax.nn.silu(t @ exp_w1[e]) * (t @ exp_w3[e])
        y = y + comb[:, e:e + 1] * (he @ exp_w2[e])
    return y.reshape(B_, L, D)


def setup_inputs(seed: int = 0) -> dict:
    key = jax.random.key(seed)
    ks = jax.random.split(key, 32)
    f32 = jnp.float32
    nrm = lambda k, shape, s: (jax.random.normal(k, shape, f32) * s).astype(f32)
    D = D_MODEL
    return {
        "x": nrm(ks[0], (BATCH, SEQ, D), 1.0),
        "meta": nrm(ks[1], (N_META, D), 1.0),
        "norm1_g": 1.0 + nrm(ks[2], (DEPTH, D), 0.02),
        "w_in": nrm(ks[3], (DEPTH, D, N_IN), D ** -0.5),
        "b_forget": jax.random.uniform(ks[4], (DEPTH, FOX_HEADS), f32, 1.0, 6.0),
        "pool_w": nrm(ks[5], (DEPTH, POOL_GROUPS, POOL_GDIM, POOL_GDIM), POOL_GDIM ** -0.5),
        "pool_b": nrm(ks[6], (DEPTH, POOL_GROUPS, POOL_GDIM), 0.01),
        "pool_scale": 1.0 + nrm(ks[7], (DEPTH, POOL_WIDTH), 0.1),
        "conv_w": nrm(ks[8], (DEPTH, CONV_KSIZE, CONV_WIDTH), CONV_KSIZE ** -0.5),
        "conv_b": nrm(ks[9], (DEPTH, CONV_WIDTH), 0.01),
        "conv_ln_g": 1.0 + nrm(ks[10], (DEPTH, CONV_WIDTH), 0.02),
        "conv_ln_b": nrm(ks[11], (DEPTH, CONV_WIDTH), 0.01),
        "w_out_a": nrm(ks[12], (DEPTH, POOL_WIDTH, D), POOL_WIDTH ** -0.5),
        "w_out_b": nrm(ks[13], (DEPTH, CONV_WIDTH, D), CONV_WIDTH ** -0.5),
        "w_out_c": nrm(ks[14], (DEPTH, FOX_WIDTH, D), FOX_WIDTH ** -0.5),
        "w_o": nrm(ks[15], (DEPTH, D, D), D ** -0.5),
        "norm2_g": 1.0 + nrm(ks[16], (DEPTH, D), 0.02),
        "router_g": nrm(ks[17], (DEPTH, D, N_GROUPS), D ** -0.5),
        "router_g_b": nrm(ks[18], (DEPTH, N_GROUPS), 0.01),
        "router_e": nrm(ks[19], (DEPTH, D, N_EXPERTS), D ** -0.5),
        "router_e_b": nrm(ks[20], (DEPTH, N_EXPERTS), 0.01),
        "exp_w1": nrm(ks[21], (DEPTH, N_EXPERTS, D, EXPERT_HIDDEN), D ** -0.5),
        "exp_w3": nrm(ks[22], (DEPTH, N_EXPERTS, D, EXPERT_HIDDEN), D ** -0.5),
        "exp_w2": nrm(ks[23], (DEPTH, N_EXPERTS, EXPERT_HIDDEN, D), EXPERT_HIDDEN ** -0.5),
        "final_g": 1.0 + nrm(ks[24], (D,), 0.02),
    }


def reference(x, meta, norm1_g, w_in, b_forget, pool_w, pool_b, pool_scale, conv_w, conv_b, conv_ln_g,
              conv_ln_b, w_out_a, w_out_b, w_out_c, w_o, norm2_g, router_g, router_g_b, router_e,
              router_e_b, exp_w1, exp_w3, exp_w2, final_g):
    B_ = x.shape[0]
    m = jnp.broadcast_to(meta.astype(x.dtype)[None], (B_, N_META, D_MODEL))
    h = jnp.concatenate([m, x], axis=1)
    for l in range(DEPTH):
        u = rms_norm(h, norm1_g[l])
        h = h + mixer_layer(u, w_in[l], b_forget[l], pool_w[l], pool_b[l], pool_scale[l], conv_w[l],
                            conv_b[l], conv_ln_g[l], conv_ln_b[l], w_out_a[l], w_out_b[l], w_out_c[l], w_o[l])
        v = rms_norm(h, norm2_g[l])
        h = h + hier_moe(v, router_g[l], router_g_b[l], router_e[l], router_e_b[l], exp_w1[l], exp_w3[l], exp_w2[l])
    return rms_norm(h[:, N_META:], final_g)
```

```python
import numpy as np
import ml_dtypes
from contextlib import ExitStack
import concourse.bass as bass
import concourse.mybir as mybir
from concourse.bass_utils import run_bass_kernel_spmd

F32 = mybir.dt.float32
BF16 = mybir.dt.bfloat16
I32 = mybir.dt.int32
SPARSE_MOE = True
ALU = mybir.AluOpType
AF = mybir.ActivationFunctionType

D = 1024
DEPTH = 2
NCORES = 8
CH = 384
NM = 11
OWN = NM * CH
NG = 2 * NM
NGT = NG * 3
NOWNT = NM * 3
N_IN = 5384
EPS = 1e-6
NEG = -30000.0

C_U, C_ONES, C_ODIV, C_SEL, C_MASK, C_INVC, C_ID = 0, 128, 256, 384, 386, 386 + 2304, 386 + 2304 + 1536
C_JT = C_ID + 128
C_TH = C_JT + 82
C_PIDX = C_TH + 33
C_TB = C_PIDX + 1
NCST = C_TB + 20
NSLT = 82
V_G1, V_G2, V_BF, V_RB = 0, 1024, 2048, 2056
NV = 2076
CP_PB, CP_PS, CP_CB, CP_LG, CP_LB, CP_CW = 0, 2, 4, 6, 8, 10
NCP = 10 + 62

SAME_ENGINE_SYNC = True
SEM_CAP = 16000
N_DMA_SEMS = 20
SEM_REARM_WAIT = ("sp", "act")


class Prog:
    def __init__(self, nc, stack):
        self.nc = nc
        self.stack = stack
        self.ops = []
        self.last_w = {}
        self.readers = {}
        self.last_compute = {}
        self.asyncs = []

    def add(self, eng, fn, reads=(), writes=(), kind="c", extra=()):
        i = len(self.ops)
        deps = {}
        for j in extra:
            deps[j] = "raw"
        for r in reads:
            w = self.last_w.get(r)
            if w is not None:
                deps[w] = "raw"
        for k in writes:
            w = self.last_w.get(k)
            if w is not None:
                deps[w] = "raw"
            for _, j in self.readers.get(k, {}).items():
                if j not in deps:
                    deps[j] = "war"
        for r in reads:
            self.readers.setdefault(r, {})[eng if kind == "c" else (eng, i)] = i
        for k in writes:
            self.last_w[k] = i
            self.readers[k] = {}
        self.ops.append(dict(eng=eng, fn=fn, deps=deps, kind=kind))
        if fn is not None:
            if kind == "c":
                self.last_compute[eng] = i
            else:
                self.asyncs.append(i)
        return i

    def emit(self):
        nc = self.nc
        ops = self.ops
        engs = ["pe", "act", "dve", "pool", "sp"]
        known = {e: {} for e in engs}
        needed = set()
        waits = [None] * len(ops)
        for i, op in enumerate(ops):
            E = op["eng"]
            wl = []
            best = {}
            for j, kd in op["deps"].items():
                oj = ops[j]
                if oj["kind"] != "c":
                    if known[E].get(("d", j)):
                        continue
                    known[E][("d", j)] = True
                    wl.append(j)
                    continue
                X = oj["eng"]
                if X == E and op["kind"] == "c":
                    if E == "pe" or kd == "war" or not SAME_ENGINE_SYNC:
                        continue
                if j > best.get(X, -1):
                    best[X] = j
            for X, j in best.items():
                if known[E].get(X, -1) >= j:
                    continue
                known[E][X] = j
                wl.append(j)
            waits[i] = wl
            for j in wl:
                assert ops[j]["fn"] is not None
                needed.add(j)
        sem_of = {}
        cnt = {e: 0 for e in engs}
        eng_sems = {e: [] for e in engs}
        dma_sems = {e: [] for e in engs}
        dma_cnt = {}
        dma_rr = {e: 0 for e in engs}
        dma_last = {}
        nsem = [0]

        def new_sem(tag):
            nsem[0] += 1
            return self.stack.enter_context(nc.semaphore("%s_%d" % (tag, nsem[0])))

        for i, op in enumerate(ops):
            E = op["eng"]
            if op["kind"] == "c":
                if i in needed:
                    n = cnt[E]
                    cnt[E] += 1
                    si = n // SEM_CAP
                    while len(eng_sems[E]) <= si:
                        eng_sems[E].append(new_sem("e" + E))
                    sem_of[i] = (eng_sems[E][si], n % SEM_CAP + 1, 1)
            elif op["kind"] == "d":
                if len(dma_sems[E]) < N_DMA_SEMS:
                    dma_sems[E].append(new_sem("d" + E))
                    dma_cnt[(E, len(dma_sems[E]) - 1)] = 0
                k = dma_rr[E] % len(dma_sems[E]) if len(dma_sems[E]) == N_DMA_SEMS else len(dma_sems[E]) - 1
                dma_rr[E] += 1
                pj = dma_last.get((E, k))
                if pj is not None and pj not in waits[i] and E in SEM_REARM_WAIT:
                    waits[i].append(pj)
                dma_last[(E, k)] = i
                dma_cnt[(E, k)] += 16
                sem_of[i] = (dma_sems[E][k], dma_cnt[(E, k)], 16)
            else:
                sem_of[i] = (new_sem("cc"), 1, None)
        per_eng = {e: [] for e in engs}
        for i, op in enumerate(ops):
            per_eng[op["eng"]].append(i)

        def run(E, e):
            for i in per_eng[E]:
                op = ops[i]
                for j in waits[i]:
                    s, v, _ = sem_of[j]
                    e.wait_ge(s, v)
                if op["fn"] is None:
                    continue
                ins = op["fn"](e)
                if i in sem_of:
                    s, v, inc = sem_of[i]
                    if inc is None:
                        ins.then_inc(s)
                    else:
                        ins.then_inc(s, inc)

        with nc.Block() as block:
            @block.tensor
            def _(e):
                run("pe", e)

            @block.scalar
            def _(e):
                run("act", e)

            @block.vector
            def _(e):
                run("dve", e)

            @block.gpsimd
            def _(e):
                run("pool", e)

            @block.sync
            def _(e):
                run("sp", e)


def build_program(debug=None):
    debug = debug or {}
    nc = bass.Bass("TRN2", target_bir_lowering=False)
    stack = ExitStack()
    P = Prog(nc, stack)

    def din(name, shape, dt=F32):
        return nc.dram_tensor(name, list(shape), dt, kind="ExternalInput").ap()

    h0 = din("h0", [OWN, D])
    cst_d = din("cst", [128, NCST])
    vecs_d = din("vecs", [DEPTH, 128, NV])
    fing_d = din("fing", [128, D])
    chp_d = din("chp", [DEPTH, 128, NCP])
    w_in = din("w_in", [DEPTH, D, N_IN])
    pool_w = din("pool_w", [DEPTH, 4, 64, 64])
    w_oa = din("w_out_a", [DEPTH, 256, D])
    w_ob = din("w_out_b", [DEPTH, 256, D])
    w_oc = din("w_out_c", [DEPTH, 512, D])
    w_o = din("w_o", [DEPTH, D, D])
    rw_d = din("rw", [DEPTH, D, 20])
    ew1 = din("exp_w1", [DEPTH, 16, D, 256])
    ew3 = din("exp_w3", [DEPTH, 16, D, 256])
    ew2 = din("exp_w2", [DEPTH, 16, 256, D])
    out_d = nc.dram_tensor("out", [OWN, D], F32, kind="ExternalOutput").ap()

    hbuf = nc.dram_tensor("hbuf", [OWN, D], F32)
    qx = nc.dram_tensor("qx", [8, 66, OWN], BF16)
    ox = nc.dram_tensor("ox", [512, OWN], BF16)
    NPC = 4
    def pc_of(m):
        return m // 3, m % 3
    PCN = [3, 3, 3, 2]
    kx_loc = [nc.dram_tensor("kx_loc%d" % p, [512, PCN[p] * CH], BF16) for p in range(NPC)]
    kx_all = [nc.dram_tensor("kx_all%d" % p, [1024, PCN[p] * CH], BF16) for p in range(NPC)]
    vx_loc = [nc.dram_tensor("vx_loc%d" % p, [PCN[p] * CH, 520], BF16) for p in range(NPC)]
    vx_all = [nc.dram_tensor("vx_all%d" % p, [2 * PCN[p] * CH, 520], BF16) for p in range(NPC)]
    ag_loc = [nc.dram_tensor("ag_loc%d" % p, [512, PCN[p] * CH], BF16) for p in range(NPC)]
    ag_all = [nc.dram_tensor("ag_all%d" % p, [1024, PCN[p] * CH], BF16) for p in range(NPC)]
    lf_loc = nc.dram_tensor("lf_loc", [NM * 128, 24], F32)
    lf_all = nc.dram_tensor("lf_all", [2 * NM * 128, 24], F32)
    W1s = [nc.dram_tensor("W1s%d" % l, [2048, 2048], BF16) for l in range(DEPTH)]
    W3s = [nc.dram_tensor("W3s%d" % l, [2048, 2048], BF16) for l in range(DEPTH)]
    W2s = [nc.dram_tensor("W2s%d" % l, [2048, 2048], BF16) for l in range(DEPTH)]
    ux = nc.dram_tensor("ux", [NM, 128, 8 * CH], BF16)
    XS = nc.dram_tensor("XS", [NSLT * 128, D], BF16)
    YS = nc.dram_tensor("YS", [NSLT * 128, D], F32)
    scratch = dict(hbuf=hbuf, qx=qx, ox=ox, lf_all=lf_all)
    for p in range(NPC):
        scratch["kx_all%d" % p] = kx_all[p]
        scratch["vx_all%d" % p] = vx_all[p]
        scratch["ag_all%d" % p] = ag_all[p]
    dbg_out = {}
    for nm in debug.get("dump", []):
        t = scratch[nm]
        dbg_out[nm] = nc.dram_tensor("dbg_" + nm, list(t.shape), t.dtype, kind="ExternalOutput")
    if "moe" in debug.get("dump_sb", []):
        dbg_out["moe"] = nc.dram_tensor("dbg_moe", [128, 512], F32, kind="ExternalOutput")
    if "F" in debug.get("dump_sb", []):
        dbg_out["F"] = nc.dram_tensor("dbg_F", [128, 528], F32, kind="ExternalOutput")

    def sb(name, shape, dt=F32):
        return stack.enter_context(nc.sbuf_tensor("sb_" + name, list(shape), dt))

    cst = sb("cst", [128, NCST])
    ident = sb("ident", [128, 128], BF16)
    vecs = sb("vecs", [128, NV])
    chp = sb("chp", [128, NCP])
    fing = sb("fing", [128, D])
    small = sb("small", [128, 64])
    FTP = sb("FTP", [128, 528 + 528 + 88 + 132])
    AR_WORDS = 44128
    AR = sb("AR", [128, AR_WORDS])

    class Arena:
        def __init__(self):
            self.off = 0

        def f32(self, n, parts=128):
            a = AR[0:parts, self.off:self.off + n]
            self.off += (n + 7) // 8 * 8
            assert self.off <= AR_WORDS, self.off
            return a

        def bf16(self, n, parts=128):
            w = (n + 1) // 2
            a = AR[0:parts, self.off:self.off + w].bitcast(BF16)
            self.off += (w + 7) // 8 * 8
            assert self.off <= AR_WORDS, self.off
            return a[:, 0:n]

    ps = [stack.enter_context(nc.psum_tensor("ps%d" % i, [128, 512], F32)) for i in range(7)]
    psT = stack.enter_context(nc.psum_tensor("psT", [128, 1024], BF16))

    def barrier():
        engs = ["pe", "act", "dve", "pool", "sp"]
        lasts = [i for i in (P.last_compute.get(x) for x in engs) if i is not None]
        asyncs = list(P.asyncs)
        P.asyncs = []
        for E in engs:
            P.add(E, None, (), (), extra=lasts + asyncs)

    rr = [0]

    def nbank(lo=0, hi=5):
        b = lo + rr[0] % (hi - lo)
        rr[0] += 1
        return b

    def psk(b):
        return ("ps", b)

    def dma(q, out, in_, reads, writes):
        return P.add(q, lambda e: e.dma_start(out=out, in_=in_), reads, writes, kind="d")

    def mm(out, lhsT, rhs, start, stop, reads, writes):
        return P.add("pe", lambda e: e.matmul(out, lhsT, rhs, start=start, stop=stop, skip_group_check=True),
                     reads, writes)

    def act(out, in_, func, reads, writes, bias=None, scale=None):
        kw = {}
        if bias is not None:
            kw["bias"] = bias
        if scale is not None:
            kw["scale"] = scale
        return P.add("act", lambda e: e.activation(out, in_, func, **kw), reads, writes)

    def tt(eng, out, in0, in1, op, reads, writes):
        return P.add(eng, lambda e: e.tensor_tensor(out, in0, in1, op), reads, writes)

    def ts(eng, out, in0, s1, s2, op0, op1, reads, writes):
        if op1 is None:
            return P.add(eng, lambda e: e.tensor_scalar(out, in0, s1, None, op0), reads, writes)
        return P.add(eng, lambda e: e.tensor_scalar(out, in0, s1, s2, op0, op1), reads, writes)

    def stt(out, in0, scalar, in1, op0, op1, reads, writes, accum=None):
        return P.add("dve", lambda e: e.scalar_tensor_tensor(out, in0, scalar, in1, op0, op1, accum_out=accum),
                     reads, writes)

    def cp(eng, out, in_, reads, writes):
        if eng == "act":
            return P.add("act", lambda e: e.copy(out, in_), reads, writes)
        return P.add(eng, lambda e: e.tensor_copy(out, in_), reads, writes)

    def memset(eng, ap, val, writes):
        return P.add(eng, lambda e: e.memset(ap, val), (), writes)

    def rsqrt(out, in_, scale, reads, writes):
        act(out, in_, AF.Sqrt, list(reads) + ["epsc"], writes, bias=epsc[0:out.shape[0], :], scale=scale)
        P.add("dve", lambda e: e.reciprocal(out, out), writes, writes)

    def v3(ap, a):
        return ap.rearrange("p (a b) -> p a b", a=a)

    dma("sp", cst[:, :], cst_d[:, :], (), ["cst"])
    dma("sp", fing[:, :], fing_d[:, :], (), ["fing"])
    cp("dve", ident[:, :], cst[:, C_ID:C_ID + 128], ["cst"], ["ident"])
    epsc = small[:, 60:61]
    onec = small[:, 61:62]
    memset("dve", epsc, EPS, ["epsc"])
    memset("dve", onec, 1.0, ["onec"])
    selA = cst[:, C_SEL:C_SEL + 1]
    selB = cst[:, C_SEL + 1:C_SEL + 2]

    def maskap(w, kt, qlo):
        o = C_MASK + (w * 3 + kt) * CH
        return cst[:, o + qlo:o + CH]

    FT = FTP[:, 0:528].rearrange("p (g h) -> p g h", h=8)
    CY = FTP[:, 528:1056].rearrange("p (g h) -> p g h", h=8)
    CS = FTP[:, 1056:1144].rearrange("p (m h) -> p m h", h=8)
    BI = [FTP[:, 1144 + i * 66:1144 + (i + 1) * 66] for i in range(2)]

    def load_h(src, m, htile, key):
        dma("sp", htile, src[m * CH:(m + 1) * CH, :].rearrange("(i p) d -> p i d", p=128),
            (("hsrc", m),), [key])

    def norm_to_uT(htile, hkey, ubf, U, ukey, goff, l):
        for i in range(3):
            ss = small[:, i:i + 1]
            stt(ubf, htile[:, i, :], 1.0, htile[:, i, :], ALU.mult, ALU.mult,
                [hkey], ["ubf", ("ss", i)], accum=ss)
            rs = small[:, 4 + i:5 + i]
            rsqrt(rs, ss, 1.0 / D, [("ss", i)], [("rs", i)])
            stt(ubf, htile[:, i, :], rs, vecs[:, goff:goff + D], ALU.mult, ALU.mult,
                [hkey, ("rs", i), ("vecs", l)], ["ubf"])
            for k in range(8):
                P.add("pe", lambda e, k=k: e.transpose(psT[:, k * 128:(k + 1) * 128], ubf[:, k * 128:(k + 1) * 128],
                                                       ident[:, :]),
                      ["ubf", "ident"], ["psT"])
            cp("act", U[:, :, i * 128:(i + 1) * 128], psT[:, :].rearrange("p (k t) -> p k t", k=8),
               ["psT"], [ukey])

    def norm_dve(htile, hkey, i, ub, goff, l):
        ss = small[:, i:i + 1]
        stt(ub, htile[:, i, :], 1.0, htile[:, i, :], ALU.mult, ALU.mult, [hkey], [("ub3", i), ("ss", i)], accum=ss)
        rs = small[:, 4 + i:5 + i]
        rsqrt(rs, ss, 1.0 / D, [("ss", i)], [("rs", i)])
        stt(ub, htile[:, i, :], rs, vecs[:, goff:goff + D], ALU.mult, ALU.mult,
            [hkey, ("rs", i), ("vecs", l)], [("ub3", i)])

    def norm_pe(ub, i, U, ukey):
        for k in range(8):
            P.add("pe", lambda e, k=k: e.transpose(psT[:, k * 128:(k + 1) * 128], ub[:, k * 128:(k + 1) * 128],
                                                   ident[:, :]),
                  [("ub3", i), "ident"], ["psT"])
        cp("act", U[:, :, i * 128:(i + 1) * 128], psT[:, :].rearrange("p (k t) -> p k t", k=8), ["psT"], [ukey])

    def load_layer_vecs(l):
        dma("sp", vecs[:, :], vecs_d[l, :, :], (), [("vecs", l)])
        dma("sp", chp[:, :], chp_d[l, :, :], (), [("chp", l)])

    def phase1(l, hsrc):
        A = Arena()
        W1 = A.bf16(8 * 2312).rearrange("p (k c) -> p k c", k=8)
        ht = [v3(A.f32(3 * D), 3) for _ in range(2)]
        ubf = A.bf16(D)
        ub3 = [A.bf16(D) for _ in range(3)]
        uT = [v3(A.bf16(8 * CH), 8) for _ in range(2)]
        sg = v3(A.f32(2 * CH), 2)
        ab = v3(A.bf16(2 * CH), 2)
        gb = v3(A.bf16(2 * CH), 2)
        kb = v3(A.bf16(4 * CH), 4)
        qb = v3(A.bf16(4 * CH), 4)
        vts = [A.bf16(520) for _ in range(2)]
        lts = [A.f32(24) for _ in range(2)]
        for (c0, c1) in ((0, 768), (768, 1280), (1280, 1792), (1792, 2312)):
            dma("pool", W1[:, :, c0:c1], w_in[l, :, c0:c1].rearrange("(k p) c -> p k c", p=128),
                (), [("W1", c0)])
        wkeys = [("W1", 0), ("W1", 768), ("W1", 1280), ("W1", 1792)]
        for i in range(2):
            memset("pool", vts[i], 1.0, [("vtile", i)])
        stg = [(A.bf16(2048), A.bf16(2048), A.bf16(2048)) for _ in range(4)]

        def prep_load(e):
            a1, a3, a2 = stg[e % 4]
            dma("pool", v3(a1, 8), ew1[l, e, :, :].rearrange("(k p) c -> p k c", p=128), (), [("pw1", e % 4)])
            dma("pool", v3(a3, 8), ew3[l, e, :, :].rearrange("(k p) c -> p k c", p=128), (), [("pw3", e % 4)])
            dma("pool", v3(a2, 2), ew2[l, e, :, :].rearrange("(k p) c -> p k c", p=128), (), [("pw2", e % 4)])

        def prep_store(e):
            a1, a3, a2 = stg[e % 4]
            dma("sp", W1s[l][e * 128:(e + 1) * 128, :], a1, [("pw1", e % 4)], [("W1s", e)])
            dma("sp", W3s[l][e * 128:(e + 1) * 128, :], a3, [("pw3", e % 4)], [("W3s", e)])
            dma("sp", W2s[l][e * 128:(e + 1) * 128, :], a2, [("pw2", e % 4)], [("W2s", e)])
        load_h(hsrc, 0, ht[0], ("ht", 0))
        for m in range(NM):
            hb = m % 2
            if m + 1 < NM:
                load_h(hsrc, m + 1, ht[1 - hb], ("ht", 1 - hb))
            U = uT[hb]
            ukey = ("uT", hb)
            if m < 8:
                prep_load(2 * m)
                prep_load(2 * m + 1)
            if 1 <= m < 9:
                prep_store(2 * m - 2)
                prep_store(2 * m - 1)
            if m == 0:
                for i in range(3):
                    norm_dve(ht[hb], ("ht", hb), i, ub3[i], V_G1, l)
                    norm_pe(ub3[i], i, U, ukey)
            dma("sp", ux[m, :, :], U.rearrange("p k t -> p (k t)"), [ukey], [("ux", m)])
            nxt = m + 1 < NM

            def early_norm(i):
                if nxt:
                    norm_dve(ht[1 - hb], ("ht", 1 - hb), i, ub3[i], V_G1, l)
            osl = slice(m * CH, (m + 1) * CH)
            pp, pm = pc_of(m)
            psl = slice(pm * CH, (pm + 1) * CH)
            p1stop = debug.get("p1stop", 99)
            if p1stop < 1:
                continue

            def fm_group(c0, M):
                b = nbank()
                for k in range(8):
                    mm(ps[b][0:M, 0:CH], W1[:, k, c0:c0 + M], U[:, k, :], k == 0, k == 7,
                       wkeys + [ukey], [psk(b)])
                return b
            for cc in range(2):
                b = fm_group(cc * 128, 128)
                cp("act", ab[:, cc, :], ps[b][:, 0:CH], [psk(b)], [("ab", cc)])
            dma("sp", ag_loc[pp][0:256, psl].rearrange("(c p) t -> p c t", p=128), ab,
                [("ab", 0), ("ab", 1)], [("ag_loc", m, 0)])
            early_norm(0)
            for cc in range(2):
                b2 = fm_group(512 + cc * 128, 128)
                act(sg[:, cc, :], ps[b2][:, 0:CH], AF.Sigmoid, [psk(b2)], [("sg", cc)])
                b1 = fm_group(256 + cc * 128, 128)
                tt("dve", gb[:, cc, :], ps[b1][:, 0:CH], sg[:, cc, :], ALU.mult, [psk(b1), ("sg", cc)], [("gb", cc)])
            dma("sp", ag_loc[pp][256:512, psl].rearrange("(c p) t -> p c t", p=128), gb,
                [("gb", 0), ("gb", 1)], [("ag_loc", m, 1)])
            early_norm(1)
            for kp in range(4):
                b = fm_group(768 + kp * 128, 128)
                P.add("act", lambda e, b=b, kp=kp: e.mul(qb[:, kp, :], ps[b][:, 0:CH], 0.125),
                      [psk(b)], [("qb", kp)])
                b = fm_group(1280 + kp * 128, 128)
                cp("dve", kb[:, kp, :], ps[b][:, 0:CH], [psk(b)], [("kb", kp)])
            for h in range(8):
                dma("sp", qx[h, 0:64, osl], qb[(h % 2) * 64:(h % 2) * 64 + 64, h // 2, :],
                    [("qb", h // 2)], [("qx", m, h)])
            dma("sp", kx_loc[pp][:, psl].rearrange("(k p) t -> p k t", p=128), kb[:, :, :],
                [("kb", kp) for kp in range(4)], [("kx_loc", m)])
            early_norm(2)
            for i in range(3):
                vb = (m * 3 + i) % 2
                vt = vts[vb].rearrange("p (h d) -> p h d", h=8)
                b = nbank()
                for k in range(8):
                    mm(ps[b][:, 0:512], U[:, k, i * 128:(i + 1) * 128], W1[:, k, 1792:2304], k == 0, k == 7,
                       wkeys + [ukey], [psk(b)])
                cp("act", vt[:, :, 0:64], ps[b][:, 0:512].rearrange("p (h d) -> p h d", h=8), [psk(b)],
                   [("vtile", vb)])
                r0 = pm * CH + i * 128
                dma("sp", vx_loc[pp][r0:r0 + 128, :], vts[vb], [("vtile", vb)], [("vx_loc", m, i)])
                if p1stop < 5:
                    continue
                b = nbank()
                for k in range(8):
                    mm(ps[b][:, 0:8], U[:, k, i * 128:(i + 1) * 128], W1[:, k, 2304:2312], k == 0, k == 7,
                       wkeys + [ukey], [psk(b)])
                lt = lts[m % 2][:, i * 8:(i + 1) * 8]
                vb = (m % 2, i)
                tt("dve", lt, ps[b][:, 0:8], vecs[:, V_BF:V_BF + 8], ALU.add, [psk(b), ("vecs", l)], [("lt", vb)])
                if p1stop < 6:
                    continue
                if not debug.get("noact"):
                    act(lt, lt, AF.Exp, [("lt", vb)], [("lt", vb)], scale=-1.0)
                    act(lt, lt, AF.Ln, [("lt", vb), "onec"], [("lt", vb)], bias=onec)
                    ts("dve", lt, lt, -1.0, None, ALU.mult, None, [("lt", vb)], [("lt", vb)])
            dma("sp", lf_loc[m * 128:(m + 1) * 128, :], lts[m % 2], [("lt", (m % 2, i)) for i in range(3)],
                [("lf_loc", m)])
            if nxt:
                for i in range(3):
                    norm_pe(ub3[i], i, uT[1 - hb], ("uT", 1 - hb))
            if m == NM - 1 or (m + 1) % 3 == 0:
                exchange_piece(m // 3)

    groups = [[0, 1], [2, 3], [4, 5], [6, 7]]

    def cc(src, dst, rkeys, wkey):
        P.add("pool", lambda e: e.collective_compute("AllGather", ALU.bypass, replica_groups=groups,
                                                     ins=[src.ap().opt()], outs=[dst.ap().opt()]),
              rkeys, [wkey], kind="cc")

    def exchange_piece(p):
        ms = [m for m in range(NM) if m // 3 == p]
        cc(kx_loc[p], kx_all[p], [("kx_loc", m) for m in ms], ("kx_all", p))
        cc(vx_loc[p], vx_all[p], [("vx_loc", m, i) for m in ms for i in range(3)], ("vx_all", p))
        cc(ag_loc[p], ag_all[p], [("ag_loc", m, j) for m in ms for j in range(2)], ("ag_all", p))

    def exchange():
        cc(lf_loc, lf_all, [("lf_loc", m) for m in range(NM)], "lf_all")

    def phase2a():
        A = Arena()
        LFf = A.f32(528)
        LF = LFf.rearrange("p (m r i h) -> p m r i h", m=NM, r=2, i=3)
        TT = A.f32(528)
        INC = A.f32(528)
        ONE = A.f32(66)
        DSf = A.f32(264)
        DS = DSf.rearrange("p (j h) -> p j h", h=8)
        DH = A.bf16(264)
        DL = A.bf16(264)
        TRB = v3(A.bf16(3 * 128), 3)
        for r in range(2):
            dma("sp", LFf.rearrange("p (m r x) -> p m r x", m=NM, r=2)[:, :, r, :],
                lf_all[r * NM * 128:(r + 1) * NM * 128, :].rearrange("(m p) x -> p m x", p=128),
                ["lf_all"], [("LF", r)])
        lk = [("LF", 0), ("LF", 1)]
        memset("dve", ONE, 1.0, ["ONE"])
        FTf = FT.rearrange("p g h -> p (g h)")
        CYf = CY.rearrange("p g h -> p (g h)")
        for half in range(2):
            cs = slice(half * 264, (half + 1) * 264)
            mm(ps[half][:, 0:264], cst[:, C_U:C_U + 128], LFf[:, cs], True, True, lk + ["cst"], [psk(half)])
            mm(ps[2 + half][:, 0:264], cst[:, C_ONES:C_ONES + 128], LFf[:, cs], True, True, lk + ["cst"],
               [psk(2 + half)])
            cp("act", FTf[:, cs], ps[half][:, 0:264], [psk(half)], [("FTw", half)])
            cp("dve", TT[:, cs], ps[2 + half][:, 0:264], [psk(2 + half)], [("TT", half)])
        TT3 = TT.rearrange("p (g h) -> p g h", h=8)
        INC3 = INC.rearrange("p (g h) -> p g h", h=8)
        for h in range(8):
            P.add("dve", lambda e, h=h: e.tensor_tensor_scan(INC3[:, :, h], ONE, TT3[:, :, h], 0.0, ALU.mult, ALU.add),
                  [("TT", 0), ("TT", 1), "ONE"], [("INC", h)])
        ik = [("INC", h) for h in range(8)]
        tt("dve", CYf, INC, TT, ALU.subtract, ik + [("TT", 0), ("TT", 1)], ["CY"])
        tt("dve", FTf, FTf, CYf, ALU.add, [("FTw", 0), ("FTw", 1), "CY"], ["FT"])
        CY4 = CY.rearrange("p (m r i) h -> p m r i h", r=2, i=3)
        FT4 = FT.rearrange("p (m r i) h -> p m r i h", r=2, i=3)
        ts("dve", CS, CY4[:, :, 0, 0, :], selA, None, ALU.mult, None, ["CY", "cst"], ["CS"])
        stt(CS, CY4[:, :, 1, 0, :], selB, CS, ALU.mult, ALU.add, ["CY", "cst", "CS"], ["CS"])
        DS4 = DS.rearrange("p (m i) h -> p m i h", i=3)
        for i in range(3):
            ts("dve", DS4[:, :, i, :], FT4[:, :, 0, i, :], selA, None, ALU.mult, None, ["FT", "cst"], [("DS", i)])
            stt(DS4[:, :, i, :], FT4[:, :, 1, i, :], selB, DS4[:, :, i, :], ALU.mult, ALU.add,
                ["FT", "cst", ("DS", i)], [("DS", i)])
            tt("dve", DS4[:, :, i, :], DS4[:, :, i, :], CS, ALU.subtract, [("DS", i), "CS"], [("DS", i)])
        dk = [("DS", i) for i in range(3)]
        cp("dve", DH, DSf, dk, ["DH"])
        tt("dve", DSf, DSf, DH, ALU.subtract, dk + ["DH"], ["DS2"] + dk)
        cp("dve", DL, DSf, ["DS2"], ["DL"])
        for which, src in ((0, DH), (1, DL)):
            for a, (c0, n) in enumerate(((0, 128), (128, 128), (256, 8))):
                P.add("pe", lambda e, a=a, c0=c0, n=n, src=src: e.transpose(psT[0:n, a * 128:(a + 1) * 128],
                                                                             src[:, c0:c0 + n], ident[:, :]),
                      ["DH", "DL", "ident"], ["psT"])
            cp("act", TRB[:, :, :], psT[:, 0:384].rearrange("p (a t) -> p a t", a=3), ["psT"], ["TRB"])
            for a, nj in ((0, 16), (1, 16), (2, 1)):
                for jj in range(nj):
                    j = a * 16 + jj
                    dma("sp", qx[:, 64 + which, j * 128:(j + 1) * 128],
                        TRB[jj * 8:(jj + 1) * 8, a, :], ["TRB"], [("qxaug", which, j)])
        if "F" in dbg_out:
            dma("sp", dbg_out["F"][:, :], FTf, ["FT"], ["dbgF"])

    def phase2b():
        A = Arena()
        QH = [A.bf16(OWN, parts=66) for _ in range(2)]
        KT = [A.bf16(NG * CH, parts=66).rearrange("p (g t) -> p g t", t=CH) for _ in range(2)]
        VT = [A.bf16(NGT * 65).rearrange("p (m r i d) -> p m r i d", r=2, i=3, d=65) for _ in range(2)]
        PT = [A.bf16(CH) for _ in range(10)]
        MT = [A.f32(CH) for _ in range(2)]
        OS2 = [A.f32(CH) for _ in range(2)]
        RC2 = [A.f32(CH) for _ in range(2)]
        OB = [A.bf16(CH) for _ in range(2)]
        BI3 = [A.f32(72) for _ in range(3)]
        qk = [("qx", m, h_) for m in range(NM) for h_ in range(8)] + \
            [("qxaug", w, j) for w in range(2) for j in range(NOWNT)]
        for i in range(2):
            memset("pool", KT[i][64:66, :, :], 1.0, [("KTones", i)])

        def load_kv(h, hb):
            dma("act", QH[hb][0:66, :], qx[h, :, :], qk, [("QH", hb)])
            for r in range(2):
                for p in range(NPC):
                    n = PCN[p]
                    dma("act", KT[hb][0:64, :, :].rearrange("p (m r) t -> p m r t", r=2)[:, 3 * p:3 * p + n, r, :],
                        kx_all[p][r * 512 + h * 64:r * 512 + (h + 1) * 64, :].rearrange("p (m t) -> p m t", t=CH),
                        [("kx_all", p)], [("KT", hb, r, p)])
                    for i in range(3):
                        dma("act", VT[hb][:, 3 * p:3 * p + n, r, i, :],
                            vx_all[p][r * n * CH:(r + 1) * n * CH, h * 65:(h + 1) * 65].rearrange(
                                "(m i p) d -> p m i d", i=3, p=128)[:, :, i, :],
                            [("vx_all", p)], [("VT", hb, r, p, i)])
        load_kv(0, 0)
        LAG = 4
        items = []
        for h in range(8):
            for m in range(NM):
                for gt in range(6 * m + 6):
                    items.append((h, m, gt))
        state = {}
        pend = {}
        NPT = len(PT)

        def s_stage(idx):
            h, m, gt = items[idx]
            hb = h % 2
            it = h * NM + m
            bi = it % 3
            if gt == 0:
                ts("dve", BI3[bi][:, 0:66], FT[:, :, h], -1.0, CS[:, m, h:h + 1], ALU.mult, ALU.add, ["FT", "CS"],
                   [("BI", bi)])
            kvk = [("KTones", hb), ("QH", hb)] + [("KT", hb, r, p) for r in range(2) for p in range(NPC)]
            G, kt = gt // 3, gt % 3
            band = G - 2 * m
            qlo = kt * 128 if band == 1 else 0
            ncol = CH - qlo
            q0 = m * CH
            b = nbank()
            mm(ps[b][:, 0:ncol], KT[hb][0:66, G, kt * 128:(kt + 1) * 128], QH[hb][0:66, q0 + qlo:q0 + CH],
               True, True, kvk, [psk(b)])
            pt = PT[idx % NPT]
            pk = ("PT", idx % NPT)
            if band >= 0:
                mt = MT[idx % 2]
                mk = ("MT", idx % 2)
                tt("dve", mt[:, 0:ncol], ps[b][:, 0:ncol], maskap(band, kt, qlo), ALU.add, [psk(b), "cst"], [mk])
                act(pt[:, 0:ncol], mt[:, 0:ncol], AF.Exp, [mk, ("BI", bi)], [pk], bias=BI3[bi][:, gt:gt + 1])
            else:
                act(pt[:, 0:ncol], ps[b][:, 0:ncol], AF.Exp, [psk(b), ("BI", bi)], [pk],
                    bias=BI3[bi][:, gt:gt + 1])
            state[idx] = (pt, pk, qlo, ncol)

        def pv_stage(idx, step):
            h, m, gt = items[idx]
            hb = h % 2
            it = h * NM + m
            ob = 5 + it % 2
            ngt = 6 * m + 6
            pt, pk, qlo, ncol = state.pop(idx)
            if gt == 0 and m == 0 and h + 1 < 8:
                load_kv(h + 1, 1 - hb)
            vk = [("VT", hb, r, p, i) for r in range(2) for p in range(NPC) for i in range(3)]
            Vt = VT[hb].rearrange("p m r i d -> p (m r i) d")
            mm(ps[ob][0:65, qlo:CH], Vt[:, gt, 0:65], pt[:, 0:ncol], gt == 0, gt == ngt - 1, vk + [pk], [psk(ob)])
            if gt == ngt - 1:
                ri = it % 2
                P.add("dve", lambda e: e.reciprocal(RC2[ri][64:65, :], ps[ob][64:65, 0:CH]), [psk(ob)], [("RC", ri)])
                cp("act", OS2[ri][0:64, :], ps[ob][0:64, 0:CH], [psk(ob)], [("OS", ri)])
                pend.setdefault(step + 2, []).append((h, m, ri))

        def norm2(h, m, ri):
            q0 = m * CH
            b = nbank()
            mm(ps[b][0:64, 0:CH], cst[64:65, C_ONES:C_ONES + 64], RC2[ri][64:65, :], True, True, [("RC", ri), "cst"],
               [psk(b)])
            obt = OB[ri]
            tt("dve", obt[0:64, :], OS2[ri][0:64, :], ps[b][0:64, 0:CH], ALU.mult, [("OS", ri), psk(b)], [("OB", ri)])
            dma("sp", ox[h * 64:(h + 1) * 64, q0:q0 + CH], obt[0:64, :], [("OB", ri)], [("ox", h, m)])

        nit = len(items)
        for step in range(nit + LAG + 3):
            if step < nit:
                s_stage(step)
            if 0 <= step - LAG < nit:
                pv_stage(step - LAG, step)
            for a in pend.pop(step, []):
                norm2(*a)
        assert not pend and not state

    def phase3a(l, hsrc):
        A = Arena()
        WG = v3(A.bf16(8 * 3072), 8)
        WOA = v3(A.bf16(2 * 1024), 2)
        WOB = v3(A.bf16(2 * 1024), 2)
        WOC = v3(A.bf16(4 * 1024), 4)
        WO = v3(A.bf16(8 * 1024), 8)
        BD = v3(A.bf16(256), 2)
        DG = A.bf16(62 * 128).rearrange("p (j d) -> p j d", d=128)
        htile = v3(A.f32(3 * D), 3)
        U2 = [v3(A.bf16(8 * CH), 8) for _ in range(2)]
        XA = v3(A.f32(800), 2)
        S2 = v3(A.f32(800), 2)
        S4 = v3(A.f32(800), 2)
        YC = v3(A.f32(768), 2)
        YQ = v3(A.f32(768), 2)
        MEAN = A.f32(CH)
        RSTD = A.f32(CH)
        T1 = A.f32(CH)
        T2 = A.f32(CH)
        T3 = A.f32(CH)
        HA = v3(A.f32(64), 2)
        SGB = [A.f32(CH) for _ in range(3)]
        XG = v3(A.bf16(832), 2)
        DF = v3(A.bf16(768), 2)
        YP = v3(A.bf16(768), 2)
        YB = v3(A.bf16(768), 2)
        HB0 = v3(A.bf16(128), 2)
        HB1 = v3(A.bf16(128), 2)
        MG = v3(A.bf16(8 * CH), 8)
        AB = v3(A.bf16(2 * CH), 2)
        OC = v3(A.bf16(4 * CH), 4)
        pbs = small[:, 40:42]
        for g3 in range(3):
            dma("pool", WG[:, :, g3 * 1024:(g3 + 1) * 1024],
                w_in[l, :, 2312 + g3 * 1024:2312 + (g3 + 1) * 1024].rearrange("(k p) c -> p k c", p=128),
                (), [("WG", g3)])
        dma("pool", WOA, w_oa[l, :, :].rearrange("(k p) c -> p k c", p=128), (), ["WOA"])
        dma("pool", WOB, w_ob[l, :, :].rearrange("(k p) c -> p k c", p=128), (), ["WOB"])
        dma("pool", WOC, w_oc[l, :, :].rearrange("(k p) c -> p k c", p=128), (), ["WOC"])
        dma("pool", WO, w_o[l, :, :].rearrange("(k p) c -> p k c", p=128), (), ["WO"])
        memset("dve", BD, 0.0, ["BD0"])
        for g in range(4):
            r0 = (g % 2) * 64
            dma("pool", BD[r0:r0 + 64, g // 2, r0:r0 + 64], pool_w[l, g, :, :], ["BD0"], [("BD", g)])
        bdk = [("BD", g) for g in range(4)]
        for cc in range(2):
            for j in range(31):
                ts("dve", DG[:, cc * 31 + j, :], ident[:, :], chp[:, CP_CW + cc * 31 + j:CP_CW + cc * 31 + j + 1],
                   None, ALU.mult, None, ["ident", ("chp", l)], [("DG", cc)])
        tt("dve", pbs, chp[:, CP_PB:CP_PB + 2], chp[:, CP_PS:CP_PS + 2], ALU.mult, [("chp", l)], ["pbs"])
        wgk = [("WG", g3) for g3 in range(3)]
        YP2 = [YP, v3(A.bf16(768), 2)]
        YB2 = [YB, v3(A.bf16(768), 2)]
        OC2 = [OC, v3(A.bf16(4 * CH), 4)]

        def prep(m, s_):
            YP_, YB_, OC_ = YP2[s_], YB2[s_], OC2[s_]
            osl = slice(m * CH, (m + 1) * CH)
            pp, pm = pc_of(m)
            psl = slice(pm * CH, (pm + 1) * CH)
            dma("sp", XG[:, :, 32:416], ag_loc[pp][256:512, psl].rearrange("(c p) t -> p c t", p=128),
                [("ag_loc", m, 1)], ["XGo"])
            dma("sp", AB, ag_loc[pp][0:256, psl].rearrange("(c p) t -> p c t", p=128),
                [("ag_loc", m, 0)], ["ABo"])
            dma("sp", OC_, ox[:, osl].rearrange("(k p) t -> p k t", p=128),
                [("ox", h, m) for h in range(8)], [("OC", s_)])
            if m > 0:
                qp, qm = pc_of(m - 1)
                e0 = (qm + 1) * CH
                dma("sp", HB0[:, :, 0:32],
                    ag_all[qp][768:1024, e0 - 32:e0].rearrange("(c p) t -> p c t", p=128),
                    [("ag_all", qp)], ["HB0g"])
                dma("sp", HB0[:, :, 32:48],
                    ag_all[qp][512:768, e0 - 16:e0].rearrange("(c p) t -> p c t", p=128),
                    [("ag_all", qp)], ["HB0a"])
            else:
                memset("pool", HB0, 0.0, ["HB0g", "HB0a"])
            e1 = (pm + 1) * CH
            dma("sp", HB1[:, :, 0:32],
                ag_all[pp][256:512, e1 - 32:e1].rearrange("(c p) t -> p c t", p=128),
                [("ag_all", pp)], ["HB1g"])
            dma("sp", HB1[:, :, 32:48],
                ag_all[pp][0:256, e1 - 16:e1].rearrange("(c p) t -> p c t", p=128),
                [("ag_all", pp)], ["HB1a"])
            hk = ["HB0g", "HB0a", "HB1g", "HB1a"]
            ts("dve", HA[:, :, 0:32], HB0[:, :, 0:32], selA, None, ALU.mult, None, hk + ["cst"], ["HAg"])
            stt(XG[:, :, 0:32], HB1[:, :, 0:32], selB, HA[:, :, 0:32], ALU.mult, ALU.add, hk + ["cst", "HAg"], ["XGh"])
            ts("dve", HA[:, :, 0:16], HB0[:, :, 32:48], selA, None, ALU.mult, None, hk + ["cst", "XGh"], ["HAa"])
            stt(XA[:, :, 0:16], HB1[:, :, 32:48], selB, HA[:, :, 0:16], ALU.mult, ALU.add, hk + ["cst", "HAa"], ["XAh"])
            cp("dve", XA[:, :, 16:400], AB, ["ABo"], ["XAo"])
            xak = ["XAh", "XAo"]
            tt("dve", S2[:, :, 1:400], XA[:, :, 1:400], XA[:, :, 0:399], ALU.add, xak, ["S2"])
            tt("dve", S4[:, :, 3:400], S2[:, :, 3:400], S2[:, :, 1:398], ALU.add, ["S2"], ["S4"])
            tt("dve", S2[:, 1, 7:400], S4[:, 1, 7:400], S4[:, 1, 3:396], ALU.add, ["S4", "S2"], ["S8", "S2"])
            tt("dve", S4[64:128, 1, 15:400], S2[64:128, 1, 15:400], S2[64:128, 1, 7:392], ALU.add, ["S8", "S4"],
               ["S16", "S4"])
            ico = C_INVC + (0 if m == 0 else 768)
            IC = cst[:, ico:ico + 768].rearrange("p (c t) -> p c t", c=2)
            wk = ["S2", "S4", "S8", "S16", "cst"]
            PT1 = YC[:, 0, :]
            for cc in range(2):
                tt("dve", PT1[0:64, :], S2[0:64, cc, 16:400], IC[0:64, cc, :], ALU.mult, wk, [("YC", 0)])
                tt("dve", PT1[64:128, :], S4[64:128, cc, 16:400], IC[64:128, cc, :], ALU.mult, wk + [("YC", 0)],
                   [("YC", 0)])
                tt("dve", DF[:, cc, :], PT1, XA[:, cc, 16:400], ALU.subtract, [("YC", 0)] + xak, [("DF", cc)])
            yield
            for cc in range(2):
                b = nbank(0, 7)
                mm(ps[b][:, 0:CH], BD[:, cc, :], DF[:, cc, :], True, True, bdk + [("DF", cc)], [psk(b)])
                act(YP_[:, cc, :], ps[b][:, 0:CH], AF.Identity, [psk(b), ("chp", l), "pbs"], [("YP", s_, cc)],
                    bias=pbs[:, cc:cc + 1], scale=chp[:, CP_PS + cc:CP_PS + cc + 1])
            for cc in range(2):
                b = nbank(0, 7)
                for j in range(31):
                    mm(ps[b][:, 0:CH], DG[:, cc * 31 + j, :], XG[:, cc, 2 + j:2 + j + CH], j == 0, j == 30,
                       [("DG", 0), ("DG", 1), "XGo", "XGh"], [psk(b)])
                act(YC[:, cc, :], ps[b][:, 0:CH], AF.Identity, [psk(b), ("chp", l)], [("YC", cc)],
                    bias=chp[:, CP_CB + cc:CP_CB + cc + 1])
                act(YQ[:, cc, :], YC[:, cc, :], AF.Square, [("YC", cc)], [("YQ", cc)])
            bm = nbank(0, 7)
            bq = nbank(0, 7)
            for cc in range(2):
                mm(ps[bm][:, 0:CH], cst[:, C_ODIV:C_ODIV + 128], YC[:, cc, :], cc == 0, cc == 1, ["cst", ("YC", cc)],
                   [psk(bm)])
            for cc in range(2):
                mm(ps[bq][:, 0:CH], cst[:, C_ODIV:C_ODIV + 128], YQ[:, cc, :], cc == 0, cc == 1, ["cst", ("YQ", cc)],
                   [psk(bq)])
            PT2 = YQ[:, 0, :]
            PT3 = YQ[:, 1, :]
            cp("act", MEAN, ps[bm][:, 0:CH], [psk(bm)], ["MEAN"])
            tt("dve", PT2, MEAN, MEAN, ALU.mult, ["MEAN"], [("YQ", 0)])
            tt("dve", RSTD, ps[bq][:, 0:CH], PT2, ALU.subtract, [psk(bq), ("YQ", 0)], ["RSTD"])
            rsqrt(RSTD, RSTD, 1.0, ["RSTD"], ["RSTD"])
            for cc in range(2):
                tt("dve", PT3, YC[:, cc, :], MEAN, ALU.subtract, [("YC", cc), "MEAN"], [("YQ", 1)])
                tt("dve", PT3, PT3, RSTD, ALU.mult, [("YQ", 1), "RSTD"], [("YQ", 1)])
                act(YB_[:, cc, :], PT3, AF.Silu, [("YQ", 1), ("chp", l)], [("YB", s_, cc)],
                    bias=chp[:, CP_LB + cc:CP_LB + cc + 1], scale=chp[:, CP_LG + cc:CP_LG + cc + 1])

        for _ in prep(0, 0):
            pass
        for m in range(NM):
            s_ = m % 2
            YP_, YB_, OC_ = YP2[s_], YB2[s_], OC2[s_]
            if m == 0:
                dma("sp", U2[0].rearrange("p k t -> p (k t)"), ux[0, :, :], [("ux", 0)], [("U3", 0)])
            if m + 1 < NM:
                dma("sp", U2[(m + 1) % 2].rearrange("p k t -> p (k t)"), ux[m + 1, :, :], [("ux", m + 1)],
                    [("U3", (m + 1) % 2)])
            load_h(hsrc, m, htile, "ht3")
            ukey = ("U3", m % 2)
            U = U2[m % 2]
            pg = prep(m + 1, 1 - s_) if m + 1 < NM else iter(())
            for f in range(8):
                if f == 1 or f == 4:
                    next(pg, None)
                for br in range(3):
                    b = nbank(0, 7)
                    c0 = br * 1024 + f * 128
                    for k in range(8):
                        mm(ps[b][:, 0:CH], WG[:, k, c0:c0 + 128], U[:, k, :], k == 0, k == 7, wgk + [ukey], [psk(b)])
                    act(SGB[br], ps[b][:, 0:CH], AF.Sigmoid, [psk(b)], [("SGB", br)])
                fs = slice(f * 128, (f + 1) * 128)
                ba = nbank(0, 7)
                for cc in range(2):
                    mm(ps[ba][:, 0:CH], WOA[:, cc, fs], YP_[:, cc, :], cc == 0, cc == 1,
                       ["WOA", ("YP", s_, 0), ("YP", s_, 1)], [psk(ba)])
                tt("dve", T1, ps[ba][:, 0:CH], SGB[0], ALU.mult, [psk(ba), ("SGB", 0)], ["T1"])
                bb = nbank(0, 7)
                for cc in range(2):
                    mm(ps[bb][:, 0:CH], WOB[:, cc, fs], YB_[:, cc, :], cc == 0, cc == 1,
                       ["WOB", ("YB", s_, 0), ("YB", s_, 1)], [psk(bb)])
                tt("dve", T2, ps[bb][:, 0:CH], SGB[1], ALU.mult, [psk(bb), ("SGB", 1)], ["T2"])
                bc = nbank(0, 7)
                for k in range(4):
                    mm(ps[bc][:, 0:CH], WOC[:, k, fs], OC_[:, k, :], k == 0, k == 3, ["WOC", ("OC", s_)], [psk(bc)])
                tt("dve", T3, ps[bc][:, 0:CH], SGB[2], ALU.mult, [psk(bc), ("SGB", 2)], ["T3"])
                tt("pool", T1, T1, T2, ALU.add, ["T1", "T2"], ["T1"])
                tt("pool", MG[:, f, :], T1, T3, ALU.add, ["T1", "T3"], [("MG", f)])
            for _ in pg:
                pass
            mgk = [("MG", f) for f in range(8)]
            for i in range(3):
                for half in range(2):
                    b = nbank(0, 7)
                    for k in range(8):
                        mm(ps[b][:, 0:512], MG[:, k, i * 128:(i + 1) * 128], WO[:, k, half * 512:(half + 1) * 512],
                           k == 0, k == 7, mgk + ["WO"], [psk(b)])
                    tt("dve", htile[:, i, half * 512:(half + 1) * 512], htile[:, i, half * 512:(half + 1) * 512],
                       ps[b][:, 0:512], ALU.add, ["ht3", psk(b)], ["ht3"])
            dma("sp", hbuf[m * CH:(m + 1) * CH, :].rearrange("(i p) d -> p i d", p=128), htile,
                ["ht3"], [("hsrc", m)])

    def phase3b(l, last):
        SCS = [list(range(0, 6)), list(range(6, NM))]
        A = Arena()
        ACCT = A.f32(18 * D).rearrange("p (t d) -> p t d", d=D)
        VTT = A.bf16(6 * 8 * CH).rearrange("p (s k t) -> p s k t", s=6, k=8)
        WS = []
        for s in range(2):
            WS.append((v3(A.bf16(2048), 8), v3(A.bf16(2048), 8), v3(A.bf16(2048), 2)))
        RW = v3(A.bf16(160), 8)
        ubf = A.bf16(D)
        LG = A.f32(20)
        GM = A.f32(4)
        OH = A.f32(4)
        EX = A.f32(4)
        ES = v3(A.f32(16), 4)
        EL = A.f32(4)
        M1 = A.f32(4)
        M2 = A.f32(4)
        E2 = A.f32(4)
        SC1 = A.f32(8)
        WGp = A.f32(4)
        CMB = A.f32(18 * 16).rearrange("p (t e) -> p t e", e=16)
        S1 = [A.f32(CH) for _ in range(2)]
        HE = A.bf16(4 * CH).rearrange("p (s c t) -> p s c t", s=2, c=2)
        OT = A.f32(D)
        dma("pool", RW, rw_d[l, :, :].rearrange("(k p) c -> p k c", p=128), (), ["RW"])
        ecount = 0
        for sc in SCS:
            for si, m in enumerate(sc):
                for i in range(3):
                    t = si * 3 + i
                    dma("sp", ACCT[:, t, :], hbuf[m * CH + i * 128:m * CH + (i + 1) * 128, :], [("hsrc", m)],
                        [("ACC", t)])
            for si, m in enumerate(sc):
                for i in range(3):
                    t = si * 3 + i
                    ss = small[:, 48:49]
                    rs = small[:, 49:50]
                    stt(ubf, ACCT[:, t, :], 1.0, ACCT[:, t, :], ALU.mult, ALU.mult, [("ACC", t)], ["ubf", "ss2"],
                        accum=ss)
                    rsqrt(rs, ss, 1.0 / D, ["ss2"], ["rs2"])
                    stt(ubf, ACCT[:, t, :], rs, vecs[:, V_G2:V_G2 + D], ALU.mult, ALU.mult,
                        [("ACC", t), "rs2", ("vecs", l)], ["ubf"])
                    for k in range(8):
                        P.add("pe", lambda e, k=k: e.transpose(psT[:, k * 128:(k + 1) * 128],
                                                               ubf[:, k * 128:(k + 1) * 128], ident[:, :]),
                              ["ubf", "ident"], ["psT"])
                    cp("act", VTT[:, si, :, i * 128:(i + 1) * 128], psT[:, :].rearrange("p (k t) -> p k t", k=8),
                       ["psT"], [("VTT", si)])
                    b = nbank(0, 7)
                    for k in range(8):
                        mm(ps[b][:, 0:20], VTT[:, si, k, i * 128:(i + 1) * 128], RW[:, k, :], k == 0, k == 7,
                           [("VTT", si), "RW"], [psk(b)])
                    tt("dve", LG, ps[b][:, 0:20], vecs[:, V_RB:V_RB + 20], ALU.add, [psk(b), ("vecs", l)], ["LG"])
                    AXX = mybir.AxisListType.X
                    P.add("dve", lambda e: e.reduce_max(GM[:, 0:1], LG[:, 0:4], AXX), ["LG"], ["GM"])
                    ts("dve", OH, LG[:, 0:4], GM[:, 0:1], None, ALU.is_equal, None, ["LG", "GM"], ["OH"])
                    ts("dve", EX, LG[:, 0:4], GM[:, 0:1], None, ALU.subtract, None, ["LG", "GM"], ["EX"])
                    act(EX, EX, AF.Exp, ["EX"], ["EX"])
                    P.add("dve", lambda e: e.reduce_sum(GM[:, 1:2], EX, AXX), ["EX", "GM"], ["GS"])
                    P.add("dve", lambda e: e.reciprocal(GM[:, 2:3], GM[:, 1:2]), ["GS"], ["GW"])
                    LE = LG[:, 4:20].rearrange("p (g e) -> p g e", g=4)
                    for g in range(4):
                        ts("dve", ES[:, g, :], LE[:, g, :], OH[:, g:g + 1], None, ALU.mult, None, ["LG", "OH"],
                           [("ES", g)])
                    esk = [("ES", g) for g in range(4)]
                    tt("dve", ES[:, 0, :], ES[:, 0, :], ES[:, 1, :], ALU.add, esk, [("ES", 0)])
                    tt("dve", ES[:, 2, :], ES[:, 2, :], ES[:, 3, :], ALU.add, esk, [("ES", 2)])
                    tt("dve", EL, ES[:, 0, :], ES[:, 2, :], ALU.add, [("ES", 0), ("ES", 2)], ["EL"])
                    P.add("dve", lambda e: e.reduce_max(SC1[:, 0:1], EL, AXX), ["EL"], ["m1"])
                    ts("dve", M1, EL, SC1[:, 0:1], None, ALU.is_equal, None, ["EL", "m1"], ["M1"])
                    stt(E2, M1, NEG, EL, ALU.mult, ALU.add, ["M1", "EL"], ["E2"])
                    P.add("dve", lambda e: e.reduce_max(SC1[:, 1:2], E2, AXX), ["E2"], ["m2"])
                    ts("dve", M2, E2, SC1[:, 1:2], None, ALU.is_equal, None, ["E2", "m2"], ["M2"])
                    tt("dve", SC1[:, 2:3], SC1[:, 0:1], SC1[:, 1:2], ALU.subtract, ["m1", "m2"], ["dm"])
                    act(SC1[:, 3:4], SC1[:, 2:3], AF.Sigmoid, ["dm"], ["p1"])
                    ts("dve", SC1[:, 4:5], SC1[:, 3:4], -1.0, 1.0, ALU.mult, ALU.add, ["p1"], ["p2"])
                    tt("dve", SC1[:, 3:4], SC1[:, 3:4], GM[:, 2:3], ALU.mult, ["p1", "GW", "p2"], ["p1g"])
                    tt("dve", SC1[:, 4:5], SC1[:, 4:5], GM[:, 2:3], ALU.mult, ["p2", "GW"], ["p2g"])
                    ts("dve", WGp, M1, SC1[:, 3:4], None, ALU.mult, None, ["M1", "p1g"], ["WGp"])
                    stt(WGp, M2, SC1[:, 4:5], WGp, ALU.mult, ALU.add, ["M2", "p2g", "WGp"], ["WGp"])
                    for g in range(4):
                        ts("dve", CMB[:, t, g * 4:(g + 1) * 4], WGp, OH[:, g:g + 1], None, ALU.mult, None,
                           ["WGp", "OH"], [("CMB", t)])

            def load_expert(e, s):
                w1, w3, w2 = WS[s]
                dma("pool", w1, ew1[l, e, :, :].rearrange("(k p) c -> p k c", p=128), (), [("EW1", s)])
                dma("pool", w3, ew3[l, e, :, :].rearrange("(k p) c -> p k c", p=128), (), [("EW3", s)])
                dma("pool", w2, ew2[l, e, :, :].rearrange("(k p) c -> p k c", p=128), (), [("EW2", s)])
            load_expert(0, ecount % 2)
            for e in range(16):
                s = ecount % 2
                ecount += 1
                if e + 1 < 16:
                    load_expert(e + 1, 1 - s)
                w1, w3, w2 = WS[s]
                for si, m in enumerate(sc):
                    hs = si % 2
                    for hc in range(2):
                        b1 = nbank(0, 7)
                        for k in range(8):
                            mm(ps[b1][:, 0:CH], w1[:, k, hc * 128:(hc + 1) * 128], VTT[:, si, k, :], k == 0, k == 7,
                               [("EW1", s), ("VTT", si)], [psk(b1)])
                        b3 = nbank(0, 7)
                        for k in range(8):
                            mm(ps[b3][:, 0:CH], w3[:, k, hc * 128:(hc + 1) * 128], VTT[:, si, k, :], k == 0, k == 7,
                               [("EW3", s), ("VTT", si)], [psk(b3)])
                        act(S1[hc], ps[b1][:, 0:CH], AF.Silu, [psk(b1)], [("S1", hc)])
                        tt("dve", HE[:, hs, hc, :], S1[hc], ps[b3][:, 0:CH], ALU.mult, [("S1", hc), psk(b3)],
                           [("HE", hs, hc)])
                    for i in range(3):
                        t = si * 3 + i
                        for half in range(2):
                            b = nbank(0, 7)
                            for hc in range(2):
                                mm(ps[b][:, 0:512], HE[:, hs, hc, i * 128:(i + 1) * 128],
                                   w2[:, hc, half * 512:(half + 1) * 512], hc == 0, hc == 1,
                                   [("HE", hs, 0), ("HE", hs, 1), ("EW2", s)], [psk(b)])
                            stt(ACCT[:, t, half * 512:(half + 1) * 512], ps[b][:, 0:512], CMB[:, t, e:e + 1],
                                ACCT[:, t, half * 512:(half + 1) * 512], ALU.mult, ALU.add,
                                [psk(b), ("CMB", t), ("ACC", t)], [("ACC", t)])
            for si, m in enumerate(sc):
                for i in range(3):
                    t = si * 3 + i
                    r0 = m * CH + i * 128
                    if not last:
                        dma("sp", hbuf[r0:r0 + 128, :], ACCT[:, t, :], [("ACC", t)], [("hsrc", m), ("hw", m, i)])
                    else:
                        ss = small[:, 52:53]
                        rs = small[:, 53:54]
                        stt(OT, ACCT[:, t, :], 1.0, ACCT[:, t, :], ALU.mult, ALU.mult, [("ACC", t)], ["OT", "ss3"],
                            accum=ss)
                        rsqrt(rs, ss, 1.0 / D, ["ss3"], ["rs3"])
                        stt(OT, ACCT[:, t, :], rs, fing[:, :], ALU.mult, ALU.mult, [("ACC", t), "rs3", "fing"], ["OT"])
                        dma("sp", out_d[r0:r0 + 128, :], OT, ["OT"], [("out", m, i)])

    def phase3b_sparse(l, last):
        A = Arena()
        AXX = mybir.AxisListType.X
        NT = NOWNT
        VBF = A.bf16(NT * D).rearrange("p (t d) -> p t d", d=D)
        INDb = A.bf16(NT * 16)
        M1G = A.f32(NT * 16)
        M2G = A.f32(NT * 16)
        RIN = A.f32(NT * 16)
        TOT = A.f32(NT * 16)
        INC = A.f32(NT * 16)
        RB = A.f32(NT * 16)
        PRD = A.f32(NT * 16)
        PW = A.f32(NT * 2).rearrange("p (t j) -> p t j", j=2)
        SLF = [A.f32(NT) for _ in range(2)]
        SLI = [AR[:, A.off + i * 40:A.off + i * 40 + NT].bitcast(I32) for i in range(2)]
        A.off += 80
        CNT = A.f32(16)
        NTL = A.f32(16)
        PC = A.f32(16)
        BEND = A.f32(16)
        BASE = A.f32(16)
        ONE16 = A.f32(16)
        ONE33 = A.f32(NT)
        TMP33 = A.f32(NT)
        TE = A.f32(NSLT)
        WIF = A.f32(NSLT)
        WII = AR[:, A.off:A.off + NSLT].bitcast(I32)
        A.off += 88
        UST = A.bf16(128)
        ONEb = A.bf16(128)
        RW = v3(A.bf16(160), 8)
        ubfx = A.bf16(D)
        HT = [A.f32(D) for _ in range(3)]
        NRS = 3
        RSET = [dict(VTt=v3(A.bf16(8 * 128), 8), LG=A.f32(20), GM=A.f32(4), OH=A.f32(4), EX=A.f32(4),
                     ES=v3(A.f32(16), 4), EL=A.f32(4), M1=A.f32(4), M2=A.f32(4), E2=A.f32(4), SC1=A.f32(8),
                     ss=A.f32(1), rs=A.f32(1)) for _ in range(NRS)]
        NWB = 3
        WB = [(A.bf16(2048), A.bf16(2048), A.bf16(2048)) for _ in range(NWB)]
        XI = [A.bf16(D) for _ in range(3)]
        XT = [v3(A.bf16(8 * 128), 8) for _ in range(2)]
        S1 = A.f32(256)
        HEb = [A.bf16(256) for _ in range(2)]
        YT = [A.f32(D) for _ in range(2)]
        Y1 = [WB[hb_][0].bitcast(F32) for hb_ in range(2)]
        Y2 = [WB[hb_][1].bitcast(F32) for hb_ in range(2)]
        OT = YT[0]
        M1G3 = M1G.rearrange("p (t e) -> p t e", e=16)
        M2G3 = M2G.rearrange("p (t e) -> p t e", e=16)
        IND3 = INDb.rearrange("p (t e) -> p t e", e=16)
        dma("pool", RW, rw_d[l, :, :].rearrange("(k p) c -> p k c", p=128), (), ["RW"])
        tt("dve", UST, cst[:, C_U:C_U + 128], cst[:, C_ID:C_ID + 128], ALU.subtract, ["cst"], ["UST"])
        cp("dve", ONEb, cst[:, C_ONES:C_ONES + 128], ["cst"], ["ONEb"])
        memset("dve", ONE16, 1.0, ["ONE16"])
        memset("dve", ONE33, 1.0, ["ONE33"])
        def router_tile(t):
            R_ = RSET[t % NRS]
            q = t % NRS
            VTt, LG, GM, OH, EX, ES, EL, M1, M2, E2, SC1 = (R_[k] for k in
                                                            ("VTt", "LG", "GM", "OH", "EX", "ES", "EL", "M1", "M2", "E2", "SC1"))
            K = lambda nm: (nm, q)
            m, i = t // 3, t % 3
            hb = t % 3
            r0 = m * CH + i * 128
            dma("sp", HT[hb], hbuf[r0:r0 + 128, :], [("hsrc", m)], [("HT", hb)])
            ss = R_["ss"]
            rs = R_["rs"]
            stt(ubfx, HT[hb], 1.0, HT[hb], ALU.mult, ALU.mult, [("HT", hb)], ["ubfx", K("ss2")], accum=ss)
            yield
            rsqrt(rs, ss, 1.0 / D, [K("ss2")], [K("rs2")])
            yield
            stt(VBF[:, t, :], HT[hb], rs, vecs[:, V_G2:V_G2 + D], ALU.mult, ALU.mult,
                [("HT", hb), K("rs2"), ("vecs", l)], [("VBF", t)])
            for k in range(8):
                P.add("pe", lambda e, k=k, t=t: e.transpose(psT[:, k * 128:(k + 1) * 128],
                                                            VBF[:, t, k * 128:(k + 1) * 128], ident[:, :]),
                      [("VBF", t), "ident"], ["psT"])
            cp("act", VTt, psT[:, :].rearrange("p (k t) -> p k t", k=8), ["psT"], [K("VTt")])
            b = nbank(0, 7)
            for k in range(8):
                mm(ps[b][:, 0:20], VTt[:, k, :], RW[:, k, :], k == 0, k == 7, [K("VTt"), "RW"], [psk(b)])
            yield
            tt("dve", LG, ps[b][:, 0:20], vecs[:, V_RB:V_RB + 20], ALU.add, [psk(b), ("vecs", l)], [K("LG")])
            yield
            tt("dve", LG, LG, cst[:, C_TB:C_TB + 20], ALU.add, [K("LG"), "cst"], [K("LG")])
            yield
            P.add("dve", lambda e: e.reduce_max(GM[:, 0:1], LG[:, 0:4], AXX), [K("LG")], [K("GM")])
            yield
            ts("dve", OH, LG[:, 0:4], GM[:, 0:1], None, ALU.is_equal, None, [K("LG"), K("GM")], [K("OH")])
            ts("dve", EX, LG[:, 0:4], GM[:, 0:1], None, ALU.subtract, None, [K("LG"), K("GM")], [K("EX")])
            yield
            act(EX, EX, AF.Exp, [K("EX")], [K("EX")])
            LE = LG[:, 4:20].rearrange("p (g e) -> p g e", g=4)
            for g in range(4):
                ts("dve", ES[:, g, :], LE[:, g, :], OH[:, g:g + 1], None, ALU.mult, None, [K("LG"), K("OH")],
                   [K(("ES", g))])
            yield
            esk = [K(("ES", g)) for g in range(4)]
            tt("dve", ES[:, 0, :], ES[:, 0, :], ES[:, 1, :], ALU.add, esk, [K(("ES", 0))])
            tt("dve", ES[:, 2, :], ES[:, 2, :], ES[:, 3, :], ALU.add, esk, [K(("ES", 2))])
            yield
            tt("dve", EL, ES[:, 0, :], ES[:, 2, :], ALU.add, [K(("ES", 0)), K(("ES", 2))], [K("EL")])
            P.add("dve", lambda e: e.reduce_sum(GM[:, 1:2], EX, AXX), [K("EX"), K("GM")], [K("GS")])
            yield
            P.add("dve", lambda e: e.reduce_max(SC1[:, 0:1], EL, AXX), [K("EL")], [K("m1")])
            P.add("dve", lambda e: e.reciprocal(GM[:, 2:3], GM[:, 1:2]), [K("GS")], [K("GW")])
            yield
            ts("dve", M1, EL, SC1[:, 0:1], None, ALU.is_equal, None, [K("EL"), K("m1")], [K("M1")])
            yield
            stt(E2, M1, NEG, EL, ALU.mult, ALU.add, [K("M1"), K("EL")], [K("E2")])
            yield
            P.add("dve", lambda e: e.reduce_max(SC1[:, 1:2], E2, AXX), [K("E2")], [K("m2")])
            yield
            ts("dve", M2, E2, SC1[:, 1:2], None, ALU.is_equal, None, [K("E2"), K("m2")], [K("M2")])
            tt("dve", SC1[:, 2:3], SC1[:, 0:1], SC1[:, 1:2], ALU.subtract, [K("m1"), K("m2")], [K("dm")])
            yield
            act(SC1[:, 3:4], SC1[:, 2:3], AF.Sigmoid, [K("dm")], [K("p1")])
            for g in range(4):
                ts("dve", M1G3[:, t, g * 4:(g + 1) * 4], M1, OH[:, g:g + 1], None, ALU.mult, None, [K("M1"), K("OH")],
                   [("M1G", t)])
                ts("dve", M2G3[:, t, g * 4:(g + 1) * 4], M2, OH[:, g:g + 1], None, ALU.mult, None, [K("M2"), K("OH")],
                   [("M2G", t)])
            yield
            ts("dve", SC1[:, 4:5], SC1[:, 3:4], -1.0, 1.0, ALU.mult, ALU.add, [K("p1")], [K("p2")])
            tt("dve", IND3[:, t, :], M1G3[:, t, :], M2G3[:, t, :], ALU.add, [("M1G", t), ("M2G", t)], [("IND", t)])
            yield
            tt("dve", PW[:, t, 0:1], SC1[:, 3:4], GM[:, 2:3], ALU.mult, [K("p1"), K("GW"), K("p2")], [("PW", t)])
            tt("dve", PW[:, t, 1:2], SC1[:, 4:5], GM[:, 2:3], ALU.mult, [K("p2"), K("GW"), ("PW", t)], [("PW", t)])
            yield

        for t0 in range(0, NT, NRS):
            gens = [router_tile(t) for t in range(t0, min(NT, t0 + NRS))]
            while gens:
                for g_ in list(gens):
                    try:
                        next(g_)
                    except StopIteration:
                        gens.remove(g_)
        indk = [("IND", t) for t in range(NT)]
        m1k = [("M1G", t) for t in range(NT)]
        m2k = [("M2G", t) for t in range(NT)]
        for half in range(2):
            cs = slice(half * 264, (half + 1) * 264)
            mm(ps[half][:, 0:264], UST, INDb[:, cs], True, True, indk + ["UST"], [psk(half)])
            mm(ps[2 + half][:, 0:264], ONEb, INDb[:, cs], True, True, indk + ["ONEb"], [psk(2 + half)])
            cp("act", RIN[:, cs], ps[half][:, 0:264], [psk(half)], [("RIN", half)])
            cp("dve", TOT[:, cs], ps[2 + half][:, 0:264], [psk(2 + half)], [("TOT", half)])
        TOT3 = TOT.rearrange("p (t e) -> p t e", e=16)
        INC3 = INC.rearrange("p (t e) -> p t e", e=16)
        RB3 = RB.rearrange("p (t e) -> p t e", e=16)
        for e_ in range(16):
            P.add("dve", lambda e, e_=e_: e.tensor_tensor_scan(INC3[:, :, e_], ONE33, TOT3[:, :, e_], 0.0,
                                                              ALU.mult, ALU.add),
                  [("TOT", 0), ("TOT", 1), "ONE33"], [("INC", e_)])
        ik = [("INC", e_) for e_ in range(16)]
        cp("dve", CNT, INC3[:, NT - 1, :], ik, ["CNT"])
        tt("dve", RB, INC, TOT, ALU.subtract, ik + [("TOT", 0), ("TOT", 1)], ["RB"])
        tt("dve", RB, RB, RIN, ALU.add, ["RB", ("RIN", 0), ("RIN", 1)], ["RB"])
        for e_ in range(16):
            ts("dve", TMP33, cst[:, C_TH:C_TH + NT], CNT[:, e_:e_ + 1], None, ALU.is_le, None, ["cst", "CNT"], ["TMP33"])
            P.add("dve", lambda e, e_=e_: e.reduce_sum(NTL[:, e_:e_ + 1], TMP33, AXX), ["TMP33"], [("NTL", e_)])
        ts("dve", PC, NTL, 128.0, None, ALU.mult, None, [("NTL", e_) for e_ in range(16)], ["PC"])
        P.add("dve", lambda e: e.tensor_tensor_scan(BEND, ONE16, PC, 0.0, ALU.mult, ALU.add), ["PC", "ONE16"], ["BEND"])
        tt("dve", BASE, BEND, PC, ALU.subtract, ["BEND", "PC"], ["BASE"])
        for t in range(NT):
            tt("dve", RB3[:, t, :], RB3[:, t, :], BASE, ALU.add, ["RB", "BASE"], ["RB"])
        for j, (MG_, mk_) in enumerate(((M1G, m1k), (M2G, m2k))):
            tt("dve", PRD, MG_, RB, ALU.mult, mk_ + ["RB"], ["PRD"])
            P.add("dve", lambda e, j=j: e.tensor_reduce(SLF[j], PRD.rearrange("p (t e) -> p t e", e=16), AXX, ALU.add),
                  ["PRD"], [("SLF", j)])
            cp("dve", SLI[j], SLF[j], [("SLF", j)], [("SLI", j)])
        memset("dve", TE, 0.0, ["TE"])
        for e_ in range(16):
            stt(TE, cst[:, C_JT:C_JT + NSLT], BEND[:, e_:e_ + 1], TE, ALU.is_ge, ALU.add, ["cst", "BEND", "TE"], ["TE"])
        ts("dve", TE, TE, 15.0, None, ALU.min, None, ["TE"], ["TE"])
        ts("dve", WIF, TE, 128.0, cst[:, C_PIDX:C_PIDX + 1], ALU.mult, ALU.add, ["TE", "cst"], ["WIF"])
        cp("dve", WII, WIF, ["WIF"], ["WII"])
        if "moe" in dbg_out and l == 0:
            dm = dbg_out["moe"]
            for (o, n, ap, k) in ((0, NT, SLF[0], ("SLF", 0)), (40, NT, SLF[1], ("SLF", 1)), (80, 16, CNT, "CNT"),
                                  (96, 16, PC, "PC"), (112, 16, BEND, "BEND"), (128, 16, BASE, "BASE"),
                                  (144, NSLT, TE, "TE"), (232, NSLT, WIF, "WIF"),
                                  (320, 66, PW.rearrange("p t j -> p (t j)"), None)):
                rk = [k] if k is not None else [("PW", t) for t in range(NT)]
                dma("sp", dm[:, o:o + n], ap, rk, [("dbgmoe", o)])
        for t in range(NT):
            for j in range(2):
                P.add("pool", lambda e, t=t, j=j: e.indirect_dma_start(
                    out=XS[:, :], out_offset=bass.IndirectOffsetOnAxis(ap=SLI[j][:, t:t + 1], axis=0),
                    in_=VBF[:, t, :], in_offset=None), [("VBF", t), ("SLI", j)], [("XS", t, j)], kind="d")
        xsk = [("XS", t, j) for t in range(NT) for j in range(2)]
        wsk = [(nm, e_) for nm in ("W1s", "W3s", "W2s") for e_ in range(16)]
        def load_slot(j):
            wb = WB[j % NWB]
            for a, (Wd, nm) in enumerate(((W1s[l], "w1"), (W3s[l], "w3"), (W2s[l], "w2"))):
                P.add("pool", lambda e, a=a, Wd=Wd, wb=wb, j=j: e.indirect_dma_start(
                    out=wb[a], out_offset=None, in_=Wd[:, :],
                    in_offset=bass.IndirectOffsetOnAxis(ap=WII[:, j:j + 1], axis=0)),
                    ["WII"] + wsk, [("WB", j % NWB, a)], kind="d")
            dma("sp", XI[j % 3], XS[j * 128:(j + 1) * 128, :], xsk, [("XI", j % 3)])
        load_slot(0)
        load_slot(1)
        for j in range(NSLT):
            if j + 2 < NSLT:
                load_slot(j + 2)
            w1, w3, w2 = WB[j % NWB]
            w1 = v3(w1, 8)
            w3 = v3(w3, 8)
            w2 = v3(w2, 2)
            wk = [("WB", j % NWB, a) for a in range(3)]
            xi = XI[j % 3]
            xt = XT[j % 2]
            for k in range(8):
                P.add("pe", lambda e, k=k, xi=xi: e.transpose(psT[:, k * 128:(k + 1) * 128],
                                                              xi[:, k * 128:(k + 1) * 128], ident[:, :]),
                      [("XI", j % 3), "ident"], ["psT"])
            cp("act", xt, psT[:, :].rearrange("p (k t) -> p k t", k=8), ["psT"], [("XT", j % 2)])
            b1 = nbank(0, 7)
            b3 = nbank(0, 7)
            for hc in range(2):
                for k in range(8):
                    mm(ps[b1][:, hc * 128:(hc + 1) * 128], w1[:, k, hc * 128:(hc + 1) * 128], xt[:, k, :],
                       k == 0, k == 7, wk + [("XT", j % 2)], [psk(b1)])
                for k in range(8):
                    mm(ps[b3][:, hc * 128:(hc + 1) * 128], w3[:, k, hc * 128:(hc + 1) * 128], xt[:, k, :],
                       k == 0, k == 7, wk + [("XT", j % 2)], [psk(b3)])
            act(S1, ps[b1][:, 0:256], AF.Silu, [psk(b1)], ["S1s"])
            he = HEb[j % 2]
            tt("dve", he, S1, ps[b3][:, 0:256], ALU.mult, ["S1s", psk(b3)], [("HEb", j % 2)])
            yt = YT[j % 2]
            for half in range(2):
                b = nbank(0, 7)
                for hc in range(2):
                    mm(ps[b][:, 0:512], he[:, hc * 128:(hc + 1) * 128], w2[:, hc, half * 512:(half + 1) * 512],
                       hc == 0, hc == 1, wk + [("HEb", j % 2)], [psk(b)])
                cp("act" if half == 0 else "dve", yt[:, half * 512:(half + 1) * 512], ps[b][:, 0:512], [psk(b)],
                   [("YT", j % 2, half)])
            dma("sp", YS[j * 128:(j + 1) * 128, :], yt, [("YT", j % 2, 0), ("YT", j % 2, 1)], [("YS", j)])
        ysk = [("YS", j) for j in range(NSLT)]
        def load_comb(t):
            m, i = t // 3, t % 3
            r0 = m * CH + i * 128
            hb = t % 2
            dma("sp", HT[hb], hbuf[r0:r0 + 128, :], [("hsrc", m)], [("HT", hb)])
            for j, Yb in enumerate((Y1, Y2)):
                P.add("pool", lambda e, t=t, j=j, Yb=Yb, hb=hb: e.indirect_dma_start(
                    out=Yb[hb], out_offset=None, in_=YS[:, :],
                    in_offset=bass.IndirectOffsetOnAxis(ap=SLI[j][:, t:t + 1], axis=0)),
                    [("SLI", j)] + ysk, [("Yg", j, hb), ("WB", hb, j)], kind="d")
        load_comb(0)
        for t in range(NT):
            if t + 1 < NT:
                load_comb(t + 1)
            m, i = t // 3, t % 3
            r0 = m * CH + i * 128
            hb = t % 2
            stt(HT[hb], Y1[hb], PW[:, t, 0:1], HT[hb], ALU.mult, ALU.add, [("Yg", 0, hb), ("PW", t), ("HT", hb)],
                [("HT", hb)])
            stt(HT[hb], Y2[hb], PW[:, t, 1:2], HT[hb], ALU.mult, ALU.add, [("Yg", 1, hb), ("PW", t), ("HT", hb)],
                [("HT", hb)])
            if not last:
                dma("sp", hbuf[r0:r0 + 128, :], HT[hb], [("HT", hb)], [("hsrc", m), ("hw", m, i)])
            else:
                ss = small[:, 52:53]
                rs = small[:, 53:54]
                otk = [("YT", 0, 0), ("YT", 0, 1)]
                stt(OT, HT[hb], 1.0, HT[hb], ALU.mult, ALU.mult, [("HT", hb)], otk + ["ss3"], accum=ss)
                rsqrt(rs, ss, 1.0 / D, ["ss3"], ["rs3"])
                stt(OT, HT[hb], rs, fing[:, :], ALU.mult, ALU.mult, [("HT", hb), "rs3", "fing"], otk)
                dma("sp", out_d[r0:r0 + 128, :], OT, otk, [("out", m, i)])

    phases = debug.get("phases", None)
    n = 0

    def want():
        return phases is None or n < phases
    for l in range(DEPTH):
        src = h0 if l == 0 else hbuf
        for ph in ("p1", "ex", "p2a", "p2b", "p3a", "p3b"):
            if not want():
                break
            if ph == "p1":
                load_layer_vecs(l)
                phase1(l, src)
            elif ph == "ex":
                exchange()
            elif ph == "p2a":
                phase2a()
            elif ph == "p2b":
                phase2b()
            elif ph == "p3a":
                phase3a(l, src)
            else:
                if SPARSE_MOE:
                    phase3b_sparse(l, l == DEPTH - 1)
                else:
                    phase3b(l, l == DEPTH - 1)
            barrier()
            n += 1
    for nm, t in dbg_out.items():
        if nm in ("F", "moe"):
            continue
        src_t = scratch[nm]
        P.add("sp", lambda e, t=t, src_t=src_t: e.dma_start(out=t.ap(), in_=src_t.ap()), (), [("dbg", nm)], kind="d")
    barrier()
    P.emit()
    return nc, stack


def _constants(c):
    cst = np.zeros((128, NCST), np.float32)
    k = np.arange(128)
    cst[:, C_U:C_U + 128] = (k[:, None] <= k[None, :]).astype(np.float32)
    cst[:, C_ONES:C_ONES + 128] = 1.0
    cst[:, C_ODIV:C_ODIV + 128] = 1.0 / 256.0
    cst[:, C_SEL] = 1.0 if c == 0 else 0.0
    cst[:, C_SEL + 1] = 0.0 if c == 0 else 1.0
    q = np.arange(CH)
    for w in range(2):
        for kt in range(3):
            kpos = kt * 128 + k
            causal = np.where(kpos[:, None] <= q[None, :], 0.0, NEG).astype(np.float32)
            if c == 0:
                mk = causal if w == 0 else np.full((128, CH), NEG, np.float32)
            else:
                mk = np.zeros((128, CH), np.float32) if w == 0 else causal
            o = C_MASK + (w * 3 + kt) * CH
            cst[:, o:o + CH] = mk
    wins = np.array([2, 4, 8, 16], np.float32)
    for first in range(2):
        for cc in range(2):
            for half in range(2):
                W = wins[cc * 2 + half]
                t = np.arange(CH, dtype=np.float32)
                if first == 0 and c == 0:
                    cnt = np.minimum(t + 1.0, W)
                else:
                    cnt = np.full(CH, W, np.float32)
                o = C_INVC + (first * 2 + cc) * CH
                cst[half * 64:(half + 1) * 64, o:o + CH] = (1.0 / cnt)[None, :]
    cst[:, C_ID:C_ID + 128] = np.eye(128, dtype=np.float32)
    cst[:, C_JT:C_JT + 82] = (128.0 * np.arange(82, dtype=np.float32))[None, :]
    cst[:, C_TH:C_TH + 33] = (128.0 * np.arange(33, dtype=np.float32) + 1.0)[None, :]
    cst[:, C_PIDX] = np.arange(128, dtype=np.float32)
    cst[:, C_TB:C_TB + 4] = (-1e-7 * np.arange(4, dtype=np.float32))[None, :]
    cst[:, C_TB + 4:C_TB + 20] = np.tile(-1e-7 * np.arange(4, dtype=np.float32), 4)[None, :]
    return cst


_CACHE = {}


def kernel(x, meta, norm1_g, w_in, b_forget, pool_w, pool_b, pool_scale, conv_w, conv_b, conv_ln_g,
           conv_ln_b, w_out_a, w_out_b, w_out_c, w_o, norm2_g, router_g, router_g_b, router_e,
           router_e_b, exp_w1, exp_w3, exp_w2, final_g, _debug=None):
    f = lambda a: np.ascontiguousarray(np.asarray(a, dtype=np.float32))
    x = f(x)
    B = x.shape[0]
    L = 16 + x.shape[1]
    LP = NG * CH
    meta = f(meta)
    vecs = np.zeros((DEPTH, 128, NV), np.float32)
    chp = np.zeros((DEPTH, 128, NCP), np.float32)
    for l in range(DEPTH):
        vecs[l, :, V_G1:V_G1 + D] = f(norm1_g)[l][None, :]
        vecs[l, :, V_G2:V_G2 + D] = f(norm2_g)[l][None, :]
        vecs[l, :, V_BF:V_BF + 8] = f(b_forget)[l][None, :]
        vecs[l, :, V_RB:V_RB + 4] = f(router_g_b)[l][None, :]
        vecs[l, :, V_RB + 4:V_RB + 20] = f(router_e_b)[l][None, :]
        for cc in range(2):
            sl = slice(cc * 128, (cc + 1) * 128)
            chp[l, :, CP_PB + cc] = f(pool_b)[l].reshape(256)[sl]
            chp[l, :, CP_PS + cc] = f(pool_scale)[l][sl]
            chp[l, :, CP_CB + cc] = f(conv_b)[l][sl]
            chp[l, :, CP_LG + cc] = f(conv_ln_g)[l][sl]
            chp[l, :, CP_LB + cc] = f(conv_ln_b)[l][sl]
            chp[l, :, CP_CW + cc * 31:CP_CW + (cc + 1) * 31] = f(conv_w)[l][:, sl].T
    fing = np.ascontiguousarray(np.broadcast_to(f(final_g)[None, :], (128, D)))
    rw = np.ascontiguousarray(np.concatenate([f(router_g), f(router_e)], axis=-1))
    shared = dict(vecs=vecs, fing=fing, chp=chp, w_in=f(w_in), pool_w=f(pool_w), w_out_a=f(w_out_a),
                  w_out_b=f(w_out_b), w_out_c=f(w_out_c), w_o=f(w_o), rw=rw, exp_w1=f(exp_w1),
                  exp_w3=f(exp_w3), exp_w2=f(exp_w2))
    csts = [_constants(0), _constants(1)]
    in_maps = []
    for core in range(NCORES):
        b, c = core // 2, core % 2
        seq = np.zeros((LP, D), np.float32)
        seq[:16] = meta
        seq[16:L] = x[b]
        own = seq.reshape(NM, 2, CH, D)[:, c].reshape(OWN, D)
        d = dict(shared)
        d["h0"] = np.ascontiguousarray(own)
        d["cst"] = csts[c]
        in_maps.append(d)
    key = repr(sorted((_debug or {}).items()))
    if key not in _CACHE:
        _CACHE[key] = build_program(_debug)
    nc, _ = _CACHE[key]
    res = run_bass_kernel_spmd(nc, in_maps, core_ids=list(range(NCORES)))
    out = np.zeros((B, LP, D), np.float32)
    for core in range(NCORES):
        b, c = core // 2, core % 2
        out[b].reshape(NM, 2, CH, D)[:, c] = res.results[core]["out"].reshape(NM, CH, D)
    if _debug:
        kernel.last = res
    return np.ascontiguousarray(out[:, 16:L])
```

```python
import numpy as np
import ml_dtypes
from contextlib import ExitStack
import concourse.bass as bass
import concourse.mybir as mybir
from concourse.bass_utils import run_bass_kernel_spmd

F32 = mybir.dt.float32
BF16 = mybir.dt.bfloat16
I32 = mybir.dt.int32
SPARSE_MOE = True
ALU = mybir.AluOpType
AF = mybir.ActivationFunctionType

D = 1024
DEPTH = 2
NCORES = 8
CH = 384
NM = 11
OWN = NM * CH
NG = 2 * NM
NGT = NG * 3
NOWNT = NM * 3
N_IN = 5384
EPS = 1e-6
NEG = -30000.0

C_U, C_ONES, C_ODIV, C_SEL, C_MASK, C_INVC, C_ID = 0, 128, 256, 384, 386, 386 + 2304, 386 + 2304 + 1536
C_JT = C_ID + 128
C_TH = C_JT + 82
C_PIDX = C_TH + 33
C_TB = C_PIDX + 1
NCST = C_TB + 20
NSLT = 82
V_G1, V_G2, V_BF, V_RB = 0, 1024, 2048, 2056
V_BF3 = 2076
NV = 2100
CP_PB, CP_PS, CP_CB, CP_LG, CP_LB, CP_CW = 0, 2, 4, 6, 8, 10
NCP = 10 + 62

SAME_ENGINE_SYNC = True
SEM_CAP = 16000
N_DMA_SEMS = 20
SEM_REARM_WAIT = ("sp", "act")


class Prog:
    def __init__(self, nc, stack):
        self.nc = nc
        self.stack = stack
        self.ops = []
        self.last_w = {}
        self.readers = {}
        self.last_compute = {}
        self.asyncs = []

    def add(self, eng, fn, reads=(), writes=(), kind="c", extra=()):
        i = len(self.ops)
        deps = {}
        for j in extra:
            deps[j] = "raw"
        for r in reads:
            w = self.last_w.get(r)
            if w is not None:
                deps[w] = "raw"
        for k in writes:
            w = self.last_w.get(k)
            if w is not None:
                deps[w] = "raw"
            for _, j in self.readers.get(k, {}).items():
                if j not in deps:
                    deps[j] = "war"
        for r in reads:
            self.readers.setdefault(r, {})[eng if kind == "c" else (eng, i)] = i
        for k in writes:
            self.last_w[k] = i
            self.readers[k] = {}
        self.ops.append(dict(eng=eng, fn=fn, deps=deps, kind=kind))
        if fn is not None:
            if kind == "c":
                self.last_compute[eng] = i
            else:
                self.asyncs.append(i)
        return i

    def emit(self):
        nc = self.nc
        ops = self.ops
        engs = ["pe", "act", "dve", "pool", "sp"]
        known = {e: {} for e in engs}
        needed = set()
        waits = [None] * len(ops)
        for i, op in enumerate(ops):
            E = op["eng"]
            wl = []
            best = {}
            for j, kd in op["deps"].items():
                oj = ops[j]
                if oj["kind"] != "c":
                    if known[E].get(("d", j)):
                        continue
                    known[E][("d", j)] = True
                    wl.append(j)
                    continue
                X = oj["eng"]
                if X == E and op["kind"] == "c":
                    if E == "pe" or kd == "war" or not SAME_ENGINE_SYNC:
                        continue
                if j > best.get(X, -1):
                    best[X] = j
            for X, j in best.items():
                if known[E].get(X, -1) >= j:
                    continue
                known[E][X] = j
                wl.append(j)
            waits[i] = wl
            for j in wl:
                assert ops[j]["fn"] is not None
                needed.add(j)
        sem_of = {}
        cnt = {e: 0 for e in engs}
        eng_sems = {e: [] for e in engs}
        dma_sems = {e: [] for e in engs}
        dma_cnt = {}
        dma_rr = {e: 0 for e in engs}
        dma_last = {}
        nsem = [0]

        def new_sem(tag):
            nsem[0] += 1
            return self.stack.enter_context(nc.semaphore("%s_%d" % (tag, nsem[0])))

        for i, op in enumerate(ops):
            E = op["eng"]
            if op["kind"] == "c":
                if i in needed:
                    n = cnt[E]
                    cnt[E] += 1
                    si = n // SEM_CAP
                    while len(eng_sems[E]) <= si:
                        eng_sems[E].append(new_sem("e" + E))
                    sem_of[i] = (eng_sems[E][si], n % SEM_CAP + 1, 1)
            elif op["kind"] == "d":
                if len(dma_sems[E]) < N_DMA_SEMS:
                    dma_sems[E].append(new_sem("d" + E))
                    dma_cnt[(E, len(dma_sems[E]) - 1)] = 0
                k = dma_rr[E] % len(dma_sems[E]) if len(dma_sems[E]) == N_DMA_SEMS else len(dma_sems[E]) - 1
                dma_rr[E] += 1
                pj = dma_last.get((E, k))
                if pj is not None and pj not in waits[i] and E in SEM_REARM_WAIT:
                    waits[i].append(pj)
                dma_last[(E, k)] = i
                dma_cnt[(E, k)] += 16
                sem_of[i] = (dma_sems[E][k], dma_cnt[(E, k)], 16)
            else:
                sem_of[i] = (new_sem("cc"), 1, None)
        per_eng = {e: [] for e in engs}
        for i, op in enumerate(ops):
            per_eng[op["eng"]].append(i)

        def run(E, e):
            for i in per_eng[E]:
                op = ops[i]
                for j in waits[i]:
                    s, v, _ = sem_of[j]
                    e.wait_ge(s, v)
                if op["fn"] is None:
                    continue
                ins = op["fn"](e)
                if i in sem_of:
                    s, v, inc = sem_of[i]
                    if inc is None:
                        ins.then_inc(s)
                    else:
                        ins.then_inc(s, inc)

        with nc.Block() as block:
            @block.tensor
            def _(e):
                run("pe", e)

            @block.scalar
            def _(e):
                run("act", e)

            @block.vector
            def _(e):
                run("dve", e)

            @block.gpsimd
            def _(e):
                run("pool", e)

            @block.sync
            def _(e):
                run("sp", e)


def build_program(debug=None):
    debug = debug or {}
    nc = bass.Bass("TRN2", target_bir_lowering=False)
    stack = ExitStack()
    P = Prog(nc, stack)

    def din(name, shape, dt=F32):
        return nc.dram_tensor(name, list(shape), dt, kind="ExternalInput").ap()

    h0 = din("h0", [OWN, D])
    cst_d = din("cst", [128, NCST])
    vecs_d = din("vecs", [DEPTH, 128, NV])
    fing_d = din("fing", [128, D])
    chp_d = din("chp", [DEPTH, 128, NCP])
    w_in = din("w_in", [DEPTH, D, N_IN])
    pool_w = din("pool_w", [DEPTH, 4, 64, 64])
    w_oa = din("w_out_a", [DEPTH, 256, D])
    w_ob = din("w_out_b", [DEPTH, 256, D])
    w_oc = din("w_out_c", [DEPTH, 512, D])
    w_o = din("w_o", [DEPTH, D, D])
    rw_d = din("rw", [DEPTH, D, 20])
    ew1 = din("exp_w1", [DEPTH, 16, D, 256])
    ew3 = din("exp_w3", [DEPTH, 16, D, 256])
    ew2 = din("exp_w2", [DEPTH, 16, 256, D])
    out_d = nc.dram_tensor("out", [OWN, D], F32, kind="ExternalOutput").ap()

    hbuf = nc.dram_tensor("hbuf", [OWN, D], F32)
    qx = nc.dram_tensor("qx", [8, 66, OWN], BF16)
    ox = nc.dram_tensor("ox", [512, OWN], BF16)
    NPC = 4
    def pc_of(m):
        return m // 3, m % 3
    PCN = [3, 3, 3, 2]
    kx_loc = [nc.dram_tensor("kx_loc%d" % p, [512, PCN[p] * CH], BF16) for p in range(NPC)]
    kx_all = [nc.dram_tensor("kx_all%d" % p, [1024, PCN[p] * CH], BF16) for p in range(NPC)]
    vx_loc = [nc.dram_tensor("vx_loc%d" % p, [PCN[p] * CH, 520], BF16) for p in range(NPC)]
    vx_all = [nc.dram_tensor("vx_all%d" % p, [2 * PCN[p] * CH, 520], BF16) for p in range(NPC)]
    ag_loc = [nc.dram_tensor("ag_loc%d" % p, [512, PCN[p] * CH], BF16) for p in range(NPC)]
    ag_all = [nc.dram_tensor("ag_all%d" % p, [1024, PCN[p] * CH], BF16) for p in range(NPC)]
    lf_loc = nc.dram_tensor("lf_loc", [NM * 128, 24], F32)
    lf_all = nc.dram_tensor("lf_all", [2 * NM * 128, 24], F32)
    W1s = [nc.dram_tensor("W1s%d" % l, [2048, 2048], BF16) for l in range(DEPTH)]
    W3s = [nc.dram_tensor("W3s%d" % l, [2048, 2048], BF16) for l in range(DEPTH)]
    W2s = [nc.dram_tensor("W2s%d" % l, [2048, 2048], BF16) for l in range(DEPTH)]
    ux = nc.dram_tensor("ux", [NM, 128, 8 * CH], BF16)
    XS = nc.dram_tensor("XS", [NSLT * 128, D], BF16)
    YS = nc.dram_tensor("YS", [NSLT * 128, D], F32)
    scratch = dict(hbuf=hbuf, qx=qx, ox=ox, lf_all=lf_all)
    for p in range(NPC):
        scratch["kx_all%d" % p] = kx_all[p]
        scratch["vx_all%d" % p] = vx_all[p]
        scratch["ag_all%d" % p] = ag_all[p]
    dbg_out = {}
    for nm in debug.get("dump", []):
        t = scratch[nm]
        dbg_out[nm] = nc.dram_tensor("dbg_" + nm, list(t.shape), t.dtype, kind="ExternalOutput")
    if "moe" in debug.get("dump_sb", []):
        dbg_out["moe"] = nc.dram_tensor("dbg_moe", [128, 512], F32, kind="ExternalOutput")
    if "F" in debug.get("dump_sb", []):
        dbg_out["F"] = nc.dram_tensor("dbg_F", [128, 528], F32, kind="ExternalOutput")

    def sb(name, shape, dt=F32):
        return stack.enter_context(nc.sbuf_tensor("sb_" + name, list(shape), dt))

    cst = sb("cst", [128, NCST])
    ident = sb("ident", [128, 128], BF16)
    vecs = sb("vecs", [128, NV])
    chp = sb("chp", [128, NCP])
    fing = sb("fing", [128, D])
    small = sb("small", [128, 64])
    FTP = sb("FTP", [128, 528 + 528 + 88 + 132])
    AR_WORDS = 44096
    AR = sb("AR", [128, AR_WORDS])

    class Arena:
        def __init__(self):
            self.off = 0

        def f32(self, n, parts=128):
            a = AR[0:parts, self.off:self.off + n]
            self.off += (n + 7) // 8 * 8
            assert self.off <= AR_WORDS, self.off
            return a

        def bf16(self, n, parts=128):
            w = (n + 1) // 2
            a = AR[0:parts, self.off:self.off + w].bitcast(BF16)
            self.off += (w + 7) // 8 * 8
            assert self.off <= AR_WORDS, self.off
            return a[:, 0:n]

    ps = [stack.enter_context(nc.psum_tensor("ps%d" % i, [128, 512], F32)) for i in range(7)]
    psT = stack.enter_context(nc.psum_tensor("psT", [128, 1024], BF16))

    def barrier():
        engs = ["pe", "act", "dve", "pool", "sp"]
        lasts = [i for i in (P.last_compute.get(x) for x in engs) if i is not None]
        asyncs = list(P.asyncs)
        P.asyncs = []
        for E in engs:
            P.add(E, None, (), (), extra=lasts + asyncs)

    rr = [0]

    def nbank(lo=0, hi=5):
        b = lo + rr[0] % (hi - lo)
        rr[0] += 1
        return b

    def psk(b):
        return ("ps", b)

    def dma(q, out, in_, reads, writes):
        return P.add(q, lambda e: e.dma_start(out=out, in_=in_), reads, writes, kind="d")

    def mm(out, lhsT, rhs, start, stop, reads, writes):
        return P.add("pe", lambda e: e.matmul(out, lhsT, rhs, start=start, stop=stop, skip_group_check=True),
                     reads, writes)

    def act(out, in_, func, reads, writes, bias=None, scale=None):
        kw = {}
        if bias is not None:
            kw["bias"] = bias
        if scale is not None:
            kw["scale"] = scale
        return P.add("act", lambda e: e.activation(out, in_, func, **kw), reads, writes)

    def tt(eng, out, in0, in1, op, reads, writes):
        return P.add(eng, lambda e: e.tensor_tensor(out, in0, in1, op), reads, writes)

    def ts(eng, out, in0, s1, s2, op0, op1, reads, writes):
        if op1 is None:
            return P.add(eng, lambda e: e.tensor_scalar(out, in0, s1, None, op0), reads, writes)
        return P.add(eng, lambda e: e.tensor_scalar(out, in0, s1, s2, op0, op1), reads, writes)

    def stt(out, in0, scalar, in1, op0, op1, reads, writes, accum=None):
        return P.add("dve", lambda e: e.scalar_tensor_tensor(out, in0, scalar, in1, op0, op1, accum_out=accum),
                     reads, writes)

    def cp(eng, out, in_, reads, writes):
        if eng == "act":
            return P.add("act", lambda e: e.copy(out, in_), reads, writes)
        return P.add(eng, lambda e: e.tensor_copy(out, in_), reads, writes)

    def memset(eng, ap, val, writes):
        return P.add(eng, lambda e: e.memset(ap, val), (), writes)

    def rsqrt(out, in_, scale, reads, writes):
        act(out, in_, AF.Sqrt, list(reads) + ["epsc"], writes, bias=epsc[0:out.shape[0], :], scale=scale)
        P.add("dve", lambda e: e.reciprocal(out, out), writes, writes)

    def v3(ap, a):
        return ap.rearrange("p (a b) -> p a b", a=a)

    dma("sp", cst[:, :], cst_d[:, :], (), ["cst"])
    dma("sp", fing[:, :], fing_d[:, :], (), ["fing"])
    cp("dve", ident[:, :], cst[:, C_ID:C_ID + 128], ["cst"], ["ident"])
    epsc = small[:, 60:61]
    onec = small[:, 61:62]
    memset("dve", epsc, EPS, ["epsc"])
    memset("dve", onec, 1.0, ["onec"])
    selA = cst[:, C_SEL:C_SEL + 1]
    selB = cst[:, C_SEL + 1:C_SEL + 2]

    def maskap(w, kt, qlo):
        o = C_MASK + (w * 3 + kt) * CH
        return cst[:, o + qlo:o + CH]

    FT = FTP[:, 0:528].rearrange("p (g h) -> p g h", h=8)
    CY = FTP[:, 528:1056].rearrange("p (g h) -> p g h", h=8)
    CS = FTP[:, 1056:1144].rearrange("p (m h) -> p m h", h=8)
    BI = [FTP[:, 1144 + i * 66:1144 + (i + 1) * 66] for i in range(2)]

    def load_h(src, m, htile, key):
        dma("sp", htile, src[m * CH:(m + 1) * CH, :].rearrange("(i p) d -> p i d", p=128),
            (("hsrc", m),), [key])

    def norm_to_uT(htile, hkey, ubf, U, ukey, goff, l):
        for i in range(3):
            ss = small[:, i:i + 1]
            stt(ubf, htile[:, i, :], 1.0, htile[:, i, :], ALU.mult, ALU.mult,
                [hkey], ["ubf", ("ss", i)], accum=ss)
            rs = small[:, 4 + i:5 + i]
            rsqrt(rs, ss, 1.0 / D, [("ss", i)], [("rs", i)])
            stt(ubf, htile[:, i, :], rs, vecs[:, goff:goff + D], ALU.mult, ALU.mult,
                [hkey, ("rs", i), ("vecs", l)], ["ubf"])
            for k in range(8):
                P.add("pe", lambda e, k=k: e.transpose(psT[:, k * 128:(k + 1) * 128], ubf[:, k * 128:(k + 1) * 128],
                                                       ident[:, :]),
                      ["ubf", "ident"], ["psT"])
            cp("act", U[:, :, i * 128:(i + 1) * 128], psT[:, :].rearrange("p (k t) -> p k t", k=8),
               ["psT"], [ukey])

    def norm_dve(htile, hkey, i, ub, goff, l):
        ss = small[:, i:i + 1]
        stt(ub, htile[:, i, :], 1.0, htile[:, i, :], ALU.mult, ALU.mult, [hkey], [("ub3", i), ("ss", i)], accum=ss)
        rs = small[:, 4 + i:5 + i]
        rsqrt(rs, ss, 1.0 / D, [("ss", i)], [("rs", i)])
        stt(ub, htile[:, i, :], rs, vecs[:, goff:goff + D], ALU.mult, ALU.mult,
            [hkey, ("rs", i), ("vecs", l)], [("ub3", i)])

    def norm_pe(ub, i, U, ukey):
        for k in range(8):
            P.add("pe", lambda e, k=k: e.transpose(psT[:, k * 128:(k + 1) * 128], ub[:, k * 128:(k + 1) * 128],
                                                   ident[:, :]),
                  [("ub3", i), "ident"], ["psT"])
        cp("act", U[:, :, i * 128:(i + 1) * 128], psT[:, :].rearrange("p (k t) -> p k t", k=8), ["psT"], [ukey])

    def load_layer_vecs(l):
        dma("sp", vecs[:, :], vecs_d[l, :, :], (), [("vecs", l)])
        dma("sp", chp[:, :], chp_d[l, :, :], (), [("chp", l)])

    def phase1(l, hsrc):
        A = Arena()
        W1 = A.bf16(8 * 2312).rearrange("p (k c) -> p k c", k=8)
        ht = [v3(A.f32(3 * D), 3) for _ in range(2)]
        ubf = A.bf16(D)
        ub3 = [A.bf16(D) for _ in range(3)]
        uT = [v3(A.bf16(8 * CH), 8) for _ in range(2)]
        sg = v3(A.f32(2 * CH), 2)
        ab = v3(A.bf16(2 * CH), 2)
        gb = v3(A.bf16(2 * CH), 2)
        kb = v3(A.bf16(4 * CH), 4)
        qb = v3(A.bf16(4 * CH), 4)
        vts = [A.bf16(520) for _ in range(2)]
        lts = [A.f32(24) for _ in range(2)]
        for (c0, c1) in ((0, 768), (768, 1280), (1280, 1792), (1792, 2312)):
            dma("pool", W1[:, :, c0:c1], w_in[l, :, c0:c1].rearrange("(k p) c -> p k c", p=128),
                (), [("W1", c0)])
        wkeys = [("W1", 0), ("W1", 768), ("W1", 1280), ("W1", 1792)]
        for i in range(2):
            memset("pool", vts[i], 1.0, [("vtile", i)])
        stg = [(A.bf16(2048), A.bf16(2048), A.bf16(2048)) for _ in range(4)]

        def prep_load(e):
            a1, a3, a2 = stg[e % 4]
            dma("pool", v3(a1, 8), ew1[l, e, :, :].rearrange("(k p) c -> p k c", p=128), (), [("pw1", e % 4)])
            dma("pool", v3(a3, 8), ew3[l, e, :, :].rearrange("(k p) c -> p k c", p=128), (), [("pw3", e % 4)])
            dma("pool", v3(a2, 2), ew2[l, e, :, :].rearrange("(k p) c -> p k c", p=128), (), [("pw2", e % 4)])

        def prep_store(e):
            a1, a3, a2 = stg[e % 4]
            dma("sp", W1s[l][e * 128:(e + 1) * 128, :], a1, [("pw1", e % 4)], [("W1s", e)])
            dma("sp", W3s[l][e * 128:(e + 1) * 128, :], a3, [("pw3", e % 4)], [("W3s", e)])
            dma("sp", W2s[l][e * 128:(e + 1) * 128, :], a2, [("pw2", e % 4)], [("W2s", e)])
        load_h(hsrc, 0, ht[0], ("ht", 0))
        for m in range(NM):
            hb = m % 2
            if m + 1 < NM:
                load_h(hsrc, m + 1, ht[1 - hb], ("ht", 1 - hb))
            U = uT[hb]
            ukey = ("uT", hb)
            if m < 8:
                prep_load(2 * m)
                prep_load(2 * m + 1)
            if 1 <= m < 9:
                prep_store(2 * m - 2)
                prep_store(2 * m - 1)
            if m == 0:
                for i in range(3):
                    norm_dve(ht[hb], ("ht", hb), i, ub3[i], V_G1, l)
                    norm_pe(ub3[i], i, U, ukey)
            dma("sp", ux[m, :, :], U.rearrange("p k t -> p (k t)"), [ukey], [("ux", m)])
            nxt = m + 1 < NM

            def early_norm(i):
                if nxt:
                    norm_dve(ht[1 - hb], ("ht", 1 - hb), i, ub3[i], V_G1, l)
            osl = slice(m * CH, (m + 1) * CH)
            pp, pm = pc_of(m)
            psl = slice(pm * CH, (pm + 1) * CH)
            p1stop = debug.get("p1stop", 99)
            if p1stop < 1:
                continue

            def fm_group(c0, M):
                b = nbank()
                for k in range(8):
                    mm(ps[b][0:M, 0:CH], W1[:, k, c0:c0 + M], U[:, k, :], k == 0, k == 7,
                       wkeys + [ukey], [psk(b)])
                return b
            for cc in range(2):
                b = fm_group(cc * 128, 128)
                cp("act", ab[:, cc, :], ps[b][:, 0:CH], [psk(b)], [("ab", cc)])
            dma("sp", ag_loc[pp][0:256, psl].rearrange("(c p) t -> p c t", p=128), ab,
                [("ab", 0), ("ab", 1)], [("ag_loc", m, 0)])
            early_norm(0)
            for cc in range(2):
                b2 = fm_group(512 + cc * 128, 128)
                act(sg[:, cc, :], ps[b2][:, 0:CH], AF.Sigmoid, [psk(b2)], [("sg", cc)])
                b1 = fm_group(256 + cc * 128, 128)
                tt("dve", gb[:, cc, :], ps[b1][:, 0:CH], sg[:, cc, :], ALU.mult, [psk(b1), ("sg", cc)], [("gb", cc)])
            dma("sp", ag_loc[pp][256:512, psl].rearrange("(c p) t -> p c t", p=128), gb,
                [("gb", 0), ("gb", 1)], [("ag_loc", m, 1)])
            early_norm(1)
            for kp in range(4):
                b = fm_group(768 + kp * 128, 128)
                P.add("act", lambda e, b=b, kp=kp: e.mul(qb[:, kp, :], ps[b][:, 0:CH], 0.125),
                      [psk(b)], [("qb", kp)])
                b = fm_group(1280 + kp * 128, 128)
                cp("dve", kb[:, kp, :], ps[b][:, 0:CH], [psk(b)], [("kb", kp)])
            for h in range(8):
                dma("sp", qx[h, 0:64, osl], qb[(h % 2) * 64:(h % 2) * 64 + 64, h // 2, :],
                    [("qb", h // 2)], [("qx", m, h)])
            dma("sp", kx_loc[pp][:, psl].rearrange("(k p) t -> p k t", p=128), kb[:, :, :],
                [("kb", kp) for kp in range(4)], [("kx_loc", m)])
            early_norm(2)
            if nxt:
                norm_pe(ub3[0], 0, uT[1 - hb], ("uT", 1 - hb))
                norm_pe(ub3[1], 1, uT[1 - hb], ("uT", 1 - hb))
            for i in range(3):
                vb = (m * 3 + i) % 2
                vt = vts[vb].rearrange("p (h d) -> p h d", h=8)
                b = nbank()
                for k in range(8):
                    mm(ps[b][:, 0:512], U[:, k, i * 128:(i + 1) * 128], W1[:, k, 1792:2304], k == 0, k == 7,
                       wkeys + [ukey], [psk(b)])
                cp("act", vt[:, :, 0:64], ps[b][:, 0:512].rearrange("p (h d) -> p h d", h=8), [psk(b)],
                   [("vtile", vb)])
                r0 = pm * CH + i * 128
                dma("sp", vx_loc[pp][r0:r0 + 128, :], vts[vb], [("vtile", vb)], [("vx_loc", m, i)])
                if p1stop < 5:
                    continue
                for k in range(8):
                    mm(ps[6][:, i * 8:(i + 1) * 8], U[:, k, i * 128:(i + 1) * 128], W1[:, k, 2304:2312], k == 0, k == 7,
                       wkeys + [ukey], [psk(6)])
                if i == 1 and nxt:
                    norm_pe(ub3[2], 2, uT[1 - hb], ("uT", 1 - hb))
            lt = lts[m % 2]
            lk = ("lt", m % 2)
            tt("dve", lt, ps[6][:, 0:24], vecs[:, V_BF3:V_BF3 + 24], ALU.add, [psk(6), ("vecs", l)], [lk])
            act(lt, lt, AF.Exp, [lk], [lk], scale=-1.0)
            act(lt, lt, AF.Ln, [lk, "onec"], [lk], bias=onec)
            ts("dve", lt, lt, -1.0, None, ALU.mult, None, [lk], [lk])
            dma("sp", lf_loc[m * 128:(m + 1) * 128, :], lt, [lk], [("lf_loc", m)])
            if m == NM - 1 or (m + 1) % 3 == 0:
                exchange_piece(m // 3)

    groups = [[0, 1], [2, 3], [4, 5], [6, 7]]

    def cc(src, dst, rkeys, wkey):
        P.add("pool", lambda e: e.collective_compute("AllGather", ALU.bypass, replica_groups=groups,
                                                     ins=[src.ap().opt()], outs=[dst.ap().opt()]),
              rkeys, [wkey], kind="cc")

    def exchange_piece(p):
        ms = [m for m in range(NM) if m // 3 == p]
        cc(kx_loc[p], kx_all[p], [("kx_loc", m) for m in ms], ("kx_all", p))
        cc(vx_loc[p], vx_all[p], [("vx_loc", m, i) for m in ms for i in range(3)], ("vx_all", p))
        cc(ag_loc[p], ag_all[p], [("ag_loc", m, j) for m in ms for j in range(2)], ("ag_all", p))

    def exchange():
        cc(lf_loc, lf_all, [("lf_loc", m) for m in range(NM)], "lf_all")

    def phase2a():
        A = Arena()
        LFf = A.f32(528)
        LF = LFf.rearrange("p (m r i h) -> p m r i h", m=NM, r=2, i=3)
        TT = A.f32(528)
        INC = A.f32(528)
        ONE = A.f32(66)
        DSf = A.f32(264)
        DS = DSf.rearrange("p (j h) -> p j h", h=8)
        DH = A.bf16(264)
        DL = A.bf16(264)
        TRB = v3(A.bf16(3 * 128), 3)
        for r in range(2):
            dma("sp", LFf.rearrange("p (m r x) -> p m r x", m=NM, r=2)[:, :, r, :],
                lf_all[r * NM * 128:(r + 1) * NM * 128, :].rearrange("(m p) x -> p m x", p=128),
                ["lf_all"], [("LF", r)])
        lk = [("LF", 0), ("LF", 1)]
        memset("dve", ONE, 1.0, ["ONE"])
        FTf = FT.rearrange("p g h -> p (g h)")
        CYf = CY.rearrange("p g h -> p (g h)")
        for half in range(2):
            cs = slice(half * 264, (half + 1) * 264)
            mm(ps[half][:, 0:264], cst[:, C_U:C_U + 128], LFf[:, cs], True, True, lk + ["cst"], [psk(half)])
            mm(ps[2 + half][:, 0:264], cst[:, C_ONES:C_ONES + 128], LFf[:, cs], True, True, lk + ["cst"],
               [psk(2 + half)])
            cp("act", FTf[:, cs], ps[half][:, 0:264], [psk(half)], [("FTw", half)])
            cp("dve", TT[:, cs], ps[2 + half][:, 0:264], [psk(2 + half)], [("TT", half)])
        TT3 = TT.rearrange("p (g h) -> p g h", h=8)
        INC3 = INC.rearrange("p (g h) -> p g h", h=8)
        for h in range(8):
            P.add("dve", lambda e, h=h: e.tensor_tensor_scan(INC3[:, :, h], ONE, TT3[:, :, h], 0.0, ALU.mult, ALU.add),
                  [("TT", 0), ("TT", 1), "ONE"], [("INC", h)])
        ik = [("INC", h) for h in range(8)]
        tt("dve", CYf, INC, TT, ALU.subtract, ik + [("TT", 0), ("TT", 1)], ["CY"])
        tt("dve", FTf, FTf, CYf, ALU.add, [("FTw", 0), ("FTw", 1), "CY"], ["FT"])
        CY4 = CY.rearrange("p (m r i) h -> p m r i h", r=2, i=3)
        FT4 = FT.rearrange("p (m r i) h -> p m r i h", r=2, i=3)
        ts("dve", CS, CY4[:, :, 0, 0, :], selA, None, ALU.mult, None, ["CY", "cst"], ["CS"])
        stt(CS, CY4[:, :, 1, 0, :], selB, CS, ALU.mult, ALU.add, ["CY", "cst", "CS"], ["CS"])
        DS4 = DS.rearrange("p (m i) h -> p m i h", i=3)
        for i in range(3):
            ts("dve", DS4[:, :, i, :], FT4[:, :, 0, i, :], selA, None, ALU.mult, None, ["FT", "cst"], [("DS", i)])
            stt(DS4[:, :, i, :], FT4[:, :, 1, i, :], selB, DS4[:, :, i, :], ALU.mult, ALU.add,
                ["FT", "cst", ("DS", i)], [("DS", i)])
            tt("dve", DS4[:, :, i, :], DS4[:, :, i, :], CS, ALU.subtract, [("DS", i), "CS"], [("DS", i)])
        dk = [("DS", i) for i in range(3)]
        cp("dve", DH, DSf, dk, ["DH"])
        tt("dve", DSf, DSf, DH, ALU.subtract, dk + ["DH"], ["DS2"] + dk)
        cp("dve", DL, DSf, ["DS2"], ["DL"])
        for which, src in ((0, DH), (1, DL)):
            for a, (c0, n) in enumerate(((0, 128), (128, 128), (256, 8))):
                P.add("pe", lambda e, a=a, c0=c0, n=n, src=src: e.transpose(psT[0:n, a * 128:(a + 1) * 128],
                                                                             src[:, c0:c0 + n], ident[:, :]),
                      ["DH", "DL", "ident"], ["psT"])
            cp("act", TRB[:, :, :], psT[:, 0:384].rearrange("p (a t) -> p a t", a=3), ["psT"], ["TRB"])
            for a, nj in ((0, 16), (1, 16), (2, 1)):
                for jj in range(nj):
                    j = a * 16 + jj
                    dma("sp", qx[:, 64 + which, j * 128:(j + 1) * 128],
                        TRB[jj * 8:(jj + 1) * 8, a, :], ["TRB"], [("qxaug", which, j)])
        if "F" in dbg_out:
            dma("sp", dbg_out["F"][:, :], FTf, ["FT"], ["dbgF"])

    def phase2b():
        A = Arena()
        QH = [A.bf16(OWN, parts=66) for _ in range(2)]
        KT = [A.bf16(NG * CH, parts=66).rearrange("p (g t) -> p g t", t=CH) for _ in range(2)]
        VT = [A.bf16(NGT * 65).rearrange("p (m r i d) -> p m r i d", r=2, i=3, d=65) for _ in range(2)]
        PT = [A.bf16(CH) for _ in range(10)]
        MT = [A.f32(CH) for _ in range(2)]
        OS2 = [A.f32(CH) for _ in range(2)]
        RC2 = [A.f32(CH) for _ in range(2)]
        OB = [A.bf16(CH) for _ in range(2)]
        BI3 = [A.f32(72) for _ in range(3)]
        qk = [("qx", m, h_) for m in range(NM) for h_ in range(8)] + \
            [("qxaug", w, j) for w in range(2) for j in range(NOWNT)]
        for i in range(2):
            memset("pool", KT[i][64:66, :, :], 1.0, [("KTones", i)])

        def load_kv(h, hb):
            dma("act", QH[hb][0:66, :], qx[h, :, :], qk, [("QH", hb)])
            for r in range(2):
                for p in range(NPC):
                    n = PCN[p]
                    dma("act", KT[hb][0:64, :, :].rearrange("p (m r) t -> p m r t", r=2)[:, 3 * p:3 * p + n, r, :],
                        kx_all[p][r * 512 + h * 64:r * 512 + (h + 1) * 64, :].rearrange("p (m t) -> p m t", t=CH),
                        [("kx_all", p)], [("KT", hb, r, p)])
                    for i in range(3):
                        dma("act", VT[hb][:, 3 * p:3 * p + n, r, i, :],
                            vx_all[p][r * n * CH:(r + 1) * n * CH, h * 65:(h + 1) * 65].rearrange(
                                "(m i p) d -> p m i d", i=3, p=128)[:, :, i, :],
                            [("vx_all", p)], [("VT", hb, r, p, i)])
        load_kv(0, 0)
        LAG = 4
        items = []
        for h in range(8):
            for m in range(NM):
                for gt in range(6 * m + 6):
                    items.append((h, m, gt))
        state = {}
        pend = {}
        NPT = len(PT)

        def s_stage(idx):
            h, m, gt = items[idx]
            hb = h % 2
            it = h * NM + m
            bi = it % 3
            if gt == 0:
                ts("dve", BI3[bi][:, 0:66], FT[:, :, h], -1.0, CS[:, m, h:h + 1], ALU.mult, ALU.add, ["FT", "CS"],
                   [("BI", bi)])
            kvk = [("KTones", hb), ("QH", hb)] + [("KT", hb, r, p) for r in range(2) for p in range(NPC)]
            G, kt = gt // 3, gt % 3
            band = G - 2 * m
            qlo = kt * 128 if band == 1 else 0
            ncol = CH - qlo
            q0 = m * CH
            b = nbank()
            mm(ps[b][:, 0:ncol], KT[hb][0:66, G, kt * 128:(kt + 1) * 128], QH[hb][0:66, q0 + qlo:q0 + CH],
               True, True, kvk, [psk(b)])
            pt = PT[idx % NPT]
            pk = ("PT", idx % NPT)
            if band >= 0:
                mt = MT[idx % 2]
                mk = ("MT", idx % 2)
                tt("dve", mt[:, 0:ncol], ps[b][:, 0:ncol], maskap(band, kt, qlo), ALU.add, [psk(b), "cst"], [mk])
                act(pt[:, 0:ncol], mt[:, 0:ncol], AF.Exp, [mk, ("BI", bi)], [pk], bias=BI3[bi][:, gt:gt + 1])
            else:
                act(pt[:, 0:ncol], ps[b][:, 0:ncol], AF.Exp, [psk(b), ("BI", bi)], [pk],
                    bias=BI3[bi][:, gt:gt + 1])
            state[idx] = (pt, pk, qlo, ncol)

        def pv_stage(idx, step):
            h, m, gt = items[idx]
            hb = h % 2
            it = h * NM + m
            ob = 5 + it % 2
            ngt = 6 * m + 6
            pt, pk, qlo, ncol = state.pop(idx)
            if gt == 0 and m == 0 and h + 1 < 8:
                load_kv(h + 1, 1 - hb)
            vk = [("VT", hb, r, p, i) for r in range(2) for p in range(NPC) for i in range(3)]
            Vt = VT[hb].rearrange("p m r i d -> p (m r i) d")
            mm(ps[ob][0:65, qlo:CH], Vt[:, gt, 0:65], pt[:, 0:ncol], gt == 0, gt == ngt - 1, vk + [pk], [psk(ob)])
            if gt == ngt - 1:
                ri = it % 2
                P.add("dve", lambda e: e.reciprocal(RC2[ri][64:65, :], ps[ob][64:65, 0:CH]), [psk(ob)], [("RC", ri)])
                cp("act", OS2[ri][0:64, :], ps[ob][0:64, 0:CH], [psk(ob)], [("OS", ri)])
                pend.setdefault(step + 2, []).append((h, m, ri))

        def norm2(h, m, ri):
            q0 = m * CH
            b = nbank()
            mm(ps[b][0:64, 0:CH], cst[64:65, C_ONES:C_ONES + 64], RC2[ri][64:65, :], True, True, [("RC", ri), "cst"],
               [psk(b)])
            obt = OB[ri]
            tt("dve", obt[0:64, :], OS2[ri][0:64, :], ps[b][0:64, 0:CH], ALU.mult, [("OS", ri), psk(b)], [("OB", ri)])
            dma("sp", ox[h * 64:(h + 1) * 64, q0:q0 + CH], obt[0:64, :], [("OB", ri)], [("ox", h, m)])

        nit = len(items)
        for step in range(nit + LAG + 3):
            if step < nit:
                s_stage(step)
            if 0 <= step - LAG < nit:
                pv_stage(step - LAG, step)
            for a in pend.pop(step, []):
                norm2(*a)
        assert not pend and not state

    def phase3a(l, hsrc):
        A = Arena()
        WG = v3(A.bf16(8 * 3072), 8)
        WOA = v3(A.bf16(2 * 1024), 2)
        WOB = v3(A.bf16(2 * 1024), 2)
        WOC = v3(A.bf16(4 * 1024), 4)
        WO = v3(A.bf16(8 * 1024), 8)
        BD = v3(A.bf16(256), 2)
        DG = A.bf16(62 * 128).rearrange("p (j d) -> p j d", d=128)
        htile = v3(A.f32(3 * D), 3)
        U2 = [v3(A.bf16(8 * CH), 8) for _ in range(2)]
        XA = v3(A.f32(800), 2)
        S2 = v3(A.f32(800), 2)
        S4 = v3(A.f32(800), 2)
        YC = v3(A.f32(768), 2)
        YQ = v3(A.f32(768), 2)
        MEAN = A.f32(CH)
        RSTD = A.f32(CH)
        T1 = A.f32(CH)
        T2 = A.f32(CH)
        T3 = A.f32(CH)
        HA = v3(A.f32(64), 2)
        SGB = [A.f32(CH) for _ in range(3)]
        XG = v3(A.bf16(832), 2)
        DF = v3(A.bf16(768), 2)
        YP = v3(A.bf16(768), 2)
        YB = v3(A.bf16(768), 2)
        HB0 = v3(A.bf16(128), 2)
        HB1 = v3(A.bf16(128), 2)
        MG = v3(A.bf16(8 * CH), 8)
        AB = v3(A.bf16(2 * CH), 2)
        OC = v3(A.bf16(4 * CH), 4)
        pbs = small[:, 40:42]
        for g3 in range(3):
            dma("pool", WG[:, :, g3 * 1024:(g3 + 1) * 1024],
                w_in[l, :, 2312 + g3 * 1024:2312 + (g3 + 1) * 1024].rearrange("(k p) c -> p k c", p=128),
                (), [("WG", g3)])
        dma("pool", WOA, w_oa[l, :, :].rearrange("(k p) c -> p k c", p=128), (), ["WOA"])
        dma("pool", WOB, w_ob[l, :, :].rearrange("(k p) c -> p k c", p=128), (), ["WOB"])
        dma("pool", WOC, w_oc[l, :, :].rearrange("(k p) c -> p k c", p=128), (), ["WOC"])
        dma("pool", WO, w_o[l, :, :].rearrange("(k p) c -> p k c", p=128), (), ["WO"])
        memset("dve", BD, 0.0, ["BD0"])
        for g in range(4):
            r0 = (g % 2) * 64
            dma("pool", BD[r0:r0 + 64, g // 2, r0:r0 + 64], pool_w[l, g, :, :], ["BD0"], [("BD", g)])
        bdk = [("BD", g) for g in range(4)]
        for cc in range(2):
            for j in range(31):
                ts("dve", DG[:, cc * 31 + j, :], ident[:, :], chp[:, CP_CW + cc * 31 + j:CP_CW + cc * 31 + j + 1],
                   None, ALU.mult, None, ["ident", ("chp", l)], [("DG", cc)])
        tt("dve", pbs, chp[:, CP_PB:CP_PB + 2], chp[:, CP_PS:CP_PS + 2], ALU.mult, [("chp", l)], ["pbs"])
        wgk = [("WG", g3) for g3 in range(3)]
        YP2 = [YP, v3(A.bf16(768), 2)]
        YB2 = [YB, v3(A.bf16(768), 2)]
        OC2 = [OC, v3(A.bf16(4 * CH), 4)]

        def prep(m, s_):
            YP_, YB_, OC_ = YP2[s_], YB2[s_], OC2[s_]
            osl = slice(m * CH, (m + 1) * CH)
            pp, pm = pc_of(m)
            psl = slice(pm * CH, (pm + 1) * CH)
            dma("sp", XG[:, :, 32:416], ag_loc[pp][256:512, psl].rearrange("(c p) t -> p c t", p=128),
                [("ag_loc", m, 1)], ["XGo"])
            dma("sp", AB, ag_loc[pp][0:256, psl].rearrange("(c p) t -> p c t", p=128),
                [("ag_loc", m, 0)], ["ABo"])
            dma("sp", OC_, ox[:, osl].rearrange("(k p) t -> p k t", p=128),
                [("ox", h, m) for h in range(8)], [("OC", s_)])
            if m > 0:
                qp, qm = pc_of(m - 1)
                e0 = (qm + 1) * CH
                dma("sp", HB0[:, :, 0:32],
                    ag_all[qp][768:1024, e0 - 32:e0].rearrange("(c p) t -> p c t", p=128),
                    [("ag_all", qp)], ["HB0g"])
                dma("sp", HB0[:, :, 32:48],
                    ag_all[qp][512:768, e0 - 16:e0].rearrange("(c p) t -> p c t", p=128),
                    [("ag_all", qp)], ["HB0a"])
            else:
                memset("pool", HB0, 0.0, ["HB0g", "HB0a"])
            e1 = (pm + 1) * CH
            dma("sp", HB1[:, :, 0:32],
                ag_all[pp][256:512, e1 - 32:e1].rearrange("(c p) t -> p c t", p=128),
                [("ag_all", pp)], ["HB1g"])
            dma("sp", HB1[:, :, 32:48],
                ag_all[pp][0:256, e1 - 16:e1].rearrange("(c p) t -> p c t", p=128),
                [("ag_all", pp)], ["HB1a"])
            hk = ["HB0g", "HB0a", "HB1g", "HB1a"]
            ts("dve", HA[:, :, 0:32], HB0[:, :, 0:32], selA, None, ALU.mult, None, hk + ["cst"], ["HAg"])
            stt(XG[:, :, 0:32], HB1[:, :, 0:32], selB, HA[:, :, 0:32], ALU.mult, ALU.add, hk + ["cst", "HAg"], ["XGh"])
            ts("dve", HA[:, :, 0:16], HB0[:, :, 32:48], selA, None, ALU.mult, None, hk + ["cst", "XGh"], ["HAa"])
            stt(XA[:, :, 0:16], HB1[:, :, 32:48], selB, HA[:, :, 0:16], ALU.mult, ALU.add, hk + ["cst", "HAa"], ["XAh"])
            cp("dve", XA[:, :, 16:400], AB, ["ABo"], ["XAo"])
            xak = ["XAh", "XAo"]
            tt("dve", S2[:, :, 1:400], XA[:, :, 1:400], XA[:, :, 0:399], ALU.add, xak, ["S2"])
            tt("dve", S4[:, :, 3:400], S2[:, :, 3:400], S2[:, :, 1:398], ALU.add, ["S2"], ["S4"])
            tt("dve", S2[:, 1, 7:400], S4[:, 1, 7:400], S4[:, 1, 3:396], ALU.add, ["S4", "S2"], ["S8", "S2"])
            tt("dve", S4[64:128, 1, 15:400], S2[64:128, 1, 15:400], S2[64:128, 1, 7:392], ALU.add, ["S8", "S4"],
               ["S16", "S4"])
            ico = C_INVC + (0 if m == 0 else 768)
            IC = cst[:, ico:ico + 768].rearrange("p (c t) -> p c t", c=2)
            wk = ["S2", "S4", "S8", "S16", "cst"]
            PT1 = YC[:, 0, :]
            for cc in range(2):
                tt("dve", PT1[0:64, :], S2[0:64, cc, 16:400], IC[0:64, cc, :], ALU.mult, wk, [("YC", 0)])
                tt("dve", PT1[64:128, :], S4[64:128, cc, 16:400], IC[64:128, cc, :], ALU.mult, wk + [("YC", 0)],
                   [("YC", 0)])
                tt("dve", DF[:, cc, :], PT1, XA[:, cc, 16:400], ALU.subtract, [("YC", 0)] + xak, [("DF", cc)])
            yield
            for cc in range(2):
                b = nbank(0, 7)
                mm(ps[b][:, 0:CH], BD[:, cc, :], DF[:, cc, :], True, True, bdk + [("DF", cc)], [psk(b)])
                act(YP_[:, cc, :], ps[b][:, 0:CH], AF.Identity, [psk(b), ("chp", l), "pbs"], [("YP", s_, cc)],
                    bias=pbs[:, cc:cc + 1], scale=chp[:, CP_PS + cc:CP_PS + cc + 1])
            for cc in range(2):
                b = nbank(0, 7)
                for j in range(31):
                    mm(ps[b][:, 0:CH], DG[:, cc * 31 + j, :], XG[:, cc, 2 + j:2 + j + CH], j == 0, j == 30,
                       [("DG", 0), ("DG", 1), "XGo", "XGh"], [psk(b)])
                act(YC[:, cc, :], ps[b][:, 0:CH], AF.Identity, [psk(b), ("chp", l)], [("YC", cc)],
                    bias=chp[:, CP_CB + cc:CP_CB + cc + 1])
                act(YQ[:, cc, :], YC[:, cc, :], AF.Square, [("YC", cc)], [("YQ", cc)])
            bm = nbank(0, 7)
            bq = nbank(0, 7)
            for cc in range(2):
                mm(ps[bm][:, 0:CH], cst[:, C_ODIV:C_ODIV + 128], YC[:, cc, :], cc == 0, cc == 1, ["cst", ("YC", cc)],
                   [psk(bm)])
            for cc in range(2):
                mm(ps[bq][:, 0:CH], cst[:, C_ODIV:C_ODIV + 128], YQ[:, cc, :], cc == 0, cc == 1, ["cst", ("YQ", cc)],
                   [psk(bq)])
            PT2 = YQ[:, 0, :]
            PT3 = YQ[:, 1, :]
            cp("act", MEAN, ps[bm][:, 0:CH], [psk(bm)], ["MEAN"])
            tt("dve", PT2, MEAN, MEAN, ALU.mult, ["MEAN"], [("YQ", 0)])
            tt("dve", RSTD, ps[bq][:, 0:CH], PT2, ALU.subtract, [psk(bq), ("YQ", 0)], ["RSTD"])
            rsqrt(RSTD, RSTD, 1.0, ["RSTD"], ["RSTD"])
            for cc in range(2):
                tt("dve", PT3, YC[:, cc, :], MEAN, ALU.subtract, [("YC", cc), "MEAN"], [("YQ", 1)])
                tt("dve", PT3, PT3, RSTD, ALU.mult, [("YQ", 1), "RSTD"], [("YQ", 1)])
                act(YB_[:, cc, :], PT3, AF.Silu, [("YQ", 1), ("chp", l)], [("YB", s_, cc)],
                    bias=chp[:, CP_LB + cc:CP_LB + cc + 1], scale=chp[:, CP_LG + cc:CP_LG + cc + 1])

        for _ in prep(0, 0):
            pass
        for m in range(NM):
            s_ = m % 2
            YP_, YB_, OC_ = YP2[s_], YB2[s_], OC2[s_]
            if m == 0:
                dma("sp", U2[0].rearrange("p k t -> p (k t)"), ux[0, :, :], [("ux", 0)], [("U3", 0)])
            if m + 1 < NM:
                dma("sp", U2[(m + 1) % 2].rearrange("p k t -> p (k t)"), ux[m + 1, :, :], [("ux", m + 1)],
                    [("U3", (m + 1) % 2)])
            load_h(hsrc, m, htile, "ht3")
            ukey = ("U3", m % 2)
            U = U2[m % 2]
            pg = prep(m + 1, 1 - s_) if m + 1 < NM else iter(())
            for f in range(8):
                if f == 1 or f == 4:
                    next(pg, None)
                for br in range(3):
                    b = nbank(0, 7)
                    c0 = br * 1024 + f * 128
                    for k in range(8):
                        mm(ps[b][:, 0:CH], WG[:, k, c0:c0 + 128], U[:, k, :], k == 0, k == 7, wgk + [ukey], [psk(b)])
                    act(SGB[br], ps[b][:, 0:CH], AF.Sigmoid, [psk(b)], [("SGB", br)])
                fs = slice(f * 128, (f + 1) * 128)
                ba = nbank(0, 7)
                for cc in range(2):
                    mm(ps[ba][:, 0:CH], WOA[:, cc, fs], YP_[:, cc, :], cc == 0, cc == 1,
                       ["WOA", ("YP", s_, 0), ("YP", s_, 1)], [psk(ba)])
                tt("dve", T1, ps[ba][:, 0:CH], SGB[0], ALU.mult, [psk(ba), ("SGB", 0)], ["T1"])
                bb = nbank(0, 7)
                for cc in range(2):
                    mm(ps[bb][:, 0:CH], WOB[:, cc, fs], YB_[:, cc, :], cc == 0, cc == 1,
                       ["WOB", ("YB", s_, 0), ("YB", s_, 1)], [psk(bb)])
                tt("dve", T2, ps[bb][:, 0:CH], SGB[1], ALU.mult, [psk(bb), ("SGB", 1)], ["T2"])
                bc = nbank(0, 7)
                for k in range(4):
                    mm(ps[bc][:, 0:CH], WOC[:, k, fs], OC_[:, k, :], k == 0, k == 3, ["WOC", ("OC", s_)], [psk(bc)])
                tt("dve", T3, ps[bc][:, 0:CH], SGB[2], ALU.mult, [psk(bc), ("SGB", 2)], ["T3"])
                tt("pool", T1, T1, T2, ALU.add, ["T1", "T2"], ["T1"])
                tt("pool", MG[:, f, :], T1, T3, ALU.add, ["T1", "T3"], [("MG", f)])
            for _ in pg:
                pass
            mgk = [("MG", f) for f in range(8)]
            for i in range(3):
                for half in range(2):
                    b = nbank(0, 7)
                    for k in range(8):
                        mm(ps[b][:, 0:512], MG[:, k, i * 128:(i + 1) * 128], WO[:, k, half * 512:(half + 1) * 512],
                           k == 0, k == 7, mgk + ["WO"], [psk(b)])
                    tt("dve", htile[:, i, half * 512:(half + 1) * 512], htile[:, i, half * 512:(half + 1) * 512],
                       ps[b][:, 0:512], ALU.add, ["ht3", psk(b)], ["ht3"])
            dma("sp", hbuf[m * CH:(m + 1) * CH, :].rearrange("(i p) d -> p i d", p=128), htile,
                ["ht3"], [("hsrc", m)])

    def phase3b(l, last):
        SCS = [list(range(0, 6)), list(range(6, NM))]
        A = Arena()
        ACCT = A.f32(18 * D).rearrange("p (t d) -> p t d", d=D)
        VTT = A.bf16(6 * 8 * CH).rearrange("p (s k t) -> p s k t", s=6, k=8)
        WS = []
        for s in range(2):
            WS.append((v3(A.bf16(2048), 8), v3(A.bf16(2048), 8), v3(A.bf16(2048), 2)))
        RW = v3(A.bf16(160), 8)
        ubf = A.bf16(D)
        LG = A.f32(20)
        GM = A.f32(4)
        OH = A.f32(4)
        EX = A.f32(4)
        ES = v3(A.f32(16), 4)
        EL = A.f32(4)
        M1 = A.f32(4)
        M2 = A.f32(4)
        E2 = A.f32(4)
        SC1 = A.f32(8)
        WGp = A.f32(4)
        CMB = A.f32(18 * 16).rearrange("p (t e) -> p t e", e=16)
        S1 = [A.f32(CH) for _ in range(2)]
        HE = A.bf16(4 * CH).rearrange("p (s c t) -> p s c t", s=2, c=2)
        OT = A.f32(D)
        dma("pool", RW, rw_d[l, :, :].rearrange("(k p) c -> p k c", p=128), (), ["RW"])
        ecount = 0
        for sc in SCS:
            for si, m in enumerate(sc):
                for i in range(3):
                    t = si * 3 + i
                    dma("sp", ACCT[:, t, :], hbuf[m * CH + i * 128:m * CH + (i + 1) * 128, :], [("hsrc", m)],
                        [("ACC", t)])
            for si, m in enumerate(sc):
                for i in range(3):
                    t = si * 3 + i
                    ss = small[:, 48:49]
                    rs = small[:, 49:50]
                    stt(ubf, ACCT[:, t, :], 1.0, ACCT[:, t, :], ALU.mult, ALU.mult, [("ACC", t)], ["ubf", "ss2"],
                        accum=ss)
                    rsqrt(rs, ss, 1.0 / D, ["ss2"], ["rs2"])
                    stt(ubf, ACCT[:, t, :], rs, vecs[:, V_G2:V_G2 + D], ALU.mult, ALU.mult,
                        [("ACC", t), "rs2", ("vecs", l)], ["ubf"])
                    for k in range(8):
                        P.add("pe", lambda e, k=k: e.transpose(psT[:, k * 128:(k + 1) * 128],
                                                               ubf[:, k * 128:(k + 1) * 128], ident[:, :]),
                              ["ubf", "ident"], ["psT"])
                    cp("act", VTT[:, si, :, i * 128:(i + 1) * 128], psT[:, :].rearrange("p (k t) -> p k t", k=8),
                       ["psT"], [("VTT", si)])
                    b = nbank(0, 7)
                    for k in range(8):
                        mm(ps[b][:, 0:20], VTT[:, si, k, i * 128:(i + 1) * 128], RW[:, k, :], k == 0, k == 7,
                           [("VTT", si), "RW"], [psk(b)])
                    tt("dve", LG, ps[b][:, 0:20], vecs[:, V_RB:V_RB + 20], ALU.add, [psk(b), ("vecs", l)], ["LG"])
                    AXX = mybir.AxisListType.X
                    P.add("dve", lambda e: e.reduce_max(GM[:, 0:1], LG[:, 0:4], AXX), ["LG"], ["GM"])
                    ts("dve", OH, LG[:, 0:4], GM[:, 0:1], None, ALU.is_equal, None, ["LG", "GM"], ["OH"])
                    ts("dve", EX, LG[:, 0:4], GM[:, 0:1], None, ALU.subtract, None, ["LG", "GM"], ["EX"])
                    act(EX, EX, AF.Exp, ["EX"], ["EX"])
                    P.add("dve", lambda e: e.reduce_sum(GM[:, 1:2], EX, AXX), ["EX", "GM"], ["GS"])
                    P.add("dve", lambda e: e.reciprocal(GM[:, 2:3], GM[:, 1:2]), ["GS"], ["GW"])
                    LE = LG[:, 4:20].rearrange("p (g e) -> p g e", g=4)
                    for g in range(4):
                        ts("dve", ES[:, g, :], LE[:, g, :], OH[:, g:g + 1], None, ALU.mult, None, ["LG", "OH"],
                           [("ES", g)])
                    esk = [("ES", g) for g in range(4)]
                    tt("dve", ES[:, 0, :], ES[:, 0, :], ES[:, 1, :], ALU.add, esk, [("ES", 0)])
                    tt("dve", ES[:, 2, :], ES[:, 2, :], ES[:, 3, :], ALU.add, esk, [("ES", 2)])
                    tt("dve", EL, ES[:, 0, :], ES[:, 2, :], ALU.add, [("ES", 0), ("ES", 2)], ["EL"])
                    P.add("dve", lambda e: e.reduce_max(SC1[:, 0:1], EL, AXX), ["EL"], ["m1"])
                    ts("dve", M1, EL, SC1[:, 0:1], None, ALU.is_equal, None, ["EL", "m1"], ["M1"])
                    stt(E2, M1, NEG, EL, ALU.mult, ALU.add, ["M1", "EL"], ["E2"])
                    P.add("dve", lambda e: e.reduce_max(SC1[:, 1:2], E2, AXX), ["E2"], ["m2"])
                    ts("dve", M2, E2, SC1[:, 1:2], None, ALU.is_equal, None, ["E2", "m2"], ["M2"])
                    tt("dve", SC1[:, 2:3], SC1[:, 0:1], SC1[:, 1:2], ALU.subtract, ["m1", "m2"], ["dm"])
                    act(SC1[:, 3:4], SC1[:, 2:3], AF.Sigmoid, ["dm"], ["p1"])
                    ts("dve", SC1[:, 4:5], SC1[:, 3:4], -1.0, 1.0, ALU.mult, ALU.add, ["p1"], ["p2"])
                    tt("dve", SC1[:, 3:4], SC1[:, 3:4], GM[:, 2:3], ALU.mult, ["p1", "GW", "p2"], ["p1g"])
                    tt("dve", SC1[:, 4:5], SC1[:, 4:5], GM[:, 2:3], ALU.mult, ["p2", "GW"], ["p2g"])
                    ts("dve", WGp, M1, SC1[:, 3:4], None, ALU.mult, None, ["M1", "p1g"], ["WGp"])
                    stt(WGp, M2, SC1[:, 4:5], WGp, ALU.mult, ALU.add, ["M2", "p2g", "WGp"], ["WGp"])
                    for g in range(4):
                        ts("dve", CMB[:, t, g * 4:(g + 1) * 4], WGp, OH[:, g:g + 1], None, ALU.mult, None,
                           ["WGp", "OH"], [("CMB", t)])

            def load_expert(e, s):
                w1, w3, w2 = WS[s]
                dma("pool", w1, ew1[l, e, :, :].rearrange("(k p) c -> p k c", p=128), (), [("EW1", s)])
                dma("pool", w3, ew3[l, e, :, :].rearrange("(k p) c -> p k c", p=128), (), [("EW3", s)])
                dma("pool", w2, ew2[l, e, :, :].rearrange("(k p) c -> p k c", p=128), (), [("EW2", s)])
            load_expert(0, ecount % 2)
            for e in range(16):
                s = ecount % 2
                ecount += 1
                if e + 1 < 16:
                    load_expert(e + 1, 1 - s)
                w1, w3, w2 = WS[s]
                for si, m in enumerate(sc):
                    hs = si % 2
                    for hc in range(2):
                        b1 = nbank(0, 7)
                        for k in range(8):
                            mm(ps[b1][:, 0:CH], w1[:, k, hc * 128:(hc + 1) * 128], VTT[:, si, k, :], k == 0, k == 7,
                               [("EW1", s), ("VTT", si)], [psk(b1)])
                        b3 = nbank(0, 7)
                        for k in range(8):
                            mm(ps[b3][:, 0:CH], w3[:, k, hc * 128:(hc + 1) * 128], VTT[:, si, k, :], k == 0, k == 7,
                               [("EW3", s), ("VTT", si)], [psk(b3)])
                        act(S1[hc], ps[b1][:, 0:CH], AF.Silu, [psk(b1)], [("S1", hc)])
                        tt("dve", HE[:, hs, hc, :], S1[hc], ps[b3][:, 0:CH], ALU.mult, [("S1", hc), psk(b3)],
                           [("HE", hs, hc)])
                    for i in range(3):
                        t = si * 3 + i
                        for half in range(2):
                            b = nbank(0, 7)
                            for hc in range(2):
                                mm(ps[b][:, 0:512], HE[:, hs, hc, i * 128:(i + 1) * 128],
                                   w2[:, hc, half * 512:(half + 1) * 512], hc == 0, hc == 1,
                                   [("HE", hs, 0), ("HE", hs, 1), ("EW2", s)], [psk(b)])
                            stt(ACCT[:, t, half * 512:(half + 1) * 512], ps[b][:, 0:512], CMB[:, t, e:e + 1],
                                ACCT[:, t, half * 512:(half + 1) * 512], ALU.mult, ALU.add,
                                [psk(b), ("CMB", t), ("ACC", t)], [("ACC", t)])
            for si, m in enumerate(sc):
                for i in range(3):
                    t = si * 3 + i
                    r0 = m * CH + i * 128
                    if not last:
                        dma("sp", hbuf[r0:r0 + 128, :], ACCT[:, t, :], [("ACC", t)], [("hsrc", m), ("hw", m, i)])
                    else:
                        ss = small[:, 52:53]
                        rs = small[:, 53:54]
                        stt(OT, ACCT[:, t, :], 1.0, ACCT[:, t, :], ALU.mult, ALU.mult, [("ACC", t)], ["OT", "ss3"],
                            accum=ss)
                        rsqrt(rs, ss, 1.0 / D, ["ss3"], ["rs3"])
                        stt(OT, ACCT[:, t, :], rs, fing[:, :], ALU.mult, ALU.mult, [("ACC", t), "rs3", "fing"], ["OT"])
                        dma("sp", out_d[r0:r0 + 128, :], OT, ["OT"], [("out", m, i)])

    def phase3b_sparse(l, last):
        A = Arena()
        AXX = mybir.AxisListType.X
        NT = NOWNT
        VBF = A.bf16(NT * D).rearrange("p (t d) -> p t d", d=D)
        INDb = A.bf16(NT * 16)
        M1G = A.f32(NT * 16)
        M2G = A.f32(NT * 16)
        RIN = A.f32(NT * 16)
        TOT = A.f32(NT * 16)
        INC = A.f32(NT * 16)
        RB = A.f32(NT * 16)
        PRD = A.f32(NT * 16)
        PW = A.f32(NT * 2).rearrange("p (t j) -> p t j", j=2)
        SLF = [A.f32(NT) for _ in range(2)]
        SLI = [AR[:, A.off + i * 40:A.off + i * 40 + NT].bitcast(I32) for i in range(2)]
        A.off += 80
        CNT = A.f32(16)
        NTL = A.f32(16)
        PC = A.f32(16)
        BEND = A.f32(16)
        BASE = A.f32(16)
        ONE16 = A.f32(16)
        ONE33 = A.f32(NT)
        TMP33 = A.f32(NT)
        TE = A.f32(NSLT)
        WIF = A.f32(NSLT)
        WII = AR[:, A.off:A.off + NSLT].bitcast(I32)
        A.off += 88
        UST = A.bf16(128)
        ONEb = A.bf16(128)
        RW = v3(A.bf16(160), 8)
        ubfx = A.bf16(D)
        HT = [A.f32(D) for _ in range(3)]
        NRS = 3
        RSET = [dict(VTt=v3(A.bf16(8 * 128), 8), LG=A.f32(20), GM=A.f32(4), OH=A.f32(4), EX=A.f32(4),
                     ES=v3(A.f32(16), 4), EL=A.f32(4), M1=A.f32(4), M2=A.f32(4), E2=A.f32(4), SC1=A.f32(8),
                     ss=A.f32(1), rs=A.f32(1)) for _ in range(NRS)]
        NWB = 3
        WB = [(A.bf16(2048), A.bf16(2048), A.bf16(2048)) for _ in range(NWB)]
        XI = [A.bf16(D) for _ in range(3)]
        XT = [v3(A.bf16(8 * 128), 8) for _ in range(2)]
        S1 = A.f32(256)
        HEb = [A.bf16(256) for _ in range(2)]
        YT = [A.f32(D) for _ in range(2)]
        Y1 = [WB[hb_][0].bitcast(F32) for hb_ in range(2)]
        Y2 = [WB[hb_][1].bitcast(F32) for hb_ in range(2)]
        OT = YT[0]
        M1G3 = M1G.rearrange("p (t e) -> p t e", e=16)
        M2G3 = M2G.rearrange("p (t e) -> p t e", e=16)
        IND3 = INDb.rearrange("p (t e) -> p t e", e=16)
        dma("pool", RW, rw_d[l, :, :].rearrange("(k p) c -> p k c", p=128), (), ["RW"])
        tt("dve", UST, cst[:, C_U:C_U + 128], cst[:, C_ID:C_ID + 128], ALU.subtract, ["cst"], ["UST"])
        cp("dve", ONEb, cst[:, C_ONES:C_ONES + 128], ["cst"], ["ONEb"])
        memset("dve", ONE16, 1.0, ["ONE16"])
        memset("dve", ONE33, 1.0, ["ONE33"])
        def router_tile(t):
            R_ = RSET[t % NRS]
            q = t % NRS
            VTt, LG, GM, OH, EX, ES, EL, M1, M2, E2, SC1 = (R_[k] for k in
                                                            ("VTt", "LG", "GM", "OH", "EX", "ES", "EL", "M1", "M2", "E2", "SC1"))
            K = lambda nm: (nm, q)
            m, i = t // 3, t % 3
            hb = t % 3
            r0 = m * CH + i * 128
            dma("sp", HT[hb], hbuf[r0:r0 + 128, :], [("hsrc", m)], [("HT", hb)])
            ss = R_["ss"]
            rs = R_["rs"]
            stt(ubfx, HT[hb], 1.0, HT[hb], ALU.mult, ALU.mult, [("HT", hb)], ["ubfx", K("ss2")], accum=ss)
            yield
            rsqrt(rs, ss, 1.0 / D, [K("ss2")], [K("rs2")])
            yield
            stt(VBF[:, t, :], HT[hb], rs, vecs[:, V_G2:V_G2 + D], ALU.mult, ALU.mult,
                [("HT", hb), K("rs2"), ("vecs", l)], [("VBF", t)])
            for k in range(8):
                P.add("pe", lambda e, k=k, t=t: e.transpose(psT[:, k * 128:(k + 1) * 128],
                                                            VBF[:, t, k * 128:(k + 1) * 128], ident[:, :]),
                      [("VBF", t), "ident"], ["psT"])
            cp("act", VTt, psT[:, :].rearrange("p (k t) -> p k t", k=8), ["psT"], [K("VTt")])
            b = nbank(0, 7)
            for k in range(8):
                mm(ps[b][:, 0:20], VTt[:, k, :], RW[:, k, :], k == 0, k == 7, [K("VTt"), "RW"], [psk(b)])
            yield
            tt("dve", LG, ps[b][:, 0:20], vecs[:, V_RB:V_RB + 20], ALU.add, [psk(b), ("vecs", l)], [K("LG")])
            yield
            tt("dve", LG, LG, cst[:, C_TB:C_TB + 20], ALU.add, [K("LG"), "cst"], [K("LG")])
            yield
            P.add("dve", lambda e: e.reduce_max(GM[:, 0:1], LG[:, 0:4], AXX), [K("LG")], [K("GM")])
            yield
            ts("dve", OH, LG[:, 0:4], GM[:, 0:1], None, ALU.is_equal, None, [K("LG"), K("GM")], [K("OH")])
            ts("dve", EX, LG[:, 0:4], GM[:, 0:1], None, ALU.subtract, None, [K("LG"), K("GM")], [K("EX")])
            yield
            act(EX, EX, AF.Exp, [K("EX")], [K("EX")])
            LE = LG[:, 4:20].rearrange("p (g e) -> p g e", g=4)
            for g in range(4):
                ts("dve", ES[:, g, :], LE[:, g, :], OH[:, g:g + 1], None, ALU.mult, None, [K("LG"), K("OH")],
                   [K(("ES", g))])
            yield
            esk = [K(("ES", g)) for g in range(4)]
            tt("dve", ES[:, 0, :], ES[:, 0, :], ES[:, 1, :], ALU.add, esk, [K(("ES", 0))])
            tt("dve", ES[:, 2, :], ES[:, 2, :], ES[:, 3, :], ALU.add, esk, [K(("ES", 2))])
            yield
            tt("dve", EL, ES[:, 0, :], ES[:, 2, :], ALU.add, [K(("ES", 0)), K(("ES", 2))], [K("EL")])
            P.add("dve", lambda e: e.reduce_sum(GM[:, 1:2], EX, AXX), [K("EX"), K("GM")], [K("GS")])
            yield
            P.add("dve", lambda e: e.reduce_max(SC1[:, 0:1], EL, AXX), [K("EL")], [K("m1")])
            P.add("dve", lambda e: e.reciprocal(GM[:, 2:3], GM[:, 1:2]), [K("GS")], [K("GW")])
            yield
            ts("dve", M1, EL, SC1[:, 0:1], None, ALU.is_equal, None, [K("EL"), K("m1")], [K("M1")])
            yield
            stt(E2, M1, NEG, EL, ALU.mult, ALU.add, [K("M1"), K("EL")], [K("E2")])
            yield
            P.add("dve", lambda e: e.reduce_max(SC1[:, 1:2], E2, AXX), [K("E2")], [K("m2")])
            yield
            ts("dve", M2, E2, SC1[:, 1:2], None, ALU.is_equal, None, [K("E2"), K("m2")], [K("M2")])
            tt("dve", SC1[:, 2:3], SC1[:, 0:1], SC1[:, 1:2], ALU.subtract, [K("m1"), K("m2")], [K("dm")])
            yield
            act(SC1[:, 3:4], SC1[:, 2:3], AF.Sigmoid, [K("dm")], [K("p1")])
            for g in range(4):
                ts("dve", M1G3[:, t, g * 4:(g + 1) * 4], M1, OH[:, g:g + 1], None, ALU.mult, None, [K("M1"), K("OH")],
                   [("M1G", t)])
                ts("dve", M2G3[:, t, g * 4:(g + 1) * 4], M2, OH[:, g:g + 1], None, ALU.mult, None, [K("M2"), K("OH")],
                   [("M2G", t)])
            yield
            ts("dve", SC1[:, 4:5], SC1[:, 3:4], -1.0, 1.0, ALU.mult, ALU.add, [K("p1")], [K("p2")])
            tt("dve", IND3[:, t, :], M1G3[:, t, :], M2G3[:, t, :], ALU.add, [("M1G", t), ("M2G", t)], [("IND", t)])
            yield
            tt("dve", PW[:, t, 0:1], SC1[:, 3:4], GM[:, 2:3], ALU.mult, [K("p1"), K("GW"), K("p2")], [("PW", t)])
            tt("dve", PW[:, t, 1:2], SC1[:, 4:5], GM[:, 2:3], ALU.mult, [K("p2"), K("GW"), ("PW", t)], [("PW", t)])
            yield

        for t0 in range(0, NT, NRS):
            gens = [router_tile(t) for t in range(t0, min(NT, t0 + NRS))]
            while gens:
                for g_ in list(gens):
                    try:
                        next(g_)
                    except StopIteration:
                        gens.remove(g_)
        indk = [("IND", t) for t in range(NT)]
        m1k = [("M1G", t) for t in range(NT)]
        m2k = [("M2G", t) for t in range(NT)]
        for half in range(2):
            cs = slice(half * 264, (half + 1) * 264)
            mm(ps[half][:, 0:264], UST, INDb[:, cs], True, True, indk + ["UST"], [psk(half)])
            mm(ps[2 + half][:, 0:264], ONEb, INDb[:, cs], True, True, indk + ["ONEb"], [psk(2 + half)])
            cp("act", RIN[:, cs], ps[half][:, 0:264], [psk(half)], [("RIN", half)])
            cp("dve", TOT[:, cs], ps[2 + half][:, 0:264], [psk(2 + half)], [("TOT", half)])
        TOT3 = TOT.rearrange("p (t e) -> p t e", e=16)
        INC3 = INC.rearrange("p (t e) -> p t e", e=16)
        RB3 = RB.rearrange("p (t e) -> p t e", e=16)
        for e_ in range(16):
            P.add("dve", lambda e, e_=e_: e.tensor_tensor_scan(INC3[:, :, e_], ONE33, TOT3[:, :, e_], 0.0,
                                                              ALU.mult, ALU.add),
                  [("TOT", 0), ("TOT", 1), "ONE33"], [("INC", e_)])
        ik = [("INC", e_) for e_ in range(16)]
        cp("dve", CNT, INC3[:, NT - 1, :], ik, ["CNT"])
        tt("dve", RB, INC, TOT, ALU.subtract, ik + [("TOT", 0), ("TOT", 1)], ["RB"])
        tt("dve", RB, RB, RIN, ALU.add, ["RB", ("RIN", 0), ("RIN", 1)], ["RB"])
        for e_ in range(16):
            ts("dve", TMP33, cst[:, C_TH:C_TH + NT], CNT[:, e_:e_ + 1], None, ALU.is_le, None, ["cst", "CNT"], ["TMP33"])
            P.add("dve", lambda e, e_=e_: e.reduce_sum(NTL[:, e_:e_ + 1], TMP33, AXX), ["TMP33"], [("NTL", e_)])
        ts("dve", PC, NTL, 128.0, None, ALU.mult, None, [("NTL", e_) for e_ in range(16)], ["PC"])
        P.add("dve", lambda e: e.tensor_tensor_scan(BEND, ONE16, PC, 0.0, ALU.mult, ALU.add), ["PC", "ONE16"], ["BEND"])
        tt("dve", BASE, BEND, PC, ALU.subtract, ["BEND", "PC"], ["BASE"])
        for t in range(NT):
            tt("dve", RB3[:, t, :], RB3[:, t, :], BASE, ALU.add, ["RB", "BASE"], ["RB"])
        for j, (MG_, mk_) in enumerate(((M1G, m1k), (M2G, m2k))):
            tt("dve", PRD, MG_, RB, ALU.mult, mk_ + ["RB"], ["PRD"])
            P.add("dve", lambda e, j=j: e.tensor_reduce(SLF[j], PRD.rearrange("p (t e) -> p t e", e=16), AXX, ALU.add),
                  ["PRD"], [("SLF", j)])
            cp("dve", SLI[j], SLF[j], [("SLF", j)], [("SLI", j)])
        memset("dve", TE, 0.0, ["TE"])
        for e_ in range(16):
            stt(TE, cst[:, C_JT:C_JT + NSLT], BEND[:, e_:e_ + 1], TE, ALU.is_ge, ALU.add, ["cst", "BEND", "TE"], ["TE"])
        ts("dve", TE, TE, 15.0, None, ALU.min, None, ["TE"], ["TE"])
        ts("dve", WIF, TE, 128.0, cst[:, C_PIDX:C_PIDX + 1], ALU.mult, ALU.add, ["TE", "cst"], ["WIF"])
        cp("dve", WII, WIF, ["WIF"], ["WII"])
        if "moe" in dbg_out and l == 0:
            dm = dbg_out["moe"]
            for (o, n, ap, k) in ((0, NT, SLF[0], ("SLF", 0)), (40, NT, SLF[1], ("SLF", 1)), (80, 16, CNT, "CNT"),
                                  (96, 16, PC, "PC"), (112, 16, BEND, "BEND"), (128, 16, BASE, "BASE"),
                                  (144, NSLT, TE, "TE"), (232, NSLT, WIF, "WIF"),
                                  (320, 66, PW.rearrange("p t j -> p (t j)"), None)):
                rk = [k] if k is not None else [("PW", t) for t in range(NT)]
                dma("sp", dm[:, o:o + n], ap, rk, [("dbgmoe", o)])
        for t in range(NT):
            for j in range(2):
                P.add("pool", lambda e, t=t, j=j: e.indirect_dma_start(
                    out=XS[:, :], out_offset=bass.IndirectOffsetOnAxis(ap=SLI[j][:, t:t + 1], axis=0),
                    in_=VBF[:, t, :], in_offset=None), [("VBF", t), ("SLI", j)], [("XS", t, j)], kind="d")
        xsk = [("XS", t, j) for t in range(NT) for j in range(2)]
        wsk = [(nm, e_) for nm in ("W1s", "W3s", "W2s") for e_ in range(16)]
        def load_slot(j):
            wb = WB[j % NWB]
            for a, (Wd, nm) in enumerate(((W1s[l], "w1"), (W3s[l], "w3"), (W2s[l], "w2"))):
                P.add("pool", lambda e, a=a, Wd=Wd, wb=wb, j=j: e.indirect_dma_start(
                    out=wb[a], out_offset=None, in_=Wd[:, :],
                    in_offset=bass.IndirectOffsetOnAxis(ap=WII[:, j:j + 1], axis=0)),
                    ["WII"] + wsk, [("WB", j % NWB, a)], kind="d")
            dma("sp", XI[j % 3], XS[j * 128:(j + 1) * 128, :], xsk, [("XI", j % 3)])
        load_slot(0)
        load_slot(1)
        for j in range(NSLT):
            if j + 2 < NSLT:
                load_slot(j + 2)
            w1, w3, w2 = WB[j % NWB]
            w1 = v3(w1, 8)
            w3 = v3(w3, 8)
            w2 = v3(w2, 2)
            wk = [("WB", j % NWB, a) for a in range(3)]
            xi = XI[j % 3]
            xt = XT[j % 2]
            for k in range(8):
                P.add("pe", lambda e, k=k, xi=xi: e.transpose(psT[:, k * 128:(k + 1) * 128],
                                                              xi[:, k * 128:(k + 1) * 128], ident[:, :]),
                      [("XI", j % 3), "ident"], ["psT"])
            cp("act", xt, psT[:, :].rearrange("p (k t) -> p k t", k=8), ["psT"], [("XT", j % 2)])
            b1 = nbank(0, 7)
            b3 = nbank(0, 7)
            for hc in range(2):
                for k in range(8):
                    mm(ps[b1][:, hc * 128:(hc + 1) * 128], w1[:, k, hc * 128:(hc + 1) * 128], xt[:, k, :],
                       k == 0, k == 7, wk + [("XT", j % 2)], [psk(b1)])
                for k in range(8):
                    mm(ps[b3][:, hc * 128:(hc + 1) * 128], w3[:, k, hc * 128:(hc + 1) * 128], xt[:, k, :],
                       k == 0, k == 7, wk + [("XT", j % 2)], [psk(b3)])
            act(S1, ps[b1][:, 0:256], AF.Silu, [psk(b1)], ["S1s"])
            he = HEb[j % 2]
            tt("dve", he, S1, ps[b3][:, 0:256], ALU.mult, ["S1s", psk(b3)], [("HEb", j % 2)])
            yt = YT[j % 2]
            for half in range(2):
                b = nbank(0, 7)
                for hc in range(2):
                    mm(ps[b][:, 0:512], he[:, hc * 128:(hc + 1) * 128], w2[:, hc, half * 512:(half + 1) * 512],
                       hc == 0, hc == 1, wk + [("HEb", j % 2)], [psk(b)])
                cp("act" if half == 0 else "dve", yt[:, half * 512:(half + 1) * 512], ps[b][:, 0:512], [psk(b)],
                   [("YT", j % 2, half)])
            dma("sp", YS[j * 128:(j + 1) * 128, :], yt, [("YT", j % 2, 0), ("YT", j % 2, 1)], [("YS", j)])
        ysk = [("YS", j) for j in range(NSLT)]
        def load_comb(t):
            m, i = t // 3, t % 3
            r0 = m * CH + i * 128
            hb = t % 2
            dma("sp", HT[hb], hbuf[r0:r0 + 128, :], [("hsrc", m)], [("HT", hb)])
            for j, Yb in enumerate((Y1, Y2)):
                P.add("pool", lambda e, t=t, j=j, Yb=Yb, hb=hb: e.indirect_dma_start(
                    out=Yb[hb], out_offset=None, in_=YS[:, :],
                    in_offset=bass.IndirectOffsetOnAxis(ap=SLI[j][:, t:t + 1], axis=0)),
                    [("SLI", j)] + ysk, [("Yg", j, hb), ("WB", hb, j)], kind="d")
        load_comb(0)
        for t in range(NT):
            if t + 1 < NT:
                load_comb(t + 1)
            m, i = t // 3, t % 3
            r0 = m * CH + i * 128
            hb = t % 2
            stt(HT[hb], Y1[hb], PW[:, t, 0:1], HT[hb], ALU.mult, ALU.add, [("Yg", 0, hb), ("PW", t), ("HT", hb)],
                [("HT", hb)])
            stt(HT[hb], Y2[hb], PW[:, t, 1:2], HT[hb], ALU.mult, ALU.add, [("Yg", 1, hb), ("PW", t), ("HT", hb)],
                [("HT", hb)])
            if not last:
                dma("sp", hbuf[r0:r0 + 128, :], HT[hb], [("HT", hb)], [("hsrc", m), ("hw", m, i)])
            else:
                ss = small[:, 52:53]
                rs = small[:, 53:54]
                otk = [("YT", 0, 0), ("YT", 0, 1)]
                stt(OT, HT[hb], 1.0, HT[hb], ALU.mult, ALU.mult, [("HT", hb)], otk + ["ss3"], accum=ss)
                rsqrt(rs, ss, 1.0 / D, ["ss3"], ["rs3"])
                stt(OT, HT[hb], rs, fing[:, :], ALU.mult, ALU.mult, [("HT", hb), "rs3", "fing"], otk)
                dma("sp", out_d[r0:r0 + 128, :], OT, otk, [("out", m, i)])

    phases = debug.get("phases", None)
    n = 0

    def want():
        return phases is None or n < phases
    for l in range(DEPTH):
        src = h0 if l == 0 else hbuf
        for ph in ("p1", "ex", "p2a", "p2b", "p3a", "p3b"):
            if not want():
                break
            if ph == "p1":
                load_layer_vecs(l)
                phase1(l, src)
            elif ph == "ex":
                exchange()
            elif ph == "p2a":
                phase2a()
            elif ph == "p2b":
                phase2b()
            elif ph == "p3a":
                phase3a(l, src)
            else:
                if SPARSE_MOE:
                    phase3b_sparse(l, l == DEPTH - 1)
                else:
                    phase3b(l, l == DEPTH - 1)
            barrier()
            n += 1
    for nm, t in dbg_out.items():
        if nm in ("F", "moe"):
            continue
        src_t = scratch[nm]
        P.add("sp", lambda e, t=t, src_t=src_t: e.dma_start(out=t.ap(), in_=src_t.ap()), (), [("dbg", nm)], kind="d")
    barrier()
    P.emit()
    return nc, stack


def _constants(c):
    cst = np.zeros((128, NCST), np.float32)
    k = np.arange(128)
    cst[:, C_U:C_U + 128] = (k[:, None] <= k[None, :]).astype(np.float32)
    cst[:, C_ONES:C_ONES + 128] = 1.0
    cst[:, C_ODIV:C_ODIV + 128] = 1.0 / 256.0
    cst[:, C_SEL] = 1.0 if c == 0 else 0.0
    cst[:, C_SEL + 1] = 0.0 if c == 0 else 1.0
    q = np.arange(CH)
    for w in range(2):
        for kt in range(3):
            kpos = kt * 128 + k
            causal = np.where(kpos[:, None] <= q[None, :], 0.0, NEG).astype(np.float32)
            if c == 0:
                mk = causal if w == 0 else np.full((128, CH), NEG, np.float32)
            else:
                mk = np.zeros((128, CH), np.float32) if w == 0 else causal
            o = C_MASK + (w * 3 + kt) * CH
            cst[:, o:o + CH] = mk
    wins = np.array([2, 4, 8, 16], np.float32)
    for first in range(2):
        for cc in range(2):
            for half in range(2):
                W = wins[cc * 2 + half]
                t = np.arange(CH, dtype=np.float32)
                if first == 0 and c == 0:
                    cnt = np.minimum(t + 1.0, W)
                else:
                    cnt = np.full(CH, W, np.float32)
                o = C_INVC + (first * 2 + cc) * CH
                cst[half * 64:(half + 1) * 64, o:o + CH] = (1.0 / cnt)[None, :]
    cst[:, C_ID:C_ID + 128] = np.eye(128, dtype=np.float32)
    cst[:, C_JT:C_JT + 82] = (128.0 * np.arange(82, dtype=np.float32))[None, :]
    cst[:, C_TH:C_TH + 33] = (128.0 * np.arange(33, dtype=np.float32) + 1.0)[None, :]
    cst[:, C_PIDX] = np.arange(128, dtype=np.float32)
    cst[:, C_TB:C_TB + 4] = (-1e-7 * np.arange(4, dtype=np.float32))[None, :]
    cst[:, C_TB + 4:C_TB + 20] = np.tile(-1e-7 * np.arange(4, dtype=np.float32), 4)[None, :]
    return cst


_CACHE = {}


def kernel(x, meta, norm1_g, w_in, b_forget, pool_w, pool_b, pool_scale, conv_w, conv_b, conv_ln_g,
           conv_ln_b, w_out_a, w_out_b, w_out_c, w_o, norm2_g, router_g, router_g_b, router_e,
           router_e_b, exp_w1, exp_w3, exp_w2, final_g, _debug=None):
    f = lambda a: np.ascontiguousarray(np.asarray(a, dtype=np.float32))
    x = f(x)
    B = x.shape[0]
    L = 16 + x.shape[1]
    LP = NG * CH
    meta = f(meta)
    vecs = np.zeros((DEPTH, 128, NV), np.float32)
    chp = np.zeros((DEPTH, 128, NCP), np.float32)
    for l in range(DEPTH):
        vecs[l, :, V_G1:V_G1 + D] = f(norm1_g)[l][None, :]
        vecs[l, :, V_G2:V_G2 + D] = f(norm2_g)[l][None, :]
        vecs[l, :, V_BF:V_BF + 8] = f(b_forget)[l][None, :]
        vecs[l, :, V_BF3:V_BF3 + 24] = np.tile(f(b_forget)[l], 3)[None, :]
        vecs[l, :, V_RB:V_RB + 4] = f(router_g_b)[l][None, :]
        vecs[l, :, V_RB + 4:V_RB + 20] = f(router_e_b)[l][None, :]
        for cc in range(2):
            sl = slice(cc * 128, (cc + 1) * 128)
            chp[l, :, CP_PB + cc] = f(pool_b)[l].reshape(256)[sl]
            chp[l, :, CP_PS + cc] = f(pool_scale)[l][sl]
            chp[l, :, CP_CB + cc] = f(conv_b)[l][sl]
            chp[l, :, CP_LG + cc] = f(conv_ln_g)[l][sl]
            chp[l, :, CP_LB + cc] = f(conv_ln_b)[l][sl]
            chp[l, :, CP_CW + cc * 31:CP_CW + (cc + 1) * 31] = f(conv_w)[l][:, sl].T
    fing = np.ascontiguousarray(np.broadcast_to(f(final_g)[None, :], (128, D)))
    rw = np.ascontiguousarray(np.concatenate([f(router_g), f(router_e)], axis=-1))
    shared = dict(vecs=vecs, fing=fing, chp=chp, w_in=f(w_in), pool_w=f(pool_w), w_out_a=f(w_out_a),
                  w_out_b=f(w_out_b), w_out_c=f(w_out_c), w_o=f(w_o), rw=rw, exp_w1=f(exp_w1),
                  exp_w3=f(exp_w3), exp_w2=f(exp_w2))
    csts = [_constants(0), _constants(1)]
    in_maps = []
    for core in range(NCORES):
        b, c = core // 2, core % 2
        seq = np.zeros((LP, D), np.float32)
        seq[:16] = meta
        seq[16:L] = x[b]
        own = seq.reshape(NM, 2, CH, D)[:, c].reshape(OWN, D)
        d = dict(shared)
        d["h0"] = np.ascontiguousarray(own)
        d["cst"] = csts[c]
        in_maps.append(d)
    key = repr(sorted((_debug or {}).items()))
    if key not in _CACHE:
        _CACHE[key] = build_program(_debug)
    nc, _ = _CACHE[key]
    res = run_bass_kernel_spmd(nc, in_maps, core_ids=list(range(NCORES)))
    out = np.zeros((B, LP, D), np.float32)
    for core in range(NCORES):
        b, c = core // 2, core % 2
        out[b].reshape(NM, 2, CH, D)[:, c] = res.results[core]["out"].reshape(NM, CH, D)
    if _debug:
        kernel.last = res
    return np.ascontiguousarray(out[:, 16:L])
```

```python
import numpy as np
import ml_dtypes
from contextlib import ExitStack
import concourse.bass as bass
import concourse.mybir as mybir
from concourse.bass_utils import run_bass_kernel_spmd

F32 = mybir.dt.float32
BF16 = mybir.dt.bfloat16
I32 = mybir.dt.int32
SPARSE_MOE = True
ALU = mybir.AluOpType
AF = mybir.ActivationFunctionType

D = 1024
DEPTH = 2
NCORES = 8
CH = 384
NM = 11
OWN = NM * CH
NG = 2 * NM
NGT = NG * 3
NOWNT = NM * 3
N_IN = 5384
EPS = 1e-6
NEG = -30000.0

C_U, C_ONES, C_ODIV, C_SEL, C_MASK, C_INVC, C_ID = 0, 128, 256, 384, 386, 386 + 2304, 386 + 2304 + 1536
C_JT = C_ID + 128
C_TH = C_JT + 82
C_PIDX = C_TH + 33
C_TB = C_PIDX + 1
NCST = C_TB + 20
NSLT = 82
V_G1, V_G2, V_BF, V_RB = 0, 1024, 2048, 2056
NV = 2076
CP_PB, CP_PS, CP_CB, CP_LG, CP_LB, CP_CW = 0, 2, 4, 6, 8, 10
NCP = 10 + 62

SAME_ENGINE_SYNC = True
SEM_CAP = 16000
N_DMA_SEMS = 20
SEM_REARM_WAIT = ("sp", "act")


class Prog:
    def __init__(self, nc, stack):
        self.nc = nc
        self.stack = stack
        self.ops = []
        self.last_w = {}
        self.readers = {}
        self.last_compute = {}
        self.asyncs = []

    def add(self, eng, fn, reads=(), writes=(), kind="c", extra=()):
        i = len(self.ops)
        deps = {}
        for j in extra:
            deps[j] = "raw"
        for r in reads:
            w = self.last_w.get(r)
            if w is not None:
                deps[w] = "raw"
        for k in writes:
            w = self.last_w.get(k)
            if w is not None:
                deps[w] = "raw"
            for _, j in self.readers.get(k, {}).items():
                if j not in deps:
                    deps[j] = "war"
        for r in reads:
            self.readers.setdefault(r, {})[eng if kind == "c" else (eng, i)] = i
        for k in writes:
            self.last_w[k] = i
            self.readers[k] = {}
        self.ops.append(dict(eng=eng, fn=fn, deps=deps, kind=kind))
        if fn is not None:
            if kind == "c":
                self.last_compute[eng] = i
            else:
                self.asyncs.append(i)
        return i

    def emit(self):
        nc = self.nc
        ops = self.ops
        engs = ["pe", "act", "dve", "pool", "sp"]
        known = {e: {} for e in engs}
        needed = set()
        waits = [None] * len(ops)
        for i, op in enumerate(ops):
            E = op["eng"]
            wl = []
            best = {}
            for j, kd in op["deps"].items():
                oj = ops[j]
                if oj["kind"] != "c":
                    if known[E].get(("d", j)):
                        continue
                    known[E][("d", j)] = True
                    wl.append(j)
                    continue
                X = oj["eng"]
                if X == E and op["kind"] == "c":
                    if E == "pe" or kd == "war" or not SAME_ENGINE_SYNC:
                        continue
                if j > best.get(X, -1):
                    best[X] = j
            for X, j in best.items():
                if known[E].get(X, -1) >= j:
                    continue
                known[E][X] = j
                wl.append(j)
            waits[i] = wl
            for j in wl:
                assert ops[j]["fn"] is not None
                needed.add(j)
        sem_of = {}
        cnt = {e: 0 for e in engs}
        eng_sems = {e: [] for e in engs}
        dma_sems = {e: [] for e in engs}
        dma_cnt = {}
        dma_rr = {e: 0 for e in engs}
        dma_last = {}
        nsem = [0]

        def new_sem(tag):
            nsem[0] += 1
            return self.stack.enter_context(nc.semaphore("%s_%d" % (tag, nsem[0])))

        for i, op in enumerate(ops):
            E = op["eng"]
            if op["kind"] == "c":
                if i in needed:
                    n = cnt[E]
                    cnt[E] += 1
                    si = n // SEM_CAP
                    while len(eng_sems[E]) <= si:
                        eng_sems[E].append(new_sem("e" + E))
                    sem_of[i] = (eng_sems[E][si], n % SEM_CAP + 1, 1)
            elif op["kind"] == "d":
                if len(dma_sems[E]) < N_DMA_SEMS:
                    dma_sems[E].append(new_sem("d" + E))
                    dma_cnt[(E, len(dma_sems[E]) - 1)] = 0
                k = dma_rr[E] % len(dma_sems[E]) if len(dma_sems[E]) == N_DMA_SEMS else len(dma_sems[E]) - 1
                dma_rr[E] += 1
                pj = dma_last.get((E, k))
                if pj is not None and pj not in waits[i] and E in SEM_REARM_WAIT:
                    waits[i].append(pj)
                dma_last[(E, k)] = i
                dma_cnt[(E, k)] += 16
                sem_of[i] = (dma_sems[E][k], dma_cnt[(E, k)], 16)
            else:
                sem_of[i] = (new_sem("cc"), 1, None)
        per_eng = {e: [] for e in engs}
        for i, op in enumerate(ops):
            per_eng[op["eng"]].append(i)

        def run(E, e):
            for i in per_eng[E]:
                op = ops[i]
                for j in waits[i]:
                    s, v, _ = sem_of[j]
                    e.wait_ge(s, v)
                if op["fn"] is None:
                    continue
                ins = op["fn"](e)
                if i in sem_of:
                    s, v, inc = sem_of[i]
                    if inc is None:
                        ins.then_inc(s)
                    else:
                        ins.then_inc(s, inc)

        with nc.Block() as block:
            @block.tensor
            def _(e):
                run("pe", e)

            @block.scalar
            def _(e):
                run("act", e)

            @block.vector
            def _(e):
                run("dve", e)

            @block.gpsimd
            def _(e):
                run("pool", e)

            @block.sync
            def _(e):
                run("sp", e)


def build_program(debug=None):
    debug = debug or {}
    nc = bass.Bass("TRN2", target_bir_lowering=False)
    stack = ExitStack()
    P = Prog(nc, stack)

    def din(name, shape, dt=F32):
        return nc.dram_tensor(name, list(shape), dt, kind="ExternalInput").ap()

    h0 = din("h0", [OWN, D])
    cst_d = din("cst", [128, NCST])
    vecs_d = din("vecs", [DEPTH, 128, NV])
    fing_d = din("fing", [128, D])
    chp_d = din("chp", [DEPTH, 128, NCP])
    w_in = din("w_in", [DEPTH, D, N_IN])
    pool_w = din("pool_w", [DEPTH, 4, 64, 64])
    w_oa = din("w_out_a", [DEPTH, 256, D])
    w_ob = din("w_out_b", [DEPTH, 256, D])
    w_oc = din("w_out_c", [DEPTH, 512, D])
    w_o = din("w_o", [DEPTH, D, D])
    rw_d = din("rw", [DEPTH, D, 20])
    ew1 = din("exp_w1", [DEPTH, 16, D, 256])
    ew3 = din("exp_w3", [DEPTH, 16, D, 256])
    ew2 = din("exp_w2", [DEPTH, 16, 256, D])
    out_d = nc.dram_tensor("out", [OWN, D], F32, kind="ExternalOutput").ap()

    hbuf = nc.dram_tensor("hbuf", [OWN, D], F32)
    qx = nc.dram_tensor("qx", [8, 66, OWN], BF16)
    ox = nc.dram_tensor("ox", [512, OWN], BF16)
    NPC = 4
    def pc_of(m):
        return m // 3, m % 3
    PCN = [3, 3, 3, 2]
    kx_loc = [nc.dram_tensor("kx_loc%d" % p, [512, PCN[p] * CH], BF16) for p in range(NPC)]
    kx_all = [nc.dram_tensor("kx_all%d" % p, [1024, PCN[p] * CH], BF16) for p in range(NPC)]
    vx_loc = [nc.dram_tensor("vx_loc%d" % p, [PCN[p] * CH, 520], BF16) for p in range(NPC)]
    vx_all = [nc.dram_tensor("vx_all%d" % p, [2 * PCN[p] * CH, 520], BF16) for p in range(NPC)]
    ag_loc = [nc.dram_tensor("ag_loc%d" % p, [512, PCN[p] * CH], BF16) for p in range(NPC)]
    ag_all = [nc.dram_tensor("ag_all%d" % p, [1024, PCN[p] * CH], BF16) for p in range(NPC)]
    lf_loc = nc.dram_tensor("lf_loc", [NM * 128, 24], F32)
    lf_all = nc.dram_tensor("lf_all", [2 * NM * 128, 24], F32)
    W1s = [nc.dram_tensor("W1s%d" % l, [2048, 2048], BF16) for l in range(DEPTH)]
    W3s = [nc.dram_tensor("W3s%d" % l, [2048, 2048], BF16) for l in range(DEPTH)]
    W2s = [nc.dram_tensor("W2s%d" % l, [2048, 2048], BF16) for l in range(DEPTH)]
    ux = nc.dram_tensor("ux", [NM, 128, 8 * CH], BF16)
    XS = nc.dram_tensor("XS", [NSLT * 128, D], BF16)
    YS = nc.dram_tensor("YS", [NSLT * 128, D], F32)
    scratch = dict(hbuf=hbuf, qx=qx, ox=ox, lf_all=lf_all)
    for p in range(NPC):
        scratch["kx_all%d" % p] = kx_all[p]
        scratch["vx_all%d" % p] = vx_all[p]
        scratch["ag_all%d" % p] = ag_all[p]
    dbg_out = {}
    for nm in debug.get("dump", []):
        t = scratch[nm]
        dbg_out[nm] = nc.dram_tensor("dbg_" + nm, list(t.shape), t.dtype, kind="ExternalOutput")
    if "moe" in debug.get("dump_sb", []):
        dbg_out["moe"] = nc.dram_tensor("dbg_moe", [128, 512], F32, kind="ExternalOutput")
    if "F" in debug.get("dump_sb", []):
        dbg_out["F"] = nc.dram_tensor("dbg_F", [128, 528], F32, kind="ExternalOutput")

    def sb(name, shape, dt=F32):
        return stack.enter_context(nc.sbuf_tensor("sb_" + name, list(shape), dt))

    cst = sb("cst", [128, NCST])
    ident = sb("ident", [128, 128], BF16)
    vecs = sb("vecs", [128, NV])
    chp = sb("chp", [128, NCP])
    fing = sb("fing", [128, D])
    small = sb("small", [128, 64])
    FTP = sb("FTP", [128, 528 + 528 + 88 + 132])
    AR_WORDS = 44128
    AR = sb("AR", [128, AR_WORDS])

    class Arena:
        def __init__(self, limit=None):
            self.off = 0
            self.limit = AR_WORDS if limit is None else limit

        def check(self):
            assert self.off <= self.limit, (self.off, self.limit)

        def f32(self, n, parts=128):
            a = AR[0:parts, self.off:self.off + n]
            self.off += (n + 7) // 8 * 8
            assert self.off <= self.limit, (self.off, self.limit)
            return a

        def bf16(self, n, parts=128):
            w = (n + 1) // 2
            a = AR[0:parts, self.off:self.off + w].bitcast(BF16)
            self.off += (w + 7) // 8 * 8
            assert self.off <= self.limit, (self.off, self.limit)
            return a[:, 0:n]

    ps = [stack.enter_context(nc.psum_tensor("ps%d" % i, [128, 512], F32)) for i in range(7)]
    psT = stack.enter_context(nc.psum_tensor("psT", [128, 1024], BF16))

    def barrier():
        engs = ["pe", "act", "dve", "pool", "sp"]
        lasts = [i for i in (P.last_compute.get(x) for x in engs) if i is not None]
        asyncs = list(P.asyncs)
        P.asyncs = []
        for E in engs:
            P.add(E, None, (), (), extra=lasts + asyncs)

    rr = [0]

    def nbank(lo=0, hi=5):
        b = lo + rr[0] % (hi - lo)
        rr[0] += 1
        return b

    def psk(b):
        return ("ps", b)

    def dma(q, out, in_, reads, writes):
        return P.add(q, lambda e: e.dma_start(out=out, in_=in_), reads, writes, kind="d")

    def mm(out, lhsT, rhs, start, stop, reads, writes):
        return P.add("pe", lambda e: e.matmul(out, lhsT, rhs, start=start, stop=stop, skip_group_check=True),
                     reads, writes)

    def act(out, in_, func, reads, writes, bias=None, scale=None):
        kw = {}
        if bias is not None:
            kw["bias"] = bias
        if scale is not None:
            kw["scale"] = scale
        return P.add("act", lambda e: e.activation(out, in_, func, **kw), reads, writes)

    def tt(eng, out, in0, in1, op, reads, writes):
        return P.add(eng, lambda e: e.tensor_tensor(out, in0, in1, op), reads, writes)

    def ts(eng, out, in0, s1, s2, op0, op1, reads, writes):
        if op1 is None:
            return P.add(eng, lambda e: e.tensor_scalar(out, in0, s1, None, op0), reads, writes)
        return P.add(eng, lambda e: e.tensor_scalar(out, in0, s1, s2, op0, op1), reads, writes)

    def stt(out, in0, scalar, in1, op0, op1, reads, writes, accum=None):
        return P.add("dve", lambda e: e.scalar_tensor_tensor(out, in0, scalar, in1, op0, op1, accum_out=accum),
                     reads, writes)

    def cp(eng, out, in_, reads, writes):
        if eng == "act":
            return P.add("act", lambda e: e.copy(out, in_), reads, writes)
        return P.add(eng, lambda e: e.tensor_copy(out, in_), reads, writes)

    def memset(eng, ap, val, writes):
        return P.add(eng, lambda e: e.memset(ap, val), (), writes)

    def rsqrt(out, in_, scale, reads, writes):
        act(out, in_, AF.Sqrt, list(reads) + ["epsc"], writes, bias=epsc[0:out.shape[0], :], scale=scale)
        P.add("dve", lambda e: e.reciprocal(out, out), writes, writes)

    def v3(ap, a):
        return ap.rearrange("p (a b) -> p a b", a=a)

    dma("sp", cst[:, :], cst_d[:, :], (), ["cst"])
    dma("sp", fing[:, :], fing_d[:, :], (), ["fing"])
    cp("dve", ident[:, :], cst[:, C_ID:C_ID + 128], ["cst"], ["ident"])
    epsc = small[:, 60:61]
    onec = small[:, 61:62]
    memset("dve", epsc, EPS, ["epsc"])
    memset("dve", onec, 1.0, ["onec"])
    selA = cst[:, C_SEL:C_SEL + 1]
    selB = cst[:, C_SEL + 1:C_SEL + 2]

    def maskap(w, kt, qlo):
        o = C_MASK + (w * 3 + kt) * CH
        return cst[:, o + qlo:o + CH]

    FT = FTP[:, 0:528].rearrange("p (g h) -> p g h", h=8)
    CY = FTP[:, 528:1056].rearrange("p (g h) -> p g h", h=8)
    CS = FTP[:, 1056:1144].rearrange("p (m h) -> p m h", h=8)
    BI = [FTP[:, 1144 + i * 66:1144 + (i + 1) * 66] for i in range(2)]

    def load_h(src, m, htile, key):
        dma("sp", htile, src[m * CH:(m + 1) * CH, :].rearrange("(i p) d -> p i d", p=128),
            (("hsrc", m),), [key])

    def norm_to_uT(htile, hkey, ubf, U, ukey, goff, l):
        for i in range(3):
            ss = small[:, i:i + 1]
            stt(ubf, htile[:, i, :], 1.0, htile[:, i, :], ALU.mult, ALU.mult,
                [hkey], ["ubf", ("ss", i)], accum=ss)
            rs = small[:, 4 + i:5 + i]
            rsqrt(rs, ss, 1.0 / D, [("ss", i)], [("rs", i)])
            stt(ubf, htile[:, i, :], rs, vecs[:, goff:goff + D], ALU.mult, ALU.mult,
                [hkey, ("rs", i), ("vecs", l)], ["ubf"])
            for k in range(8):
                P.add("pe", lambda e, k=k: e.transpose(psT[:, k * 128:(k + 1) * 128], ubf[:, k * 128:(k + 1) * 128],
                                                       ident[:, :]),
                      ["ubf", "ident"], ["psT"])
            cp("act", U[:, :, i * 128:(i + 1) * 128], psT[:, :].rearrange("p (k t) -> p k t", k=8),
               ["psT"], [ukey])

    def norm_dve(htile, hkey, i, ub, goff, l):
        ss = small[:, i:i + 1]
        stt(ub, htile[:, i, :], 1.0, htile[:, i, :], ALU.mult, ALU.mult, [hkey], [("ub3", i), ("ss", i)], accum=ss)
        rs = small[:, 4 + i:5 + i]
        rsqrt(rs, ss, 1.0 / D, [("ss", i)], [("rs", i)])
        stt(ub, htile[:, i, :], rs, vecs[:, goff:goff + D], ALU.mult, ALU.mult,
            [hkey, ("rs", i), ("vecs", l)], [("ub3", i)])

    def norm_pe(ub, i, U, ukey):
        for k in range(8):
            P.add("pe", lambda e, k=k: e.transpose(psT[:, k * 128:(k + 1) * 128], ub[:, k * 128:(k + 1) * 128],
                                                   ident[:, :]),
                  [("ub3", i), "ident"], ["psT"])
        cp("act", U[:, :, i * 128:(i + 1) * 128], psT[:, :].rearrange("p (k t) -> p k t", k=8), ["psT"], [ukey])

    def load_layer_vecs(l):
        dma("sp", vecs[:, :], vecs_d[l, :, :], (), [("vecs", l)])
        dma("sp", chp[:, :], chp_d[l, :, :], (), [("chp", l)])

    def phase1(l, hsrc):
        A = Arena()
        W1 = A.bf16(8 * 2312).rearrange("p (k c) -> p k c", k=8)
        ht = [v3(A.f32(3 * D), 3) for _ in range(2)]
        ubf = A.bf16(D)
        ub3 = [A.bf16(D) for _ in range(3)]
        uT = [v3(A.bf16(8 * CH), 8) for _ in range(2)]
        sg = v3(A.f32(2 * CH), 2)
        ab = v3(A.bf16(2 * CH), 2)
        gb = v3(A.bf16(2 * CH), 2)
        kb = v3(A.bf16(4 * CH), 4)
        qb = v3(A.bf16(4 * CH), 4)
        vts = [A.bf16(520) for _ in range(2)]
        lts = [A.f32(24) for _ in range(2)]
        for (c0, c1) in ((0, 768), (768, 1280), (1280, 1792), (1792, 2312)):
            dma("pool", W1[:, :, c0:c1], w_in[l, :, c0:c1].rearrange("(k p) c -> p k c", p=128),
                (), [("W1", c0)])
        wkeys = [("W1", 0), ("W1", 768), ("W1", 1280), ("W1", 1792)]
        for i in range(2):
            memset("pool", vts[i], 1.0, [("vtile", i)])
        stg = [(A.bf16(2048), A.bf16(2048), A.bf16(2048)) for _ in range(4)]

        def prep_load(e):
            a1, a3, a2 = stg[e % 4]
            dma("pool", v3(a1, 8), ew1[l, e, :, :].rearrange("(k p) c -> p k c", p=128), (), [("pw1", e % 4)])
            dma("pool", v3(a3, 8), ew3[l, e, :, :].rearrange("(k p) c -> p k c", p=128), (), [("pw3", e % 4)])
            dma("pool", v3(a2, 2), ew2[l, e, :, :].rearrange("(k p) c -> p k c", p=128), (), [("pw2", e % 4)])

        def prep_store(e):
            a1, a3, a2 = stg[e % 4]
            dma("sp", W1s[l][e * 128:(e + 1) * 128, :], a1, [("pw1", e % 4)], [("W1s", e)])
            dma("sp", W3s[l][e * 128:(e + 1) * 128, :], a3, [("pw3", e % 4)], [("W3s", e)])
            dma("sp", W2s[l][e * 128:(e + 1) * 128, :], a2, [("pw2", e % 4)], [("W2s", e)])
        load_h(hsrc, 0, ht[0], ("ht", 0))
        for m in range(NM):
            hb = m % 2
            if m + 1 < NM:
                load_h(hsrc, m + 1, ht[1 - hb], ("ht", 1 - hb))
            U = uT[hb]
            ukey = ("uT", hb)
            if m < 8:
                prep_load(2 * m)
                prep_load(2 * m + 1)
            if 1 <= m < 9:
                prep_store(2 * m - 2)
                prep_store(2 * m - 1)
            if m == 0:
                for i in range(3):
                    norm_dve(ht[hb], ("ht", hb), i, ub3[i], V_G1, l)
                    norm_pe(ub3[i], i, U, ukey)
            dma("sp", ux[m, :, :], U.rearrange("p k t -> p (k t)"), [ukey], [("ux", m)])
            nxt = m + 1 < NM

            def early_norm(i):
                if nxt:
                    norm_dve(ht[1 - hb], ("ht", 1 - hb), i, ub3[i], V_G1, l)
            osl = slice(m * CH, (m + 1) * CH)
            pp, pm = pc_of(m)
            psl = slice(pm * CH, (pm + 1) * CH)
            p1stop = debug.get("p1stop", 99)
            if p1stop < 1:
                continue

            def fm_group(c0, M):
                b = nbank()
                for k in range(8):
                    mm(ps[b][0:M, 0:CH], W1[:, k, c0:c0 + M], U[:, k, :], k == 0, k == 7,
                       wkeys + [ukey], [psk(b)])
                return b
            for cc in range(2):
                b = fm_group(cc * 128, 128)
                cp("act", ab[:, cc, :], ps[b][:, 0:CH], [psk(b)], [("ab", cc)])
            dma("sp", ag_loc[pp][0:256, psl].rearrange("(c p) t -> p c t", p=128), ab,
                [("ab", 0), ("ab", 1)], [("ag_loc", m, 0)])
            early_norm(0)
            for cc in range(2):
                b2 = fm_group(512 + cc * 128, 128)
                act(sg[:, cc, :], ps[b2][:, 0:CH], AF.Sigmoid, [psk(b2)], [("sg", cc)])
                b1 = fm_group(256 + cc * 128, 128)
                tt("dve", gb[:, cc, :], ps[b1][:, 0:CH], sg[:, cc, :], ALU.mult, [psk(b1), ("sg", cc)], [("gb", cc)])
            dma("sp", ag_loc[pp][256:512, psl].rearrange("(c p) t -> p c t", p=128), gb,
                [("gb", 0), ("gb", 1)], [("ag_loc", m, 1)])
            early_norm(1)
            for kp in range(4):
                b = fm_group(768 + kp * 128, 128)
                P.add("act", lambda e, b=b, kp=kp: e.mul(qb[:, kp, :], ps[b][:, 0:CH], 0.125),
                      [psk(b)], [("qb", kp)])
                b = fm_group(1280 + kp * 128, 128)
                cp("dve", kb[:, kp, :], ps[b][:, 0:CH], [psk(b)], [("kb", kp)])
            for h in range(8):
                dma("sp", qx[h, 0:64, osl], qb[(h % 2) * 64:(h % 2) * 64 + 64, h // 2, :],
                    [("qb", h // 2)], [("qx", m, h)])
            dma("sp", kx_loc[pp][:, psl].rearrange("(k p) t -> p k t", p=128), kb[:, :, :],
                [("kb", kp) for kp in range(4)], [("kx_loc", m)])
            early_norm(2)
            for i in range(3):
                vb = (m * 3 + i) % 2
                vt = vts[vb].rearrange("p (h d) -> p h d", h=8)
                b = nbank()
                for k in range(8):
                    mm(ps[b][:, 0:512], U[:, k, i * 128:(i + 1) * 128], W1[:, k, 1792:2304], k == 0, k == 7,
                       wkeys + [ukey], [psk(b)])
                cp("act", vt[:, :, 0:64], ps[b][:, 0:512].rearrange("p (h d) -> p h d", h=8), [psk(b)],
                   [("vtile", vb)])
                r0 = pm * CH + i * 128
                dma("sp", vx_loc[pp][r0:r0 + 128, :], vts[vb], [("vtile", vb)], [("vx_loc", m, i)])
                if p1stop < 5:
                    continue
                b = nbank()
                for k in range(8):
                    mm(ps[b][:, 0:8], U[:, k, i * 128:(i + 1) * 128], W1[:, k, 2304:2312], k == 0, k == 7,
                       wkeys + [ukey], [psk(b)])
                lt = lts[m % 2][:, i * 8:(i + 1) * 8]
                vb = (m % 2, i)
                tt("dve", lt, ps[b][:, 0:8], vecs[:, V_BF:V_BF + 8], ALU.add, [psk(b), ("vecs", l)], [("lt", vb)])
                if p1stop < 6:
                    continue
                if not debug.get("noact"):
                    act(lt, lt, AF.Exp, [("lt", vb)], [("lt", vb)], scale=-1.0)
                    act(lt, lt, AF.Ln, [("lt", vb), "onec"], [("lt", vb)], bias=onec)
                    ts("dve", lt, lt, -1.0, None, ALU.mult, None, [("lt", vb)], [("lt", vb)])
            dma("sp", lf_loc[m * 128:(m + 1) * 128, :], lts[m % 2], [("lt", (m % 2, i)) for i in range(3)],
                [("lf_loc", m)])
            if nxt:
                for i in range(3):
                    norm_pe(ub3[i], i, uT[1 - hb], ("uT", 1 - hb))
            if m == NM - 1 or (m + 1) % 3 == 0:
                exchange_piece(m // 3)

    groups = [[0, 1], [2, 3], [4, 5], [6, 7]]

    def cc(src, dst, rkeys, wkey):
        P.add("pool", lambda e: e.collective_compute("AllGather", ALU.bypass, replica_groups=groups,
                                                     ins=[src.ap().opt()], outs=[dst.ap().opt()]),
              rkeys, [wkey], kind="cc")

    def exchange_piece(p):
        ms = [m for m in range(NM) if m // 3 == p]
        cc(kx_loc[p], kx_all[p], [("kx_loc", m) for m in ms], ("kx_all", p))
        cc(vx_loc[p], vx_all[p], [("vx_loc", m, i) for m in ms for i in range(3)], ("vx_all", p))
        cc(ag_loc[p], ag_all[p], [("ag_loc", m, j) for m in ms for j in range(2)], ("ag_all", p))

    def exchange():
        cc(lf_loc, lf_all, [("lf_loc", m) for m in range(NM)], "lf_all")

    def phase2a():
        A = Arena()
        LFf = A.f32(528)
        LF = LFf.rearrange("p (m r i h) -> p m r i h", m=NM, r=2, i=3)
        TT = A.f32(528)
        INC = A.f32(528)
        ONE = A.f32(66)
        DSf = A.f32(264)
        DS = DSf.rearrange("p (j h) -> p j h", h=8)
        DH = A.bf16(264)
        DL = A.bf16(264)
        TRB = v3(A.bf16(3 * 128), 3)
        for r in range(2):
            dma("sp", LFf.rearrange("p (m r x) -> p m r x", m=NM, r=2)[:, :, r, :],
                lf_all[r * NM * 128:(r + 1) * NM * 128, :].rearrange("(m p) x -> p m x", p=128),
                ["lf_all"], [("LF", r)])
        lk = [("LF", 0), ("LF", 1)]
        memset("dve", ONE, 1.0, ["ONE"])
        FTf = FT.rearrange("p g h -> p (g h)")
        CYf = CY.rearrange("p g h -> p (g h)")
        for half in range(2):
            cs = slice(half * 264, (half + 1) * 264)
            mm(ps[half][:, 0:264], cst[:, C_U:C_U + 128], LFf[:, cs], True, True, lk + ["cst"], [psk(half)])
            mm(ps[2 + half][:, 0:264], cst[:, C_ONES:C_ONES + 128], LFf[:, cs], True, True, lk + ["cst"],
               [psk(2 + half)])
            cp("act", FTf[:, cs], ps[half][:, 0:264], [psk(half)], [("FTw", half)])
            cp("dve", TT[:, cs], ps[2 + half][:, 0:264], [psk(2 + half)], [("TT", half)])
        TT3 = TT.rearrange("p (g h) -> p g h", h=8)
        INC3 = INC.rearrange("p (g h) -> p g h", h=8)
        for h in range(8):
            P.add("dve", lambda e, h=h: e.tensor_tensor_scan(INC3[:, :, h], ONE, TT3[:, :, h], 0.0, ALU.mult, ALU.add),
                  [("TT", 0), ("TT", 1), "ONE"], [("INC", h)])
        ik = [("INC", h) for h in range(8)]
        tt("dve", CYf, INC, TT, ALU.subtract, ik + [("TT", 0), ("TT", 1)], ["CY"])
        tt("dve", FTf, FTf, CYf, ALU.add, [("FTw", 0), ("FTw", 1), "CY"], ["FT"])
        CY4 = CY.rearrange("p (m r i) h -> p m r i h", r=2, i=3)
        FT4 = FT.rearrange("p (m r i) h -> p m r i h", r=2, i=3)
        ts("dve", CS, CY4[:, :, 0, 0, :], selA, None, ALU.mult, None, ["CY", "cst"], ["CS"])
        stt(CS, CY4[:, :, 1, 0, :], selB, CS, ALU.mult, ALU.add, ["CY", "cst", "CS"], ["CS"])
        DS4 = DS.rearrange("p (m i) h -> p m i h", i=3)
        for i in range(3):
            ts("dve", DS4[:, :, i, :], FT4[:, :, 0, i, :], selA, None, ALU.mult, None, ["FT", "cst"], [("DS", i)])
            stt(DS4[:, :, i, :], FT4[:, :, 1, i, :], selB, DS4[:, :, i, :], ALU.mult, ALU.add,
                ["FT", "cst", ("DS", i)], [("DS", i)])
            tt("dve", DS4[:, :, i, :], DS4[:, :, i, :], CS, ALU.subtract, [("DS", i), "CS"], [("DS", i)])
        dk = [("DS", i) for i in range(3)]
        cp("dve", DH, DSf, dk, ["DH"])
        tt("dve", DSf, DSf, DH, ALU.subtract, dk + ["DH"], ["DS2"] + dk)
        cp("dve", DL, DSf, ["DS2"], ["DL"])
        for which, src in ((0, DH), (1, DL)):
            for a, (c0, n) in enumerate(((0, 128), (128, 128), (256, 8))):
                P.add("pe", lambda e, a=a, c0=c0, n=n, src=src: e.transpose(psT[0:n, a * 128:(a + 1) * 128],
                                                                             src[:, c0:c0 + n], ident[:, :]),
                      ["DH", "DL", "ident"], ["psT"])
            cp("act", TRB[:, :, :], psT[:, 0:384].rearrange("p (a t) -> p a t", a=3), ["psT"], ["TRB"])
            for a, nj in ((0, 16), (1, 16), (2, 1)):
                for jj in range(nj):
                    j = a * 16 + jj
                    dma("sp", qx[:, 64 + which, j * 128:(j + 1) * 128],
                        TRB[jj * 8:(jj + 1) * 8, a, :], ["TRB"], [("qxaug", which, j)])
        if "F" in dbg_out:
            dma("sp", dbg_out["F"][:, :], FTf, ["FT"], ["dbgF"])

    def phase2b(l):
        A = Arena(limit=P3W_BASE)
        p3a_weight_loads(l)
        QH = [A.bf16(OWN, parts=66) for _ in range(2)]
        KT = [A.bf16(NG * CH, parts=66).rearrange("p (g t) -> p g t", t=CH) for _ in range(2)]
        VT = [A.bf16(NGT * 65).rearrange("p (m r i d) -> p m r i d", r=2, i=3, d=65) for _ in range(2)]
        PT = [A.bf16(CH) for _ in range(10)]
        MT = [A.f32(CH) for _ in range(2)]
        OS2 = [A.f32(CH) for _ in range(2)]
        RC2 = [A.f32(CH) for _ in range(2)]
        OB = [A.bf16(CH) for _ in range(2)]
        BI3 = [A.f32(72) for _ in range(3)]
        qk = [("qx", m, h_) for m in range(NM) for h_ in range(8)] + \
            [("qxaug", w, j) for w in range(2) for j in range(NOWNT)]
        for i in range(2):
            memset("pool", KT[i][64:66, :, :], 1.0, [("KTones", i)])

        def load_kv(h, hb):
            dma("act", QH[hb][0:66, :], qx[h, :, :], qk, [("QH", hb)])
            for r in range(2):
                for p in range(NPC):
                    n = PCN[p]
                    dma("act", KT[hb][0:64, :, :].rearrange("p (m r) t -> p m r t", r=2)[:, 3 * p:3 * p + n, r, :],
                        kx_all[p][r * 512 + h * 64:r * 512 + (h + 1) * 64, :].rearrange("p (m t) -> p m t", t=CH),
                        [("kx_all", p)], [("KT", hb, r, p)])
                    for i in range(3):
                        dma("act", VT[hb][:, 3 * p:3 * p + n, r, i, :],
                            vx_all[p][r * n * CH:(r + 1) * n * CH, h * 65:(h + 1) * 65].rearrange(
                                "(m i p) d -> p m i d", i=3, p=128)[:, :, i, :],
                            [("vx_all", p)], [("VT", hb, r, p, i)])
        load_kv(0, 0)
        LAG = 4
        items = []
        for h in range(8):
            for m in range(NM):
                for gt in range(6 * m + 6):
                    items.append((h, m, gt))
        state = {}
        pend = {}
        NPT = len(PT)

        def s_stage(idx):
            h, m, gt = items[idx]
            hb = h % 2
            it = h * NM + m
            bi = it % 3
            if gt == 0:
                ts("dve", BI3[bi][:, 0:66], FT[:, :, h], -1.0, CS[:, m, h:h + 1], ALU.mult, ALU.add, ["FT", "CS"],
                   [("BI", bi)])
            kvk = [("KTones", hb), ("QH", hb)] + [("KT", hb, r, p) for r in range(2) for p in range(NPC)]
            G, kt = gt // 3, gt % 3
            band = G - 2 * m
            qlo = kt * 128 if band == 1 else 0
            ncol = CH - qlo
            q0 = m * CH
            b = nbank()
            mm(ps[b][:, 0:ncol], KT[hb][0:66, G, kt * 128:(kt + 1) * 128], QH[hb][0:66, q0 + qlo:q0 + CH],
               True, True, kvk, [psk(b)])
            pt = PT[idx % NPT]
            pk = ("PT", idx % NPT)
            if band >= 0:
                mt = MT[idx % 2]
                mk = ("MT", idx % 2)
                tt("dve", mt[:, 0:ncol], ps[b][:, 0:ncol], maskap(band, kt, qlo), ALU.add, [psk(b), "cst"], [mk])
                act(pt[:, 0:ncol], mt[:, 0:ncol], AF.Exp, [mk, ("BI", bi)], [pk], bias=BI3[bi][:, gt:gt + 1])
            else:
                act(pt[:, 0:ncol], ps[b][:, 0:ncol], AF.Exp, [psk(b), ("BI", bi)], [pk],
                    bias=BI3[bi][:, gt:gt + 1])
            state[idx] = (pt, pk, qlo, ncol)

        def pv_stage(idx, step):
            h, m, gt = items[idx]
            hb = h % 2
            it = h * NM + m
            ob = 5 + it % 2
            ngt = 6 * m + 6
            pt, pk, qlo, ncol = state.pop(idx)
            if gt == 0 and m == 0 and h + 1 < 8:
                load_kv(h + 1, 1 - hb)
            vk = [("VT", hb, r, p, i) for r in range(2) for p in range(NPC) for i in range(3)]
            Vt = VT[hb].rearrange("p m r i d -> p (m r i) d")
            mm(ps[ob][0:65, qlo:CH], Vt[:, gt, 0:65], pt[:, 0:ncol], gt == 0, gt == ngt - 1, vk + [pk], [psk(ob)])
            if gt == ngt - 1:
                ri = it % 2
                P.add("dve", lambda e: e.reciprocal(RC2[ri][64:65, :], ps[ob][64:65, 0:CH]), [psk(ob)], [("RC", ri)])
                cp("act", OS2[ri][0:64, :], ps[ob][0:64, 0:CH], [psk(ob)], [("OS", ri)])
                pend.setdefault(step + 2, []).append((h, m, ri))

        def norm2(h, m, ri):
            q0 = m * CH
            b = nbank()
            mm(ps[b][0:64, 0:CH], cst[64:65, C_ONES:C_ONES + 64], RC2[ri][64:65, :], True, True, [("RC", ri), "cst"],
               [psk(b)])
            obt = OB[ri]
            tt("dve", obt[0:64, :], OS2[ri][0:64, :], ps[b][0:64, 0:CH], ALU.mult, [("OS", ri), psk(b)], [("OB", ri)])
            dma("sp", ox[h * 64:(h + 1) * 64, q0:q0 + CH], obt[0:64, :], [("OB", ri)], [("ox", h, m)])

        nit = len(items)
        for step in range(nit + LAG + 3):
            if step < nit:
                s_stage(step)
            if 0 <= step - LAG < nit:
                pv_stage(step - LAG, step)
            for a in pend.pop(step, []):
                norm2(*a)
        assert not pend and not state

    P3W_WORDS = (8 * 3072 + 2 * 1024 + 2 * 1024 + 4 * 1024 + 8 * 1024) // 2
    P3W_BASE = AR_WORDS - P3W_WORDS

    def p3a_weight_views():
        o = [P3W_BASE]

        def take(n):
            a = AR[:, o[0]:o[0] + n // 2].bitcast(BF16)
            o[0] += n // 2
            return a
        return (v3(take(8 * 3072), 8), v3(take(2 * 1024), 2), v3(take(2 * 1024), 2), v3(take(4 * 1024), 4),
                v3(take(8 * 1024), 8))

    def p3a_weight_loads(l):
        WG, WOA, WOB, WOC, WO = p3a_weight_views()
        for g3 in range(3):
            dma("pool", WG[:, :, g3 * 1024:(g3 + 1) * 1024],
                w_in[l, :, 2312 + g3 * 1024:2312 + (g3 + 1) * 1024].rearrange("(k p) c -> p k c", p=128),
                (), [("WG", g3)])
        dma("pool", WOA, w_oa[l, :, :].rearrange("(k p) c -> p k c", p=128), (), ["WOA"])
        dma("pool", WOB, w_ob[l, :, :].rearrange("(k p) c -> p k c", p=128), (), ["WOB"])
        dma("pool", WOC, w_oc[l, :, :].rearrange("(k p) c -> p k c", p=128), (), ["WOC"])
        dma("pool", WO, w_o[l, :, :].rearrange("(k p) c -> p k c", p=128), (), ["WO"])

    def phase3a(l, hsrc):
        A = Arena(limit=P3W_BASE)
        WG, WOA, WOB, WOC, WO = p3a_weight_views()
        BD = v3(A.bf16(256), 2)
        DG = A.bf16(62 * 128).rearrange("p (j d) -> p j d", d=128)
        htile = v3(A.f32(3 * D), 3)
        U2 = [v3(A.bf16(8 * CH), 8) for _ in range(2)]
        XA = v3(A.f32(800), 2)
        S2 = v3(A.f32(800), 2)
        S4 = v3(A.f32(800), 2)
        YC = v3(A.f32(768), 2)
        YQ = v3(A.f32(768), 2)
        MEAN = A.f32(CH)
        RSTD = A.f32(CH)
        T1 = A.f32(CH)
        T2 = A.f32(CH)
        T3 = A.f32(CH)
        HA = v3(A.f32(64), 2)
        SGB = [A.f32(CH) for _ in range(3)]
        XG = v3(A.bf16(832), 2)
        DF = v3(A.bf16(768), 2)
        YP = v3(A.bf16(768), 2)
        YB = v3(A.bf16(768), 2)
        HB0 = v3(A.bf16(128), 2)
        HB1 = v3(A.bf16(128), 2)
        MG = v3(A.bf16(8 * CH), 8)
        AB = v3(A.bf16(2 * CH), 2)
        OC = v3(A.bf16(4 * CH), 4)
        pbs = small[:, 40:42]
        memset("dve", BD, 0.0, ["BD0"])
        for g in range(4):
            r0 = (g % 2) * 64
            dma("pool", BD[r0:r0 + 64, g // 2, r0:r0 + 64], pool_w[l, g, :, :], ["BD0"], [("BD", g)])
        bdk = [("BD", g) for g in range(4)]
        for cc in range(2):
            for j in range(31):
                ts("dve", DG[:, cc * 31 + j, :], ident[:, :], chp[:, CP_CW + cc * 31 + j:CP_CW + cc * 31 + j + 1],
                   None, ALU.mult, None, ["ident", ("chp", l)], [("DG", cc)])
        tt("dve", pbs, chp[:, CP_PB:CP_PB + 2], chp[:, CP_PS:CP_PS + 2], ALU.mult, [("chp", l)], ["pbs"])
        wgk = [("WG", g3) for g3 in range(3)]
        YP2 = [YP, v3(A.bf16(768), 2)]
        YB2 = [YB, v3(A.bf16(768), 2)]
        OC2 = [OC, v3(A.bf16(4 * CH), 4)]

        def prep(m, s_):
            YP_, YB_, OC_ = YP2[s_], YB2[s_], OC2[s_]
            osl = slice(m * CH, (m + 1) * CH)
            pp, pm = pc_of(m)
            psl = slice(pm * CH, (pm + 1) * CH)
            dma("sp", XG[:, :, 32:416], ag_loc[pp][256:512, psl].rearrange("(c p) t -> p c t", p=128),
                [("ag_loc", m, 1)], ["XGo"])
            dma("sp", AB, ag_loc[pp][0:256, psl].rearrange("(c p) t -> p c t", p=128),
                [("ag_loc", m, 0)], ["ABo"])
            dma("sp", OC_, ox[:, osl].rearrange("(k p) t -> p k t", p=128),
                [("ox", h, m) for h in range(8)], [("OC", s_)])
            if m > 0:
                qp, qm = pc_of(m - 1)
                e0 = (qm + 1) * CH
                dma("sp", HB0[:, :, 0:32],
                    ag_all[qp][768:1024, e0 - 32:e0].rearrange("(c p) t -> p c t", p=128),
                    [("ag_all", qp)], ["HB0g"])
                dma("sp", HB0[:, :, 32:48],
                    ag_all[qp][512:768, e0 - 16:e0].rearrange("(c p) t -> p c t", p=128),
                    [("ag_all", qp)], ["HB0a"])
            else:
                memset("pool", HB0, 0.0, ["HB0g", "HB0a"])
            e1 = (pm + 1) * CH
            dma("sp", HB1[:, :, 0:32],
                ag_all[pp][256:512, e1 - 32:e1].rearrange("(c p) t -> p c t", p=128),
                [("ag_all", pp)], ["HB1g"])
            dma("sp", HB1[:, :, 32:48],
                ag_all[pp][0:256, e1 - 16:e1].rearrange("(c p) t -> p c t", p=128),
                [("ag_all", pp)], ["HB1a"])
            hk = ["HB0g", "HB0a", "HB1g", "HB1a"]
            ts("dve", HA[:, :, 0:32], HB0[:, :, 0:32], selA, None, ALU.mult, None, hk + ["cst"], ["HAg"])
            stt(XG[:, :, 0:32], HB1[:, :, 0:32], selB, HA[:, :, 0:32], ALU.mult, ALU.add, hk + ["cst", "HAg"], ["XGh"])
            ts("dve", HA[:, :, 0:16], HB0[:, :, 32:48], selA, None, ALU.mult, None, hk + ["cst", "XGh"], ["HAa"])
            stt(XA[:, :, 0:16], HB1[:, :, 32:48], selB, HA[:, :, 0:16], ALU.mult, ALU.add, hk + ["cst", "HAa"], ["XAh"])
            cp("dve", XA[:, :, 16:400], AB, ["ABo"], ["XAo"])
            xak = ["XAh", "XAo"]
            tt("dve", S2[:, :, 1:400], XA[:, :, 1:400], XA[:, :, 0:399], ALU.add, xak, ["S2"])
            tt("dve", S4[:, :, 3:400], S2[:, :, 3:400], S2[:, :, 1:398], ALU.add, ["S2"], ["S4"])
            tt("dve", S2[:, 1, 7:400], S4[:, 1, 7:400], S4[:, 1, 3:396], ALU.add, ["S4", "S2"], ["S8", "S2"])
            tt("dve", S4[64:128, 1, 15:400], S2[64:128, 1, 15:400], S2[64:128, 1, 7:392], ALU.add, ["S8", "S4"],
               ["S16", "S4"])
            ico = C_INVC + (0 if m == 0 else 768)
            IC = cst[:, ico:ico + 768].rearrange("p (c t) -> p c t", c=2)
            wk = ["S2", "S4", "S8", "S16", "cst"]
            PT1 = YC[:, 0, :]
            for cc in range(2):
                tt("dve", PT1[0:64, :], S2[0:64, cc, 16:400], IC[0:64, cc, :], ALU.mult, wk, [("YC", 0)])
                tt("dve", PT1[64:128, :], S4[64:128, cc, 16:400], IC[64:128, cc, :], ALU.mult, wk + [("YC", 0)],
                   [("YC", 0)])
                tt("dve", DF[:, cc, :], PT1, XA[:, cc, 16:400], ALU.subtract, [("YC", 0)] + xak, [("DF", cc)])
            yield
            for cc in range(2):
                b = nbank(0, 7)
                mm(ps[b][:, 0:CH], BD[:, cc, :], DF[:, cc, :], True, True, bdk + [("DF", cc)], [psk(b)])
                act(YP_[:, cc, :], ps[b][:, 0:CH], AF.Identity, [psk(b), ("chp", l), "pbs"], [("YP", s_, cc)],
                    bias=pbs[:, cc:cc + 1], scale=chp[:, CP_PS + cc:CP_PS + cc + 1])
            for cc in range(2):
                b = nbank(0, 7)
                for j in range(31):
                    mm(ps[b][:, 0:CH], DG[:, cc * 31 + j, :], XG[:, cc, 2 + j:2 + j + CH], j == 0, j == 30,
                       [("DG", 0), ("DG", 1), "XGo", "XGh"], [psk(b)])
                act(YC[:, cc, :], ps[b][:, 0:CH], AF.Identity, [psk(b), ("chp", l)], [("YC", cc)],
                    bias=chp[:, CP_CB + cc:CP_CB + cc + 1])
                act(YQ[:, cc, :], YC[:, cc, :], AF.Square, [("YC", cc)], [("YQ", cc)])
            bm = nbank(0, 7)
            bq = nbank(0, 7)
            for cc in range(2):
                mm(ps[bm][:, 0:CH], cst[:, C_ODIV:C_ODIV + 128], YC[:, cc, :], cc == 0, cc == 1, ["cst", ("YC", cc)],
                   [psk(bm)])
            for cc in range(2):
                mm(ps[bq][:, 0:CH], cst[:, C_ODIV:C_ODIV + 128], YQ[:, cc, :], cc == 0, cc == 1, ["cst", ("YQ", cc)],
                   [psk(bq)])
            PT2 = YQ[:, 0, :]
            PT3 = YQ[:, 1, :]
            cp("act", MEAN, ps[bm][:, 0:CH], [psk(bm)], ["MEAN"])
            tt("dve", PT2, MEAN, MEAN, ALU.mult, ["MEAN"], [("YQ", 0)])
            tt("dve", RSTD, ps[bq][:, 0:CH], PT2, ALU.subtract, [psk(bq), ("YQ", 0)], ["RSTD"])
            rsqrt(RSTD, RSTD, 1.0, ["RSTD"], ["RSTD"])
            for cc in range(2):
                tt("dve", PT3, YC[:, cc, :], MEAN, ALU.subtract, [("YC", cc), "MEAN"], [("YQ", 1)])
                tt("dve", PT3, PT3, RSTD, ALU.mult, [("YQ", 1), "RSTD"], [("YQ", 1)])
                act(YB_[:, cc, :], PT3, AF.Silu, [("YQ", 1), ("chp", l)], [("YB", s_, cc)],
                    bias=chp[:, CP_LB + cc:CP_LB + cc + 1], scale=chp[:, CP_LG + cc:CP_LG + cc + 1])

        for _ in prep(0, 0):
            pass
        for m in range(NM):
            s_ = m % 2
            YP_, YB_, OC_ = YP2[s_], YB2[s_], OC2[s_]
            if m == 0:
                dma("sp", U2[0].rearrange("p k t -> p (k t)"), ux[0, :, :], [("ux", 0)], [("U3", 0)])
            if m + 1 < NM:
                dma("sp", U2[(m + 1) % 2].rearrange("p k t -> p (k t)"), ux[m + 1, :, :], [("ux", m + 1)],
                    [("U3", (m + 1) % 2)])
            load_h(hsrc, m, htile, "ht3")
            ukey = ("U3", m % 2)
            U = U2[m % 2]
            pg = prep(m + 1, 1 - s_) if m + 1 < NM else iter(())
            for f in range(8):
                if f == 1 or f == 4:
                    next(pg, None)
                for br in range(3):
                    b = nbank(0, 7)
                    c0 = br * 1024 + f * 128
                    for k in range(8):
                        mm(ps[b][:, 0:CH], WG[:, k, c0:c0 + 128], U[:, k, :], k == 0, k == 7, wgk + [ukey], [psk(b)])
                    act(SGB[br], ps[b][:, 0:CH], AF.Sigmoid, [psk(b)], [("SGB", br)])
                fs = slice(f * 128, (f + 1) * 128)
                ba = nbank(0, 7)
                for cc in range(2):
                    mm(ps[ba][:, 0:CH], WOA[:, cc, fs], YP_[:, cc, :], cc == 0, cc == 1,
                       ["WOA", ("YP", s_, 0), ("YP", s_, 1)], [psk(ba)])
                tt("dve", T1, ps[ba][:, 0:CH], SGB[0], ALU.mult, [psk(ba), ("SGB", 0)], ["T1"])
                bb = nbank(0, 7)
                for cc in range(2):
                    mm(ps[bb][:, 0:CH], WOB[:, cc, fs], YB_[:, cc, :], cc == 0, cc == 1,
                       ["WOB", ("YB", s_, 0), ("YB", s_, 1)], [psk(bb)])
                tt("dve", T2, ps[bb][:, 0:CH], SGB[1], ALU.mult, [psk(bb), ("SGB", 1)], ["T2"])
                bc = nbank(0, 7)
                for k in range(4):
                    mm(ps[bc][:, 0:CH], WOC[:, k, fs], OC_[:, k, :], k == 0, k == 3, ["WOC", ("OC", s_)], [psk(bc)])
                tt("dve", T3, ps[bc][:, 0:CH], SGB[2], ALU.mult, [psk(bc), ("SGB", 2)], ["T3"])
                tt("pool", T1, T1, T2, ALU.add, ["T1", "T2"], ["T1"])
                tt("pool", MG[:, f, :], T1, T3, ALU.add, ["T1", "T3"], [("MG", f)])
            for _ in pg:
                pass
            mgk = [("MG", f) for f in range(8)]
            for i in range(3):
                for half in range(2):
                    b = nbank(0, 7)
                    for k in range(8):
                        mm(ps[b][:, 0:512], MG[:, k, i * 128:(i + 1) * 128], WO[:, k, half * 512:(half + 1) * 512],
                           k == 0, k == 7, mgk + ["WO"], [psk(b)])
                    tt("dve", htile[:, i, half * 512:(half + 1) * 512], htile[:, i, half * 512:(half + 1) * 512],
                       ps[b][:, 0:512], ALU.add, ["ht3", psk(b)], ["ht3"])
            dma("sp", hbuf[m * CH:(m + 1) * CH, :].rearrange("(i p) d -> p i d", p=128), htile,
                ["ht3"], [("hsrc", m)])

    def phase3b(l, last):
        SCS = [list(range(0, 6)), list(range(6, NM))]
        A = Arena()
        ACCT = A.f32(18 * D).rearrange("p (t d) -> p t d", d=D)
        VTT = A.bf16(6 * 8 * CH).rearrange("p (s k t) -> p s k t", s=6, k=8)
        WS = []
        for s in range(2):
            WS.append((v3(A.bf16(2048), 8), v3(A.bf16(2048), 8), v3(A.bf16(2048), 2)))
        RW = v3(A.bf16(160), 8)
        ubf = A.bf16(D)
        LG = A.f32(20)
        GM = A.f32(4)
        OH = A.f32(4)
        EX = A.f32(4)
        ES = v3(A.f32(16), 4)
        EL = A.f32(4)
        M1 = A.f32(4)
        M2 = A.f32(4)
        E2 = A.f32(4)
        SC1 = A.f32(8)
        WGp = A.f32(4)
        CMB = A.f32(18 * 16).rearrange("p (t e) -> p t e", e=16)
        S1 = [A.f32(CH) for _ in range(2)]
        HE = A.bf16(4 * CH).rearrange("p (s c t) -> p s c t", s=2, c=2)
        OT = A.f32(D)
        dma("pool", RW, rw_d[l, :, :].rearrange("(k p) c -> p k c", p=128), (), ["RW"])
        ecount = 0
        for sc in SCS:
            for si, m in enumerate(sc):
                for i in range(3):
                    t = si * 3 + i
                    dma("sp", ACCT[:, t, :], hbuf[m * CH + i * 128:m * CH + (i + 1) * 128, :], [("hsrc", m)],
                        [("ACC", t)])
            for si, m in enumerate(sc):
                for i in range(3):
                    t = si * 3 + i
                    ss = small[:, 48:49]
                    rs = small[:, 49:50]
                    stt(ubf, ACCT[:, t, :], 1.0, ACCT[:, t, :], ALU.mult, ALU.mult, [("ACC", t)], ["ubf", "ss2"],
                        accum=ss)
                    rsqrt(rs, ss, 1.0 / D, ["ss2"], ["rs2"])
                    stt(ubf, ACCT[:, t, :], rs, vecs[:, V_G2:V_G2 + D], ALU.mult, ALU.mult,
                        [("ACC", t), "rs2", ("vecs", l)], ["ubf"])
                    for k in range(8):
                        P.add("pe", lambda e, k=k: e.transpose(psT[:, k * 128:(k + 1) * 128],
                                                               ubf[:, k * 128:(k + 1) * 128], ident[:, :]),
                              ["ubf", "ident"], ["psT"])
                    cp("act", VTT[:, si, :, i * 128:(i + 1) * 128], psT[:, :].rearrange("p (k t) -> p k t", k=8),
                       ["psT"], [("VTT", si)])
                    b = nbank(0, 7)
                    for k in range(8):
                        mm(ps[b][:, 0:20], VTT[:, si, k, i * 128:(i + 1) * 128], RW[:, k, :], k == 0, k == 7,
                           [("VTT", si), "RW"], [psk(b)])
                    tt("dve", LG, ps[b][:, 0:20], vecs[:, V_RB:V_RB + 20], ALU.add, [psk(b), ("vecs", l)], ["LG"])
                    AXX = mybir.AxisListType.X
                    P.add("dve", lambda e: e.reduce_max(GM[:, 0:1], LG[:, 0:4], AXX), ["LG"], ["GM"])
                    ts("dve", OH, LG[:, 0:4], GM[:, 0:1], None, ALU.is_equal, None, ["LG", "GM"], ["OH"])
                    ts("dve", EX, LG[:, 0:4], GM[:, 0:1], None, ALU.subtract, None, ["LG", "GM"], ["EX"])
                    act(EX, EX, AF.Exp, ["EX"], ["EX"])
                    P.add("dve", lambda e: e.reduce_sum(GM[:, 1:2], EX, AXX), ["EX", "GM"], ["GS"])
                    P.add("dve", lambda e: e.reciprocal(GM[:, 2:3], GM[:, 1:2]), ["GS"], ["GW"])
                    LE = LG[:, 4:20].rearrange("p (g e) -> p g e", g=4)
                    for g in range(4):
                        ts("dve", ES[:, g, :], LE[:, g, :], OH[:, g:g + 1], None, ALU.mult, None, ["LG", "OH"],
                           [("ES", g)])
                    esk = [("ES", g) for g in range(4)]
                    tt("dve", ES[:, 0, :], ES[:, 0, :], ES[:, 1, :], ALU.add, esk, [("ES", 0)])
                    tt("dve", ES[:, 2, :], ES[:, 2, :], ES[:, 3, :], ALU.add, esk, [("ES", 2)])
                    tt("dve", EL, ES[:, 0, :], ES[:, 2, :], ALU.add, [("ES", 0), ("ES", 2)], ["EL"])
                    P.add("dve", lambda e: e.reduce_max(SC1[:, 0:1], EL, AXX), ["EL"], ["m1"])
                    ts("dve", M1, EL, SC1[:, 0:1], None, ALU.is_equal, None, ["EL", "m1"], ["M1"])
                    stt(E2, M1, NEG, EL, ALU.mult, ALU.add, ["M1", "EL"], ["E2"])
                    P.add("dve", lambda e: e.reduce_max(SC1[:, 1:2], E2, AXX), ["E2"], ["m2"])
                    ts("dve", M2, E2, SC1[:, 1:2], None, ALU.is_equal, None, ["E2", "m2"], ["M2"])
                    tt("dve", SC1[:, 2:3], SC1[:, 0:1], SC1[:, 1:2], ALU.subtract, ["m1", "m2"], ["dm"])
                    act(SC1[:, 3:4], SC1[:, 2:3], AF.Sigmoid, ["dm"], ["p1"])
                    ts("dve", SC1[:, 4:5], SC1[:, 3:4], -1.0, 1.0, ALU.mult, ALU.add, ["p1"], ["p2"])
                    tt("dve", SC1[:, 3:4], SC1[:, 3:4], GM[:, 2:3], ALU.mult, ["p1", "GW", "p2"], ["p1g"])
                    tt("dve", SC1[:, 4:5], SC1[:, 4:5], GM[:, 2:3], ALU.mult, ["p2", "GW"], ["p2g"])
                    ts("dve", WGp, M1, SC1[:, 3:4], None, ALU.mult, None, ["M1", "p1g"], ["WGp"])
                    stt(WGp, M2, SC1[:, 4:5], WGp, ALU.mult, ALU.add, ["M2", "p2g", "WGp"], ["WGp"])
                    for g in range(4):
                        ts("dve", CMB[:, t, g * 4:(g + 1) * 4], WGp, OH[:, g:g + 1], None, ALU.mult, None,
                           ["WGp", "OH"], [("CMB", t)])

            def load_expert(e, s):
                w1, w3, w2 = WS[s]
                dma("pool", w1, ew1[l, e, :, :].rearrange("(k p) c -> p k c", p=128), (), [("EW1", s)])
                dma("pool", w3, ew3[l, e, :, :].rearrange("(k p) c -> p k c", p=128), (), [("EW3", s)])
                dma("pool", w2, ew2[l, e, :, :].rearrange("(k p) c -> p k c", p=128), (), [("EW2", s)])
            load_expert(0, ecount % 2)
            for e in range(16):
                s = ecount % 2
                ecount += 1
                if e + 1 < 16:
                    load_expert(e + 1, 1 - s)
                w1, w3, w2 = WS[s]
                for si, m in enumerate(sc):
                    hs = si % 2
                    for hc in range(2):
                        b1 = nbank(0, 7)
                        for k in range(8):
                            mm(ps[b1][:, 0:CH], w1[:, k, hc * 128:(hc + 1) * 128], VTT[:, si, k, :], k == 0, k == 7,
                               [("EW1", s), ("VTT", si)], [psk(b1)])
                        b3 = nbank(0, 7)
                        for k in range(8):
                            mm(ps[b3][:, 0:CH], w3[:, k, hc * 128:(hc + 1) * 128], VTT[:, si, k, :], k == 0, k == 7,
                               [("EW3", s), ("VTT", si)], [psk(b3)])
                        act(S1[hc], ps[b1][:, 0:CH], AF.Silu, [psk(b1)], [("S1", hc)])
                        tt("dve", HE[:, hs, hc, :], S1[hc], ps[b3][:, 0:CH], ALU.mult, [("S1", hc), psk(b3)],
                           [("HE", hs, hc)])
                    for i in range(3):
                        t = si * 3 + i
                        for half in range(2):
                            b = nbank(0, 7)
                            for hc in range(2):
                                mm(ps[b][:, 0:512], HE[:, hs, hc, i * 128:(i + 1) * 128],
                                   w2[:, hc, half * 512:(half + 1) * 512], hc == 0, hc == 1,
                                   [("HE", hs, 0), ("HE", hs, 1), ("EW2", s)], [psk(b)])
                            stt(ACCT[:, t, half * 512:(half + 1) * 512], ps[b][:, 0:512], CMB[:, t, e:e + 1],
                                ACCT[:, t, half * 512:(half + 1) * 512], ALU.mult, ALU.add,
                                [psk(b), ("CMB", t), ("ACC", t)], [("ACC", t)])
            for si, m in enumerate(sc):
                for i in range(3):
                    t = si * 3 + i
                    r0 = m * CH + i * 128
                    if not last:
                        dma("sp", hbuf[r0:r0 + 128, :], ACCT[:, t, :], [("ACC", t)], [("hsrc", m), ("hw", m, i)])
                    else:
                        ss = small[:, 52:53]
                        rs = small[:, 53:54]
                        stt(OT, ACCT[:, t, :], 1.0, ACCT[:, t, :], ALU.mult, ALU.mult, [("ACC", t)], ["OT", "ss3"],
                            accum=ss)
                        rsqrt(rs, ss, 1.0 / D, ["ss3"], ["rs3"])
                        stt(OT, ACCT[:, t, :], rs, fing[:, :], ALU.mult, ALU.mult, [("ACC", t), "rs3", "fing"], ["OT"])
                        dma("sp", out_d[r0:r0 + 128, :], OT, ["OT"], [("out", m, i)])

    def phase3b_sparse(l, last):
        A = Arena()
        AXX = mybir.AxisListType.X
        NT = NOWNT
        VBF = A.bf16(NT * D).rearrange("p (t d) -> p t d", d=D)
        INDb = A.bf16(NT * 16)
        M1G = A.f32(NT * 16)
        M2G = A.f32(NT * 16)
        RIN = A.f32(NT * 16)
        TOT = A.f32(NT * 16)
        INC = A.f32(NT * 16)
        RB = A.f32(NT * 16)
        PRD = A.f32(NT * 16)
        PW = A.f32(NT * 2).rearrange("p (t j) -> p t j", j=2)
        SLF = [A.f32(NT) for _ in range(2)]
        SLI = [AR[:, A.off + i * 40:A.off + i * 40 + NT].bitcast(I32) for i in range(2)]
        A.off += 80
        CNT = A.f32(16)
        NTL = A.f32(16)
        PC = A.f32(16)
        BEND = A.f32(16)
        BASE = A.f32(16)
        ONE16 = A.f32(16)
        ONE33 = A.f32(NT)
        TMP33 = A.f32(NT)
        TE = A.f32(NSLT)
        WIF = A.f32(NSLT)
        WII = AR[:, A.off:A.off + NSLT].bitcast(I32)
        A.off += 88
        UST = A.bf16(128)
        ONEb = A.bf16(128)
        RW = v3(A.bf16(160), 8)
        ubfx = A.bf16(D)
        HT = [A.f32(D) for _ in range(3)]
        NRS = 3
        RSET = [dict(VTt=v3(A.bf16(8 * 128), 8), LG=A.f32(20), GM=A.f32(4), OH=A.f32(4), EX=A.f32(4),
                     ES=v3(A.f32(16), 4), EL=A.f32(4), M1=A.f32(4), M2=A.f32(4), E2=A.f32(4), SC1=A.f32(8),
                     ss=A.f32(1), rs=A.f32(1)) for _ in range(NRS)]
        NWB = 3
        WB = [(A.bf16(2048), A.bf16(2048), A.bf16(2048)) for _ in range(NWB)]
        XI = [A.bf16(D) for _ in range(3)]
        XT = [v3(A.bf16(8 * 128), 8) for _ in range(2)]
        S1 = A.f32(256)
        HEb = [A.bf16(256) for _ in range(2)]
        YT = [A.f32(D) for _ in range(2)]
        Y1 = [WB[hb_][0].bitcast(F32) for hb_ in range(2)]
        Y2 = [WB[hb_][1].bitcast(F32) for hb_ in range(2)]
        OT = YT[0]
        M1G3 = M1G.rearrange("p (t e) -> p t e", e=16)
        M2G3 = M2G.rearrange("p (t e) -> p t e", e=16)
        IND3 = INDb.rearrange("p (t e) -> p t e", e=16)
        dma("pool", RW, rw_d[l, :, :].rearrange("(k p) c -> p k c", p=128), (), ["RW"])
        tt("dve", UST, cst[:, C_U:C_U + 128], cst[:, C_ID:C_ID + 128], ALU.subtract, ["cst"], ["UST"])
        cp("dve", ONEb, cst[:, C_ONES:C_ONES + 128], ["cst"], ["ONEb"])
        memset("dve", ONE16, 1.0, ["ONE16"])
        memset("dve", ONE33, 1.0, ["ONE33"])
        def router_tile(t):
            R_ = RSET[t % NRS]
            q = t % NRS
            VTt, LG, GM, OH, EX, ES, EL, M1, M2, E2, SC1 = (R_[k] for k in
                                                            ("VTt", "LG", "GM", "OH", "EX", "ES", "EL", "M1", "M2", "E2", "SC1"))
            K = lambda nm: (nm, q)
            m, i = t // 3, t % 3
            hb = t % 3
            r0 = m * CH + i * 128
            dma("sp", HT[hb], hbuf[r0:r0 + 128, :], [("hsrc", m)], [("HT", hb)])
            ss = R_["ss"]
            rs = R_["rs"]
            stt(ubfx, HT[hb], 1.0, HT[hb], ALU.mult, ALU.mult, [("HT", hb)], ["ubfx", K("ss2")], accum=ss)
            yield
            rsqrt(rs, ss, 1.0 / D, [K("ss2")], [K("rs2")])
            yield
            stt(VBF[:, t, :], HT[hb], rs, vecs[:, V_G2:V_G2 + D], ALU.mult, ALU.mult,
                [("HT", hb), K("rs2"), ("vecs", l)], [("VBF", t)])
            for k in range(8):
                P.add("pe", lambda e, k=k, t=t: e.transpose(psT[:, k * 128:(k + 1) * 128],
                                                            VBF[:, t, k * 128:(k + 1) * 128], ident[:, :]),
                      [("VBF", t), "ident"], ["psT"])
            cp("act", VTt, psT[:, :].rearrange("p (k t) -> p k t", k=8), ["psT"], [K("VTt")])
            b = nbank(0, 7)
            for k in range(8):
                mm(ps[b][:, 0:20], VTt[:, k, :], RW[:, k, :], k == 0, k == 7, [K("VTt"), "RW"], [psk(b)])
            yield
            tt("dve", LG, ps[b][:, 0:20], vecs[:, V_RB:V_RB + 20], ALU.add, [psk(b), ("vecs", l)], [K("LG")])
            yield
            tt("dve", LG, LG, cst[:, C_TB:C_TB + 20], ALU.add, [K("LG"), "cst"], [K("LG")])
            yield
            P.add("dve", lambda e: e.reduce_max(GM[:, 0:1], LG[:, 0:4], AXX), [K("LG")], [K("GM")])
            yield
            ts("dve", OH, LG[:, 0:4], GM[:, 0:1], None, ALU.is_equal, None, [K("LG"), K("GM")], [K("OH")])
            ts("dve", EX, LG[:, 0:4], GM[:, 0:1], None, ALU.subtract, None, [K("LG"), K("GM")], [K("EX")])
            yield
            act(EX, EX, AF.Exp, [K("EX")], [K("EX")])
            LE = LG[:, 4:20].rearrange("p (g e) -> p g e", g=4)
            for g in range(4):
                ts("dve", ES[:, g, :], LE[:, g, :], OH[:, g:g + 1], None, ALU.mult, None, [K("LG"), K("OH")],
                   [K(("ES", g))])
            yield
            esk = [K(("ES", g)) for g in range(4)]
            tt("dve", ES[:, 0, :], ES[:, 0, :], ES[:, 1, :], ALU.add, esk, [K(("ES", 0))])
            tt("dve", ES[:, 2, :], ES[:, 2, :], ES[:, 3, :], ALU.add, esk, [K(("ES", 2))])
            yield
            tt("dve", EL, ES[:, 0, :], ES[:, 2, :], ALU.add, [K(("ES", 0)), K(("ES", 2))], [K("EL")])
            P.add("dve", lambda e: e.reduce_sum(GM[:, 1:2], EX, AXX), [K("EX"), K("GM")], [K("GS")])
            yield
            P.add("dve", lambda e: e.reduce_max(SC1[:, 0:1], EL, AXX), [K("EL")], [K("m1")])
            P.add("dve", lambda e: e.reciprocal(GM[:, 2:3], GM[:, 1:2]), [K("GS")], [K("GW")])
            yield
            ts("dve", M1, EL, SC1[:, 0:1], None, ALU.is_equal, None, [K("EL"), K("m1")], [K("M1")])
            yield
            stt(E2, M1, NEG, EL, ALU.mult, ALU.add, [K("M1"), K("EL")], [K("E2")])
            yield
            P.add("dve", lambda e: e.reduce_max(SC1[:, 1:2], E2, AXX), [K("E2")], [K("m2")])
            yield
            ts("dve", M2, E2, SC1[:, 1:2], None, ALU.is_equal, None, [K("E2"), K("m2")], [K("M2")])
            tt("dve", SC1[:, 2:3], SC1[:, 0:1], SC1[:, 1:2], ALU.subtract, [K("m1"), K("m2")], [K("dm")])
            yield
            act(SC1[:, 3:4], SC1[:, 2:3], AF.Sigmoid, [K("dm")], [K("p1")])
            for g in range(4):
                ts("dve", M1G3[:, t, g * 4:(g + 1) * 4], M1, OH[:, g:g + 1], None, ALU.mult, None, [K("M1"), K("OH")],
                   [("M1G", t)])
                ts("dve", M2G3[:, t, g * 4:(g + 1) * 4], M2, OH[:, g:g + 1], None, ALU.mult, None, [K("M2"), K("OH")],
                   [("M2G", t)])
            yield
            ts("dve", SC1[:, 4:5], SC1[:, 3:4], -1.0, 1.0, ALU.mult, ALU.add, [K("p1")], [K("p2")])
            tt("dve", IND3[:, t, :], M1G3[:, t, :], M2G3[:, t, :], ALU.add, [("M1G", t), ("M2G", t)], [("IND", t)])
            yield
            tt("dve", PW[:, t, 0:1], SC1[:, 3:4], GM[:, 2:3], ALU.mult, [K("p1"), K("GW"), K("p2")], [("PW", t)])
            tt("dve", PW[:, t, 1:2], SC1[:, 4:5], GM[:, 2:3], ALU.mult, [K("p2"), K("GW"), ("PW", t)], [("PW", t)])
            yield

        for t0 in range(0, NT, NRS):
            gens = [router_tile(t) for t in range(t0, min(NT, t0 + NRS))]
            while gens:
                for g_ in list(gens):
                    try:
                        next(g_)
                    except StopIteration:
                        gens.remove(g_)
        indk = [("IND", t) for t in range(NT)]
        m1k = [("M1G", t) for t in range(NT)]
        m2k = [("M2G", t) for t in range(NT)]
        for half in range(2):
            cs = slice(half * 264, (half + 1) * 264)
            mm(ps[half][:, 0:264], UST, INDb[:, cs], True, True, indk + ["UST"], [psk(half)])
            mm(ps[2 + half][:, 0:264], ONEb, INDb[:, cs], True, True, indk + ["ONEb"], [psk(2 + half)])
            cp("act", RIN[:, cs], ps[half][:, 0:264], [psk(half)], [("RIN", half)])
            cp("dve", TOT[:, cs], ps[2 + half][:, 0:264], [psk(2 + half)], [("TOT", half)])
        TOT3 = TOT.rearrange("p (t e) -> p t e", e=16)
        INC3 = INC.rearrange("p (t e) -> p t e", e=16)
        RB3 = RB.rearrange("p (t e) -> p t e", e=16)
        for e_ in range(16):
            P.add("dve", lambda e, e_=e_: e.tensor_tensor_scan(INC3[:, :, e_], ONE33, TOT3[:, :, e_], 0.0,
                                                              ALU.mult, ALU.add),
                  [("TOT", 0), ("TOT", 1), "ONE33"], [("INC", e_)])
        ik = [("INC", e_) for e_ in range(16)]
        cp("dve", CNT, INC3[:, NT - 1, :], ik, ["CNT"])
        tt("dve", RB, INC, TOT, ALU.subtract, ik + [("TOT", 0), ("TOT", 1)], ["RB"])
        tt("dve", RB, RB, RIN, ALU.add, ["RB", ("RIN", 0), ("RIN", 1)], ["RB"])
        for e_ in range(16):
            ts("dve", TMP33, cst[:, C_TH:C_TH + NT], CNT[:, e_:e_ + 1], None, ALU.is_le, None, ["cst", "CNT"], ["TMP33"])
            P.add("dve", lambda e, e_=e_: e.reduce_sum(NTL[:, e_:e_ + 1], TMP33, AXX), ["TMP33"], [("NTL", e_)])
        ts("dve", PC, NTL, 128.0, None, ALU.mult, None, [("NTL", e_) for e_ in range(16)], ["PC"])
        P.add("dve", lambda e: e.tensor_tensor_scan(BEND, ONE16, PC, 0.0, ALU.mult, ALU.add), ["PC", "ONE16"], ["BEND"])
        tt("dve", BASE, BEND, PC, ALU.subtract, ["BEND", "PC"], ["BASE"])
        for t in range(NT):
            tt("dve", RB3[:, t, :], RB3[:, t, :], BASE, ALU.add, ["RB", "BASE"], ["RB"])
        for j, (MG_, mk_) in enumerate(((M1G, m1k), (M2G, m2k))):
            tt("dve", PRD, MG_, RB, ALU.mult, mk_ + ["RB"], ["PRD"])
            P.add("dve", lambda e, j=j: e.tensor_reduce(SLF[j], PRD.rearrange("p (t e) -> p t e", e=16), AXX, ALU.add),
                  ["PRD"], [("SLF", j)])
            cp("dve", SLI[j], SLF[j], [("SLF", j)], [("SLI", j)])
        memset("dve", TE, 0.0, ["TE"])
        for e_ in range(16):
            stt(TE, cst[:, C_JT:C_JT + NSLT], BEND[:, e_:e_ + 1], TE, ALU.is_ge, ALU.add, ["cst", "BEND", "TE"], ["TE"])
        ts("dve", TE, TE, 15.0, None, ALU.min, None, ["TE"], ["TE"])
        ts("dve", WIF, TE, 128.0, cst[:, C_PIDX:C_PIDX + 1], ALU.mult, ALU.add, ["TE", "cst"], ["WIF"])
        cp("dve", WII, WIF, ["WIF"], ["WII"])
        if "moe" in dbg_out and l == 0:
            dm = dbg_out["moe"]
            for (o, n, ap, k) in ((0, NT, SLF[0], ("SLF", 0)), (40, NT, SLF[1], ("SLF", 1)), (80, 16, CNT, "CNT"),
                                  (96, 16, PC, "PC"), (112, 16, BEND, "BEND"), (128, 16, BASE, "BASE"),
                                  (144, NSLT, TE, "TE"), (232, NSLT, WIF, "WIF"),
                                  (320, 66, PW.rearrange("p t j -> p (t j)"), None)):
                rk = [k] if k is not None else [("PW", t) for t in range(NT)]
                dma("sp", dm[:, o:o + n], ap, rk, [("dbgmoe", o)])
        for t in range(NT):
            for j in range(2):
                P.add("pool", lambda e, t=t, j=j: e.indirect_dma_start(
                    out=XS[:, :], out_offset=bass.IndirectOffsetOnAxis(ap=SLI[j][:, t:t + 1], axis=0),
                    in_=VBF[:, t, :], in_offset=None), [("VBF", t), ("SLI", j)], [("XS", t, j)], kind="d")
        xsk = [("XS", t, j) for t in range(NT) for j in range(2)]
        wsk = [(nm, e_) for nm in ("W1s", "W3s", "W2s") for e_ in range(16)]
        def load_slot(j):
            wb = WB[j % NWB]
            for a, (Wd, nm) in enumerate(((W1s[l], "w1"), (W3s[l], "w3"), (W2s[l], "w2"))):
                P.add("pool", lambda e, a=a, Wd=Wd, wb=wb, j=j: e.indirect_dma_start(
                    out=wb[a], out_offset=None, in_=Wd[:, :],
                    in_offset=bass.IndirectOffsetOnAxis(ap=WII[:, j:j + 1], axis=0)),
                    ["WII"] + wsk, [("WB", j % NWB, a)], kind="d")
            dma("sp", XI[j % 3], XS[j * 128:(j + 1) * 128, :], xsk, [("XI", j % 3)])
        load_slot(0)
        load_slot(1)
        for j in range(NSLT):
            if j + 2 < NSLT:
                load_slot(j + 2)
            w1, w3, w2 = WB[j % NWB]
            w1 = v3(w1, 8)
            w3 = v3(w3, 8)
            w2 = v3(w2, 2)
            wk = [("WB", j % NWB, a) for a in range(3)]
            xi = XI[j % 3]
            xt = XT[j % 2]
            for k in range(8):
                P.add("pe", lambda e, k=k, xi=xi: e.transpose(psT[:, k * 128:(k + 1) * 128],
                                                              xi[:, k * 128:(k + 1) * 128], ident[:, :]),
                      [("XI", j % 3), "ident"], ["psT"])
            cp("act", xt, psT[:, :].rearrange("p (k t) -> p k t", k=8), ["psT"], [("XT", j % 2)])
            b1 = nbank(0, 7)
            b3 = nbank(0, 7)
            for hc in range(2):
                for k in range(8):
                    mm(ps[b1][:, hc * 128:(hc + 1) * 128], w1[:, k, hc * 128:(hc + 1) * 128], xt[:, k, :],
                       k == 0, k == 7, wk + [("XT", j % 2)], [psk(b1)])
                for k in range(8):
                    mm(ps[b3][:, hc * 128:(hc + 1) * 128], w3[:, k, hc * 128:(hc + 1) * 128], xt[:, k, :],
                       k == 0, k == 7, wk + [("XT", j % 2)], [psk(b3)])
            act(S1, ps[b1][:, 0:256], AF.Silu, [psk(b1)], ["S1s"])
            he = HEb[j % 2]
            tt("dve", he, S1, ps[b3][:, 0:256], ALU.mult, ["S1s", psk(b3)], [("HEb", j % 2)])
            yt = YT[j % 2]
            for half in range(2):
                b = nbank(0, 7)
                for hc in range(2):
                    mm(ps[b][:, 0:512], he[:, hc * 128:(hc + 1) * 128], w2[:, hc, half * 512:(half + 1) * 512],
                       hc == 0, hc == 1, wk + [("HEb", j % 2)], [psk(b)])
                cp("act" if half == 0 else "dve", yt[:, half * 512:(half + 1) * 512], ps[b][:, 0:512], [psk(b)],
                   [("YT", j % 2, half)])
            dma("sp", YS[j * 128:(j + 1) * 128, :], yt, [("YT", j % 2, 0), ("YT", j % 2, 1)], [("YS", j)])
        ysk = [("YS", j) for j in range(NSLT)]
        def load_comb(t):
            m, i = t // 3, t % 3
            r0 = m * CH + i * 128
            hb = t % 2
            dma("sp", HT[hb], hbuf[r0:r0 + 128, :], [("hsrc", m)], [("HT", hb)])
            for j, Yb in enumerate((Y1, Y2)):
                P.add("pool", lambda e, t=t, j=j, Yb=Yb, hb=hb: e.indirect_dma_start(
                    out=Yb[hb], out_offset=None, in_=YS[:, :],
                    in_offset=bass.IndirectOffsetOnAxis(ap=SLI[j][:, t:t + 1], axis=0)),
                    [("SLI", j)] + ysk, [("Yg", j, hb), ("WB", hb, j)], kind="d")
        load_comb(0)
        for t in range(NT):
            if t + 1 < NT:
                load_comb(t + 1)
            m, i = t // 3, t % 3
            r0 = m * CH + i * 128
            hb = t % 2
            stt(HT[hb], Y1[hb], PW[:, t, 0:1], HT[hb], ALU.mult, ALU.add, [("Yg", 0, hb), ("PW", t), ("HT", hb)],
                [("HT", hb)])
            stt(HT[hb], Y2[hb], PW[:, t, 1:2], HT[hb], ALU.mult, ALU.add, [("Yg", 1, hb), ("PW", t), ("HT", hb)],
                [("HT", hb)])
            if not last:
                dma("sp", hbuf[r0:r0 + 128, :], HT[hb], [("HT", hb)], [("hsrc", m), ("hw", m, i)])
            else:
                ss = small[:, 52:53]
                rs = small[:, 53:54]
                otk = [("YT", 0, 0), ("YT", 0, 1)]
                stt(OT, HT[hb], 1.0, HT[hb], ALU.mult, ALU.mult, [("HT", hb)], otk + ["ss3"], accum=ss)
                rsqrt(rs, ss, 1.0 / D, ["ss3"], ["rs3"])
                stt(OT, HT[hb], rs, fing[:, :], ALU.mult, ALU.mult, [("HT", hb), "rs3", "fing"], otk)
                dma("sp", out_d[r0:r0 + 128, :], OT, otk, [("out", m, i)])

    phases = debug.get("phases", None)
    n = 0

    def want():
        return phases is None or n < phases
    for l in range(DEPTH):
        src = h0 if l == 0 else hbuf
        for ph in ("p1", "ex", "p2a", "p2b", "p3a", "p3b"):
            if not want():
                break
            if ph == "p1":
                load_layer_vecs(l)
                phase1(l, src)
            elif ph == "ex":
                exchange()
            elif ph == "p2a":
                phase2a()
            elif ph == "p2b":
                phase2b(l)
            elif ph == "p3a":
                phase3a(l, src)
            else:
                if SPARSE_MOE:
                    phase3b_sparse(l, l == DEPTH - 1)
                else:
                    phase3b(l, l == DEPTH - 1)
            barrier()
            n += 1
    for nm, t in dbg_out.items():
        if nm in ("F", "moe"):
            continue
        src_t = scratch[nm]
        P.add("sp", lambda e, t=t, src_t=src_t: e.dma_start(out=t.ap(), in_=src_t.ap()), (), [("dbg", nm)], kind="d")
    barrier()
    P.emit()
    return nc, stack


def _constants(c):
    cst = np.zeros((128, NCST), np.float32)
    k = np.arange(128)
    cst[:, C_U:C_U + 128] = (k[:, None] <= k[None, :]).astype(np.float32)
    cst[:, C_ONES:C_ONES + 128] = 1.0
    cst[:, C_ODIV:C_ODIV + 128] = 1.0 / 256.0
    cst[:, C_SEL] = 1.0 if c == 0 else 0.0
    cst[:, C_SEL + 1] = 0.0 if c == 0 else 1.0
    q = np.arange(CH)
    for w in range(2):
        for kt in range(3):
            kpos = kt * 128 + k
            causal = np.where(kpos[:, None] <= q[None, :], 0.0, NEG).astype(np.float32)
            if c == 0:
                mk = causal if w == 0 else np.full((128, CH), NEG, np.float32)
            else:
                mk = np.zeros((128, CH), np.float32) if w == 0 else causal
            o = C_MASK + (w * 3 + kt) * CH
            cst[:, o:o + CH] = mk
    wins = np.array([2, 4, 8, 16], np.float32)
    for first in range(2):
        for cc in range(2):
            for half in range(2):
                W = wins[cc * 2 + half]
                t = np.arange(CH, dtype=np.float32)
                if first == 0 and c == 0:
                    cnt = np.minimum(t + 1.0, W)
                else:
                    cnt = np.full(CH, W, np.float32)
                o = C_INVC + (first * 2 + cc) * CH
                cst[half * 64:(half + 1) * 64, o:o + CH] = (1.0 / cnt)[None, :]
    cst[:, C_ID:C_ID + 128] = np.eye(128, dtype=np.float32)
    cst[:, C_JT:C_JT + 82] = (128.0 * np.arange(82, dtype=np.float32))[None, :]
    cst[:, C_TH:C_TH + 33] = (128.0 * np.arange(33, dtype=np.float32) + 1.0)[None, :]
    cst[:, C_PIDX] = np.arange(128, dtype=np.float32)
    cst[:, C_TB:C_TB + 4] = (-1e-7 * np.arange(4, dtype=np.float32))[None, :]
    cst[:, C_TB + 4:C_TB + 20] = np.tile(-1e-7 * np.arange(4, dtype=np.float32), 4)[None, :]
    return cst


_CACHE = {}


def kernel(x, meta, norm1_g, w_in, b_forget, pool_w, pool_b, pool_scale, conv_w, conv_b, conv_ln_g,
           conv_ln_b, w_out_a, w_out_b, w_out_c, w_o, norm2_g, router_g, router_g_b, router_e,
           router_e_b, exp_w1, exp_w3, exp_w2, final_g, _debug=None):
    f = lambda a: np.ascontiguousarray(np.asarray(a, dtype=np.float32))
    x = f(x)
    B = x.shape[0]
    L = 16 + x.shape[1]
    LP = NG * CH
    meta = f(meta)
    vecs = np.zeros((DEPTH, 128, NV), np.float32)
    chp = np.zeros((DEPTH, 128, NCP), np.float32)
    for l in range(DEPTH):
        vecs[l, :, V_G1:V_G1 + D] = f(norm1_g)[l][None, :]
        vecs[l, :, V_G2:V_G2 + D] = f(norm2_g)[l][None, :]
        vecs[l, :, V_BF:V_BF + 8] = f(b_forget)[l][None, :]
        vecs[l, :, V_RB:V_RB + 4] = f(router_g_b)[l][None, :]
        vecs[l, :, V_RB + 4:V_RB + 20] = f(router_e_b)[l][None, :]
        for cc in range(2):
            sl = slice(cc * 128, (cc + 1) * 128)
            chp[l, :, CP_PB + cc] = f(pool_b)[l].reshape(256)[sl]
            chp[l, :, CP_PS + cc] = f(pool_scale)[l][sl]
            chp[l, :, CP_CB + cc] = f(conv_b)[l][sl]
            chp[l, :, CP_LG + cc] = f(conv_ln_g)[l][sl]
            chp[l, :, CP_LB + cc] = f(conv_ln_b)[l][sl]
            chp[l, :, CP_CW + cc * 31:CP_CW + (cc + 1) * 31] = f(conv_w)[l][:, sl].T
    fing = np.ascontiguousarray(np.broadcast_to(f(final_g)[None, :], (128, D)))
    rw = np.ascontiguousarray(np.concatenate([f(router_g), f(router_e)], axis=-1))
    shared = dict(vecs=vecs, fing=fing, chp=chp, w_in=f(w_in), pool_w=f(pool_w), w_out_a=f(w_out_a),
                  w_out_b=f(w_out_b), w_out_c=f(w_out_c), w_o=f(w_o), rw=rw, exp_w1=f(exp_w1),
                  exp_w3=f(exp_w3), exp_w2=f(exp_w2))
    csts = [_constants(0), _constants(1)]
    in_maps = []
    for core in range(NCORES):
        b, c = core // 2, core % 2
        seq = np.zeros((LP, D), np.float32)
        seq[:16] = meta
        seq[16:L] = x[b]
        own = seq.reshape(NM, 2, CH, D)[:, c].reshape(OWN, D)
        d = dict(shared)
        d["h0"] = np.ascontiguousarray(own)
        d["cst"] = csts[c]
        in_maps.append(d)
    key = repr(sorted((_debug or {}).items()))
    if key not in _CACHE:
        _CACHE[key] = build_program(_debug)
    nc, _ = _CACHE[key]
    res = run_bass_kernel_spmd(nc, in_maps, core_ids=list(range(NCORES)))
    out = np.zeros((B, LP, D), np.float32)
    for core in range(NCORES):
        b, c = core // 2, core % 2
        out[b].reshape(NM, 2, CH, D)[:, c] = res.results[core]["out"].reshape(NM, CH, D)
    if _debug:
        kernel.last = res
    return np.ascontiguousarray(out[:, 16:L])
```

```python
import numpy as np
import ml_dtypes
from contextlib import ExitStack
import concourse.bass as bass
import concourse.mybir as mybir
from concourse.bass_utils import run_bass_kernel_spmd

F32 = mybir.dt.float32
BF16 = mybir.dt.bfloat16
I32 = mybir.dt.int32
SPARSE_MOE = True
ALU = mybir.AluOpType
AF = mybir.ActivationFunctionType

D = 1024
DEPTH = 2
NCORES = 8
CH = 384
NM = 11
OWN = NM * CH
NG = 2 * NM
NGT = NG * 3
NOWNT = NM * 3
N_IN = 5384
EPS = 1e-6
NEG = -30000.0

C_U, C_ONES, C_ODIV, C_SEL, C_MASK, C_INVC, C_ID = 0, 128, 256, 384, 386, 386 + 2304, 386 + 2304 + 1536
C_JT = C_ID + 128
C_TH = C_JT + 82
C_PIDX = C_TH + 33
C_TB = C_PIDX + 1
NCST = C_TB + 20
NSLT = 82
V_G1, V_G2, V_BF, V_RB = 0, 1024, 2048, 2056
NV = 2076
CP_PB, CP_PS, CP_CB, CP_LG, CP_LB, CP_CW = 0, 2, 4, 6, 8, 10
NCP = 10 + 62

SAME_ENGINE_SYNC = True
SEM_CAP = 16000
N_DMA_SEMS = 20
SEM_REARM_WAIT = ("sp", "act")


class Prog:
    def __init__(self, nc, stack):
        self.nc = nc
        self.stack = stack
        self.ops = []
        self.last_w = {}
        self.readers = {}
        self.last_compute = {}
        self.asyncs = []

    def add(self, eng, fn, reads=(), writes=(), kind="c", extra=()):
        i = len(self.ops)
        deps = {}
        for j in extra:
            deps[j] = "raw"
        for r in reads:
            w = self.last_w.get(r)
            if w is not None:
                deps[w] = "raw"
        for k in writes:
            w = self.last_w.get(k)
            if w is not None:
                deps[w] = "raw"
            for _, j in self.readers.get(k, {}).items():
                if j not in deps:
                    deps[j] = "war"
        for r in reads:
            self.readers.setdefault(r, {})[eng if kind == "c" else (eng, i)] = i
        for k in writes:
            self.last_w[k] = i
            self.readers[k] = {}
        self.ops.append(dict(eng=eng, fn=fn, deps=deps, kind=kind))
        if fn is not None:
            if kind == "c":
                self.last_compute[eng] = i
            else:
                self.asyncs.append(i)
        return i

    def emit(self):
        nc = self.nc
        ops = self.ops
        engs = ["pe", "act", "dve", "pool", "sp"]
        known = {e: {} for e in engs}
        needed = set()
        waits = [None] * len(ops)
        for i, op in enumerate(ops):
            E = op["eng"]
            wl = []
            best = {}
            for j, kd in op["deps"].items():
                oj = ops[j]
                if oj["kind"] != "c":
                    if known[E].get(("d", j)):
                        continue
                    known[E][("d", j)] = True
                    wl.append(j)
                    continue
                X = oj["eng"]
                if X == E and op["kind"] == "c":
                    if E == "pe" or kd == "war" or not SAME_ENGINE_SYNC:
                        continue
                if j > best.get(X, -1):
                    best[X] = j
            for X, j in best.items():
                if known[E].get(X, -1) >= j:
                    continue
                known[E][X] = j
                wl.append(j)
            waits[i] = wl
            for j in wl:
                assert ops[j]["fn"] is not None
                needed.add(j)
        sem_of = {}
        cnt = {e: 0 for e in engs}
        eng_sems = {e: [] for e in engs}
        dma_sems = {e: [] for e in engs}
        dma_cnt = {}
        dma_rr = {e: 0 for e in engs}
        dma_last = {}
        nsem = [0]

        def new_sem(tag):
            nsem[0] += 1
            return self.stack.enter_context(nc.semaphore("%s_%d" % (tag, nsem[0])))

        for i, op in enumerate(ops):
            E = op["eng"]
            if op["kind"] == "c":
                if i in needed:
                    n = cnt[E]
                    cnt[E] += 1
                    si = n // SEM_CAP
                    while len(eng_sems[E]) <= si:
                        eng_sems[E].append(new_sem("e" + E))
                    sem_of[i] = (eng_sems[E][si], n % SEM_CAP + 1, 1)
            elif op["kind"] == "d":
                if len(dma_sems[E]) < N_DMA_SEMS:
                    dma_sems[E].append(new_sem("d" + E))
                    dma_cnt[(E, len(dma_sems[E]) - 1)] = 0
                k = dma_rr[E] % len(dma_sems[E]) if len(dma_sems[E]) == N_DMA_SEMS else len(dma_sems[E]) - 1
                dma_rr[E] += 1
                pj = dma_last.get((E, k))
                if pj is not None and pj not in waits[i] and E in SEM_REARM_WAIT:
                    waits[i].append(pj)
                dma_last[(E, k)] = i
                dma_cnt[(E, k)] += 16
                sem_of[i] = (dma_sems[E][k], dma_cnt[(E, k)], 16)
            else:
                sem_of[i] = (new_sem("cc"), 1, None)
        per_eng = {e: [] for e in engs}
        for i, op in enumerate(ops):
            per_eng[op["eng"]].append(i)

        def run(E, e):
            for i in per_eng[E]:
                op = ops[i]
                for j in waits[i]:
                    s, v, _ = sem_of[j]
                    e.wait_ge(s, v)
                if op["fn"] is None:
                    continue
                ins = op["fn"](e)
                if i in sem_of:
                    s, v, inc = sem_of[i]
                    if inc is None:
                        ins.then_inc(s)
                    else:
                        ins.then_inc(s, inc)

        with nc.Block() as block:
            @block.tensor
            def _(e):
                run("pe", e)

            @block.scalar
            def _(e):
                run("act", e)

            @block.vector
            def _(e):
                run("dve", e)

            @block.gpsimd
            def _(e):
                run("pool", e)

            @block.sync
            def _(e):
                run("sp", e)


def build_program(debug=None):
    debug = debug or {}
    nc = bass.Bass("TRN2", target_bir_lowering=False)
    stack = ExitStack()
    P = Prog(nc, stack)

    def din(name, shape, dt=F32):
        return nc.dram_tensor(name, list(shape), dt, kind="ExternalInput").ap()

    h0 = din("h0", [OWN, D])
    cst_d = din("cst", [128, NCST])
    vecs_d = din("vecs", [DEPTH, 128, NV])
    fing_d = din("fing", [128, D])
    chp_d = din("chp", [DEPTH, 128, NCP])
    w_in = din("w_in", [DEPTH, D, N_IN])
    pool_w = din("pool_w", [DEPTH, 4, 64, 64])
    w_oa = din("w_out_a", [DEPTH, 256, D])
    w_ob = din("w_out_b", [DEPTH, 256, D])
    w_oc = din("w_out_c", [DEPTH, 512, D])
    w_o = din("w_o", [DEPTH, D, D])
    rw_d = din("rw", [DEPTH, D, 20])
    ew1 = din("exp_w1", [DEPTH, 16, D, 256])
    ew3 = din("exp_w3", [DEPTH, 16, D, 256])
    ew2 = din("exp_w2", [DEPTH, 16, 256, D])
    out_d = nc.dram_tensor("out", [OWN, D], F32, kind="ExternalOutput").ap()

    hbuf = nc.dram_tensor("hbuf", [OWN, D], F32)
    qx = nc.dram_tensor("qx", [8, 66, OWN], BF16)
    ox = nc.dram_tensor("ox", [512, OWN], BF16)
    NPC = 4
    def pc_of(m):
        return m // 3, m % 3
    PCN = [3, 3, 3, 2]
    kx_loc = [nc.dram_tensor("kx_loc%d" % p, [512, PCN[p] * CH], BF16) for p in range(NPC)]
    kx_all = [nc.dram_tensor("kx_all%d" % p, [1024, PCN[p] * CH], BF16) for p in range(NPC)]
    vx_loc = [nc.dram_tensor("vx_loc%d" % p, [PCN[p] * CH, 520], BF16) for p in range(NPC)]
    vx_all = [nc.dram_tensor("vx_all%d" % p, [2 * PCN[p] * CH, 520], BF16) for p in range(NPC)]
    ag_loc = [nc.dram_tensor("ag_loc%d" % p, [512, PCN[p] * CH], BF16) for p in range(NPC)]
    ag_all = [nc.dram_tensor("ag_all%d" % p, [1024, PCN[p] * CH], BF16) for p in range(NPC)]
    lf_loc = nc.dram_tensor("lf_loc", [NM * 128, 24], F32)
    lf_all = nc.dram_tensor("lf_all", [2 * NM * 128, 24], F32)
    W1s = [nc.dram_tensor("W1s%d" % l, [2048, 2048], BF16) for l in range(DEPTH)]
    W3s = [nc.dram_tensor("W3s%d" % l, [2048, 2048], BF16) for l in range(DEPTH)]
    W2s = [nc.dram_tensor("W2s%d" % l, [2048, 2048], BF16) for l in range(DEPTH)]
    ux = nc.dram_tensor("ux", [NM, 128, 8 * CH], BF16)
    XS = nc.dram_tensor("XS", [NSLT * 128, D], BF16)
    YS = nc.dram_tensor("YS", [NSLT * 128, D], BF16)
    scratch = dict(hbuf=hbuf, qx=qx, ox=ox, lf_all=lf_all)
    for p in range(NPC):
        scratch["kx_all%d" % p] = kx_all[p]
        scratch["vx_all%d" % p] = vx_all[p]
        scratch["ag_all%d" % p] = ag_all[p]
    dbg_out = {}
    for nm in debug.get("dump", []):
        t = scratch[nm]
        dbg_out[nm] = nc.dram_tensor("dbg_" + nm, list(t.shape), t.dtype, kind="ExternalOutput")
    if "moe" in debug.get("dump_sb", []):
        dbg_out["moe"] = nc.dram_tensor("dbg_moe", [128, 512], F32, kind="ExternalOutput")
    if "F" in debug.get("dump_sb", []):
        dbg_out["F"] = nc.dram_tensor("dbg_F", [128, 528], F32, kind="ExternalOutput")

    def sb(name, shape, dt=F32):
        return stack.enter_context(nc.sbuf_tensor("sb_" + name, list(shape), dt))

    cst = sb("cst", [128, NCST])
    ident = sb("ident", [128, 128], BF16)
    vecs = sb("vecs", [128, NV])
    chp = sb("chp", [128, NCP])
    fing = sb("fing", [128, D])
    small = sb("small", [128, 64])
    FTP = sb("FTP", [128, 528 + 528 + 88 + 132])
    AR_WORDS = 44128
    AR = sb("AR", [128, AR_WORDS])

    class Arena:
        def __init__(self):
            self.off = 0

        def f32(self, n, parts=128):
            a = AR[0:parts, self.off:self.off + n]
            self.off += (n + 7) // 8 * 8
            assert self.off <= AR_WORDS, self.off
            return a

        def bf16(self, n, parts=128):
            w = (n + 1) // 2
            a = AR[0:parts, self.off:self.off + w].bitcast(BF16)
            self.off += (w + 7) // 8 * 8
            assert self.off <= AR_WORDS, self.off
            return a[:, 0:n]

    ps = [stack.enter_context(nc.psum_tensor("ps%d" % i, [128, 512], F32)) for i in range(7)]
    psT = stack.enter_context(nc.psum_tensor("psT", [128, 1024], BF16))

    def barrier():
        engs = ["pe", "act", "dve", "pool", "sp"]
        lasts = [i for i in (P.last_compute.get(x) for x in engs) if i is not None]
        asyncs = list(P.asyncs)
        P.asyncs = []
        for E in engs:
            P.add(E, None, (), (), extra=lasts + asyncs)

    rr = [0]

    def nbank(lo=0, hi=5):
        b = lo + rr[0] % (hi - lo)
        rr[0] += 1
        return b

    def psk(b):
        return ("ps", b)

    def dma(q, out, in_, reads, writes):
        return P.add(q, lambda e: e.dma_start(out=out, in_=in_), reads, writes, kind="d")

    def mm(out, lhsT, rhs, start, stop, reads, writes):
        return P.add("pe", lambda e: e.matmul(out, lhsT, rhs, start=start, stop=stop, skip_group_check=True),
                     reads, writes)

    def act(out, in_, func, reads, writes, bias=None, scale=None):
        kw = {}
        if bias is not None:
            kw["bias"] = bias
        if scale is not None:
            kw["scale"] = scale
        return P.add("act", lambda e: e.activation(out, in_, func, **kw), reads, writes)

    def tt(eng, out, in0, in1, op, reads, writes):
        return P.add(eng, lambda e: e.tensor_tensor(out, in0, in1, op), reads, writes)

    def ts(eng, out, in0, s1, s2, op0, op1, reads, writes):
        if op1 is None:
            return P.add(eng, lambda e: e.tensor_scalar(out, in0, s1, None, op0), reads, writes)
        return P.add(eng, lambda e: e.tensor_scalar(out, in0, s1, s2, op0, op1), reads, writes)

    def stt(out, in0, scalar, in1, op0, op1, reads, writes, accum=None):
        return P.add("dve", lambda e: e.scalar_tensor_tensor(out, in0, scalar, in1, op0, op1, accum_out=accum),
                     reads, writes)

    def cp(eng, out, in_, reads, writes):
        if eng == "act":
            return P.add("act", lambda e: e.copy(out, in_), reads, writes)
        return P.add(eng, lambda e: e.tensor_copy(out, in_), reads, writes)

    def memset(eng, ap, val, writes):
        return P.add(eng, lambda e: e.memset(ap, val), (), writes)

    def rsqrt(out, in_, scale, reads, writes):
        act(out, in_, AF.Sqrt, list(reads) + ["epsc"], writes, bias=epsc[0:out.shape[0], :], scale=scale)
        P.add("dve", lambda e: e.reciprocal(out, out), writes, writes)

    def v3(ap, a):
        return ap.rearrange("p (a b) -> p a b", a=a)

    dma("sp", cst[:, :], cst_d[:, :], (), ["cst"])
    dma("sp", fing[:, :], fing_d[:, :], (), ["fing"])
    cp("dve", ident[:, :], cst[:, C_ID:C_ID + 128], ["cst"], ["ident"])
    epsc = small[:, 60:61]
    onec = small[:, 61:62]
    memset("dve", epsc, EPS, ["epsc"])
    memset("dve", onec, 1.0, ["onec"])
    selA = cst[:, C_SEL:C_SEL + 1]
    selB = cst[:, C_SEL + 1:C_SEL + 2]

    def maskap(w, kt, qlo):
        o = C_MASK + (w * 3 + kt) * CH
        return cst[:, o + qlo:o + CH]

    FT = FTP[:, 0:528].rearrange("p (g h) -> p g h", h=8)
    CY = FTP[:, 528:1056].rearrange("p (g h) -> p g h", h=8)
    CS = FTP[:, 1056:1144].rearrange("p (m h) -> p m h", h=8)
    BI = [FTP[:, 1144 + i * 66:1144 + (i + 1) * 66] for i in range(2)]

    def load_h(src, m, htile, key):
        dma("sp", htile, src[m * CH:(m + 1) * CH, :].rearrange("(i p) d -> p i d", p=128),
            (("hsrc", m),), [key])

    def norm_to_uT(htile, hkey, ubf, U, ukey, goff, l):
        for i in range(3):
            ss = small[:, i:i + 1]
            stt(ubf, htile[:, i, :], 1.0, htile[:, i, :], ALU.mult, ALU.mult,
                [hkey], ["ubf", ("ss", i)], accum=ss)
            rs = small[:, 4 + i:5 + i]
            rsqrt(rs, ss, 1.0 / D, [("ss", i)], [("rs", i)])
            stt(ubf, htile[:, i, :], rs, vecs[:, goff:goff + D], ALU.mult, ALU.mult,
                [hkey, ("rs", i), ("vecs", l)], ["ubf"])
            for k in range(8):
                P.add("pe", lambda e, k=k: e.transpose(psT[:, k * 128:(k + 1) * 128], ubf[:, k * 128:(k + 1) * 128],
                                                       ident[:, :]),
                      ["ubf", "ident"], ["psT"])
            cp("act", U[:, :, i * 128:(i + 1) * 128], psT[:, :].rearrange("p (k t) -> p k t", k=8),
               ["psT"], [ukey])

    def norm_dve(htile, hkey, i, ub, goff, l):
        ss = small[:, i:i + 1]
        stt(ub, htile[:, i, :], 1.0, htile[:, i, :], ALU.mult, ALU.mult, [hkey], [("ub3", i), ("ss", i)], accum=ss)
        rs = small[:, 4 + i:5 + i]
        rsqrt(rs, ss, 1.0 / D, [("ss", i)], [("rs", i)])
        stt(ub, htile[:, i, :], rs, vecs[:, goff:goff + D], ALU.mult, ALU.mult,
            [hkey, ("rs", i), ("vecs", l)], [("ub3", i)])

    def norm_pe(ub, i, U, ukey):
        for k in range(8):
            P.add("pe", lambda e, k=k: e.transpose(psT[:, k * 128:(k + 1) * 128], ub[:, k * 128:(k + 1) * 128],
                                                   ident[:, :]),
                  [("ub3", i), "ident"], ["psT"])
        cp("act", U[:, :, i * 128:(i + 1) * 128], psT[:, :].rearrange("p (k t) -> p k t", k=8), ["psT"], [ukey])

    def load_layer_vecs(l):
        dma("sp", vecs[:, :], vecs_d[l, :, :], (), [("vecs", l)])
        dma("sp", chp[:, :], chp_d[l, :, :], (), [("chp", l)])

    def phase1(l, hsrc):
        A = Arena()
        W1 = A.bf16(8 * 2312).rearrange("p (k c) -> p k c", k=8)
        ht = [v3(A.f32(3 * D), 3) for _ in range(2)]
        ubf = A.bf16(D)
        ub3 = [A.bf16(D) for _ in range(3)]
        uT = [v3(A.bf16(8 * CH), 8) for _ in range(2)]
        sg = v3(A.f32(2 * CH), 2)
        ab = v3(A.bf16(2 * CH), 2)
        gb = v3(A.bf16(2 * CH), 2)
        kb = v3(A.bf16(4 * CH), 4)
        qb = v3(A.bf16(4 * CH), 4)
        vts = [A.bf16(520) for _ in range(2)]
        lts = [A.f32(24) for _ in range(2)]
        for (c0, c1) in ((0, 768), (768, 1280), (1280, 1792), (1792, 2312)):
            dma("pool", W1[:, :, c0:c1], w_in[l, :, c0:c1].rearrange("(k p) c -> p k c", p=128),
                (), [("W1", c0)])
        wkeys = [("W1", 0), ("W1", 768), ("W1", 1280), ("W1", 1792)]
        for i in range(2):
            memset("pool", vts[i], 1.0, [("vtile", i)])
        stg = [(A.bf16(2048), A.bf16(2048), A.bf16(2048)) for _ in range(4)]

        def prep_load(e):
            a1, a3, a2 = stg[e % 4]
            dma("pool", v3(a1, 8), ew1[l, e, :, :].rearrange("(k p) c -> p k c", p=128), (), [("pw1", e % 4)])
            dma("pool", v3(a3, 8), ew3[l, e, :, :].rearrange("(k p) c -> p k c", p=128), (), [("pw3", e % 4)])
            dma("pool", v3(a2, 2), ew2[l, e, :, :].rearrange("(k p) c -> p k c", p=128), (), [("pw2", e % 4)])

        def prep_store(e):
            a1, a3, a2 = stg[e % 4]
            dma("sp", W1s[l][e * 128:(e + 1) * 128, :], a1, [("pw1", e % 4)], [("W1s", e)])
            dma("sp", W3s[l][e * 128:(e + 1) * 128, :], a3, [("pw3", e % 4)], [("W3s", e)])
            dma("sp", W2s[l][e * 128:(e + 1) * 128, :], a2, [("pw2", e % 4)], [("W2s", e)])
        load_h(hsrc, 0, ht[0], ("ht", 0))
        for m in range(NM):
            hb = m % 2
            if m + 1 < NM:
                load_h(hsrc, m + 1, ht[1 - hb], ("ht", 1 - hb))
            U = uT[hb]
            ukey = ("uT", hb)
            if m < 8:
                prep_load(2 * m)
                prep_load(2 * m + 1)
            if 1 <= m < 9:
                prep_store(2 * m - 2)
                prep_store(2 * m - 1)
            if m == 0:
                for i in range(3):
                    norm_dve(ht[hb], ("ht", hb), i, ub3[i], V_G1, l)
                    norm_pe(ub3[i], i, U, ukey)
            dma("sp", ux[m, :, :], U.rearrange("p k t -> p (k t)"), [ukey], [("ux", m)])
            nxt = m + 1 < NM

            def early_norm(i):
                if nxt:
                    norm_dve(ht[1 - hb], ("ht", 1 - hb), i, ub3[i], V_G1, l)
            osl = slice(m * CH, (m + 1) * CH)
            pp, pm = pc_of(m)
            psl = slice(pm * CH, (pm + 1) * CH)
            p1stop = debug.get("p1stop", 99)
            if p1stop < 1:
                continue

            def fm_group(c0, M):
                b = nbank()
                for k in range(8):
                    mm(ps[b][0:M, 0:CH], W1[:, k, c0:c0 + M], U[:, k, :], k == 0, k == 7,
                       wkeys + [ukey], [psk(b)])
                return b
            for cc in range(2):
                b = fm_group(cc * 128, 128)
                cp("act", ab[:, cc, :], ps[b][:, 0:CH], [psk(b)], [("ab", cc)])
            dma("sp", ag_loc[pp][0:256, psl].rearrange("(c p) t -> p c t", p=128), ab,
                [("ab", 0), ("ab", 1)], [("ag_loc", m, 0)])
            early_norm(0)
            for cc in range(2):
                b2 = fm_group(512 + cc * 128, 128)
                act(sg[:, cc, :], ps[b2][:, 0:CH], AF.Sigmoid, [psk(b2)], [("sg", cc)])
                b1 = fm_group(256 + cc * 128, 128)
                tt("dve", gb[:, cc, :], ps[b1][:, 0:CH], sg[:, cc, :], ALU.mult, [psk(b1), ("sg", cc)], [("gb", cc)])
            dma("sp", ag_loc[pp][256:512, psl].rearrange("(c p) t -> p c t", p=128), gb,
                [("gb", 0), ("gb", 1)], [("ag_loc", m, 1)])
            early_norm(1)
            for kp in range(4):
                b = fm_group(768 + kp * 128, 128)
                P.add("act", lambda e, b=b, kp=kp: e.mul(qb[:, kp, :], ps[b][:, 0:CH], 0.125),
                      [psk(b)], [("qb", kp)])
                b = fm_group(1280 + kp * 128, 128)
                cp("dve", kb[:, kp, :], ps[b][:, 0:CH], [psk(b)], [("kb", kp)])
            for h in range(8):
                dma("sp", qx[h, 0:64, osl], qb[(h % 2) * 64:(h % 2) * 64 + 64, h // 2, :],
                    [("qb", h // 2)], [("qx", m, h)])
            dma("sp", kx_loc[pp][:, psl].rearrange("(k p) t -> p k t", p=128), kb[:, :, :],
                [("kb", kp) for kp in range(4)], [("kx_loc", m)])
            early_norm(2)
            for i in range(3):
                vb = (m * 3 + i) % 2
                vt = vts[vb].rearrange("p (h d) -> p h d", h=8)
                b = nbank()
                for k in range(8):
                    mm(ps[b][:, 0:512], U[:, k, i * 128:(i + 1) * 128], W1[:, k, 1792:2304], k == 0, k == 7,
                       wkeys + [ukey], [psk(b)])
                cp("act", vt[:, :, 0:64], ps[b][:, 0:512].rearrange("p (h d) -> p h d", h=8), [psk(b)],
                   [("vtile", vb)])
                r0 = pm * CH + i * 128
                dma("sp", vx_loc[pp][r0:r0 + 128, :], vts[vb], [("vtile", vb)], [("vx_loc", m, i)])
                if p1stop < 5:
                    continue
                b = nbank()
                for k in range(8):
                    mm(ps[b][:, 0:8], U[:, k, i * 128:(i + 1) * 128], W1[:, k, 2304:2312], k == 0, k == 7,
                       wkeys + [ukey], [psk(b)])
                lt = lts[m % 2][:, i * 8:(i + 1) * 8]
                vb = (m % 2, i)
                tt("dve", lt, ps[b][:, 0:8], vecs[:, V_BF:V_BF + 8], ALU.add, [psk(b), ("vecs", l)], [("lt", vb)])
                if p1stop < 6:
                    continue
                if not debug.get("noact"):
                    act(lt, lt, AF.Exp, [("lt", vb)], [("lt", vb)], scale=-1.0)
                    act(lt, lt, AF.Ln, [("lt", vb), "onec"], [("lt", vb)], bias=onec)
                    ts("dve", lt, lt, -1.0, None, ALU.mult, None, [("lt", vb)], [("lt", vb)])
            dma("sp", lf_loc[m * 128:(m + 1) * 128, :], lts[m % 2], [("lt", (m % 2, i)) for i in range(3)],
                [("lf_loc", m)])
            if nxt:
                for i in range(3):
                    norm_pe(ub3[i], i, uT[1 - hb], ("uT", 1 - hb))
            if m == NM - 1 or (m + 1) % 3 == 0:
                exchange_piece(m // 3)

    groups = [[0, 1], [2, 3], [4, 5], [6, 7]]

    def cc(src, dst, rkeys, wkey):
        P.add("pool", lambda e: e.collective_compute("AllGather", ALU.bypass, replica_groups=groups,
                                                     ins=[src.ap().opt()], outs=[dst.ap().opt()]),
              rkeys, [wkey], kind="cc")

    def exchange_piece(p):
        ms = [m for m in range(NM) if m // 3 == p]
        cc(kx_loc[p], kx_all[p], [("kx_loc", m) for m in ms], ("kx_all", p))
        cc(vx_loc[p], vx_all[p], [("vx_loc", m, i) for m in ms for i in range(3)], ("vx_all", p))
        cc(ag_loc[p], ag_all[p], [("ag_loc", m, j) for m in ms for j in range(2)], ("ag_all", p))

    def exchange():
        cc(lf_loc, lf_all, [("lf_loc", m) for m in range(NM)], "lf_all")

    def phase2a():
        A = Arena()
        LFf = A.f32(528)
        LF = LFf.rearrange("p (m r i h) -> p m r i h", m=NM, r=2, i=3)
        TT = A.f32(528)
        INC = A.f32(528)
        ONE = A.f32(66)
        DSf = A.f32(264)
        DS = DSf.rearrange("p (j h) -> p j h", h=8)
        DH = A.bf16(264)
        DL = A.bf16(264)
        TRB = v3(A.bf16(3 * 128), 3)
        for r in range(2):
            dma("sp", LFf.rearrange("p (m r x) -> p m r x", m=NM, r=2)[:, :, r, :],
                lf_all[r * NM * 128:(r + 1) * NM * 128, :].rearrange("(m p) x -> p m x", p=128),
                ["lf_all"], [("LF", r)])
        lk = [("LF", 0), ("LF", 1)]
        memset("dve", ONE, 1.0, ["ONE"])
        FTf = FT.rearrange("p g h -> p (g h)")
        CYf = CY.rearrange("p g h -> p (g h)")
        for half in range(2):
            cs = slice(half * 264, (half + 1) * 264)
            mm(ps[half][:, 0:264], cst[:, C_U:C_U + 128], LFf[:, cs], True, True, lk + ["cst"], [psk(half)])
            mm(ps[2 + half][:, 0:264], cst[:, C_ONES:C_ONES + 128], LFf[:, cs], True, True, lk + ["cst"],
               [psk(2 + half)])
            cp("act", FTf[:, cs], ps[half][:, 0:264], [psk(half)], [("FTw", half)])
            cp("dve", TT[:, cs], ps[2 + half][:, 0:264], [psk(2 + half)], [("TT", half)])
        TT3 = TT.rearrange("p (g h) -> p g h", h=8)
        INC3 = INC.rearrange("p (g h) -> p g h", h=8)
        for h in range(8):
            P.add("dve", lambda e, h=h: e.tensor_tensor_scan(INC3[:, :, h], ONE, TT3[:, :, h], 0.0, ALU.mult, ALU.add),
                  [("TT", 0), ("TT", 1), "ONE"], [("INC", h)])
        ik = [("INC", h) for h in range(8)]
        tt("dve", CYf, INC, TT, ALU.subtract, ik + [("TT", 0), ("TT", 1)], ["CY"])
        tt("dve", FTf, FTf, CYf, ALU.add, [("FTw", 0), ("FTw", 1), "CY"], ["FT"])
        CY4 = CY.rearrange("p (m r i) h -> p m r i h", r=2, i=3)
        FT4 = FT.rearrange("p (m r i) h -> p m r i h", r=2, i=3)
        ts("dve", CS, CY4[:, :, 0, 0, :], selA, None, ALU.mult, None, ["CY", "cst"], ["CS"])
        stt(CS, CY4[:, :, 1, 0, :], selB, CS, ALU.mult, ALU.add, ["CY", "cst", "CS"], ["CS"])
        DS4 = DS.rearrange("p (m i) h -> p m i h", i=3)
        for i in range(3):
            ts("dve", DS4[:, :, i, :], FT4[:, :, 0, i, :], selA, None, ALU.mult, None, ["FT", "cst"], [("DS", i)])
            stt(DS4[:, :, i, :], FT4[:, :, 1, i, :], selB, DS4[:, :, i, :], ALU.mult, ALU.add,
                ["FT", "cst", ("DS", i)], [("DS", i)])
            tt("dve", DS4[:, :, i, :], DS4[:, :, i, :], CS, ALU.subtract, [("DS", i), "CS"], [("DS", i)])
        dk = [("DS", i) for i in range(3)]
        cp("dve", DH, DSf, dk, ["DH"])
        tt("dve", DSf, DSf, DH, ALU.subtract, dk + ["DH"], ["DS2"] + dk)
        cp("dve", DL, DSf, ["DS2"], ["DL"])
        for which, src in ((0, DH), (1, DL)):
            for a, (c0, n) in enumerate(((0, 128), (128, 128), (256, 8))):
                P.add("pe", lambda e, a=a, c0=c0, n=n, src=src: e.transpose(psT[0:n, a * 128:(a + 1) * 128],
                                                                             src[:, c0:c0 + n], ident[:, :]),
                      ["DH", "DL", "ident"], ["psT"])
            cp("act", TRB[:, :, :], psT[:, 0:384].rearrange("p (a t) -> p a t", a=3), ["psT"], ["TRB"])
            for a, nj in ((0, 16), (1, 16), (2, 1)):
                for jj in range(nj):
                    j = a * 16 + jj
                    dma("sp", qx[:, 64 + which, j * 128:(j + 1) * 128],
                        TRB[jj * 8:(jj + 1) * 8, a, :], ["TRB"], [("qxaug", which, j)])
        if "F" in dbg_out:
            dma("sp", dbg_out["F"][:, :], FTf, ["FT"], ["dbgF"])

    def phase2b():
        A = Arena()
        QH = [A.bf16(OWN, parts=66) for _ in range(2)]
        KT = [A.bf16(NG * CH, parts=66).rearrange("p (g t) -> p g t", t=CH) for _ in range(2)]
        VT = [A.bf16(NGT * 65).rearrange("p (m r i d) -> p m r i d", r=2, i=3, d=65) for _ in range(2)]
        PT = [A.bf16(CH) for _ in range(10)]
        MT = [A.f32(CH) for _ in range(2)]
        OS2 = [A.f32(CH) for _ in range(2)]
        RC2 = [A.f32(CH) for _ in range(2)]
        OB = [A.bf16(CH) for _ in range(2)]
        BI3 = [A.f32(72) for _ in range(3)]
        qk = [("qx", m, h_) for m in range(NM) for h_ in range(8)] + \
            [("qxaug", w, j) for w in range(2) for j in range(NOWNT)]
        for i in range(2):
            memset("pool", KT[i][64:66, :, :], 1.0, [("KTones", i)])

        def load_kv(h, hb):
            dma("act", QH[hb][0:66, :], qx[h, :, :], qk, [("QH", hb)])
            for r in range(2):
                for p in range(NPC):
                    n = PCN[p]
                    dma("act", KT[hb][0:64, :, :].rearrange("p (m r) t -> p m r t", r=2)[:, 3 * p:3 * p + n, r, :],
                        kx_all[p][r * 512 + h * 64:r * 512 + (h + 1) * 64, :].rearrange("p (m t) -> p m t", t=CH),
                        [("kx_all", p)], [("KT", hb, r, p)])
                    for i in range(3):
                        dma("act", VT[hb][:, 3 * p:3 * p + n, r, i, :],
                            vx_all[p][r * n * CH:(r + 1) * n * CH, h * 65:(h + 1) * 65].rearrange(
                                "(m i p) d -> p m i d", i=3, p=128)[:, :, i, :],
                            [("vx_all", p)], [("VT", hb, r, p, i)])
        load_kv(0, 0)
        LAG = 4
        items = []
        for h in range(8):
            for m in range(NM):
                for gt in range(6 * m + 6):
                    items.append((h, m, gt))
        state = {}
        pend = {}
        NPT = len(PT)

        def s_stage(idx):
            h, m, gt = items[idx]
            hb = h % 2
            it = h * NM + m
            bi = it % 3
            if gt == 0:
                ts("dve", BI3[bi][:, 0:66], FT[:, :, h], -1.0, CS[:, m, h:h + 1], ALU.mult, ALU.add, ["FT", "CS"],
                   [("BI", bi)])
            kvk = [("KTones", hb), ("QH", hb)] + [("KT", hb, r, p) for r in range(2) for p in range(NPC)]
            G, kt = gt // 3, gt % 3
            band = G - 2 * m
            qlo = kt * 128 if band == 1 else 0
            ncol = CH - qlo
            q0 = m * CH
            b = nbank()
            mm(ps[b][:, 0:ncol], KT[hb][0:66, G, kt * 128:(kt + 1) * 128], QH[hb][0:66, q0 + qlo:q0 + CH],
               True, True, kvk, [psk(b)])
            pt = PT[idx % NPT]
            pk = ("PT", idx % NPT)
            if band >= 0:
                mt = MT[idx % 2]
                mk = ("MT", idx % 2)
                tt("dve", mt[:, 0:ncol], ps[b][:, 0:ncol], maskap(band, kt, qlo), ALU.add, [psk(b), "cst"], [mk])
                act(pt[:, 0:ncol], mt[:, 0:ncol], AF.Exp, [mk, ("BI", bi)], [pk], bias=BI3[bi][:, gt:gt + 1])
            else:
                act(pt[:, 0:ncol], ps[b][:, 0:ncol], AF.Exp, [psk(b), ("BI", bi)], [pk],
                    bias=BI3[bi][:, gt:gt + 1])
            state[idx] = (pt, pk, qlo, ncol)

        def pv_stage(idx, step):
            h, m, gt = items[idx]
            hb = h % 2
            it = h * NM + m
            ob = 5 + it % 2
            ngt = 6 * m + 6
            pt, pk, qlo, ncol = state.pop(idx)
            if gt == 0 and m == 0 and h + 1 < 8:
                load_kv(h + 1, 1 - hb)
            vk = [("VT", hb, r, p, i) for r in range(2) for p in range(NPC) for i in range(3)]
            Vt = VT[hb].rearrange("p m r i d -> p (m r i) d")
            mm(ps[ob][0:65, qlo:CH], Vt[:, gt, 0:65], pt[:, 0:ncol], gt == 0, gt == ngt - 1, vk + [pk], [psk(ob)])
            if gt == ngt - 1:
                ri = it % 2
                P.add("dve", lambda e: e.reciprocal(RC2[ri][64:65, :], ps[ob][64:65, 0:CH]), [psk(ob)], [("RC", ri)])
                cp("act", OS2[ri][0:64, :], ps[ob][0:64, 0:CH], [psk(ob)], [("OS", ri)])
                pend.setdefault(step + 2, []).append((h, m, ri))

        def norm2(h, m, ri):
            q0 = m * CH
            b = nbank()
            mm(ps[b][0:64, 0:CH], cst[64:65, C_ONES:C_ONES + 64], RC2[ri][64:65, :], True, True, [("RC", ri), "cst"],
               [psk(b)])
            obt = OB[ri]
            tt("dve", obt[0:64, :], OS2[ri][0:64, :], ps[b][0:64, 0:CH], ALU.mult, [("OS", ri), psk(b)], [("OB", ri)])
            dma("sp", ox[h * 64:(h + 1) * 64, q0:q0 + CH], obt[0:64, :], [("OB", ri)], [("ox", h, m)])

        nit = len(items)
        for step in range(nit + LAG + 3):
            if step < nit:
                s_stage(step)
            if 0 <= step - LAG < nit:
                pv_stage(step - LAG, step)
            for a in pend.pop(step, []):
                norm2(*a)
        assert not pend and not state

    def phase3a(l, hsrc):
        A = Arena()
        WG = v3(A.bf16(8 * 3072), 8)
        WOA = v3(A.bf16(2 * 1024), 2)
        WOB = v3(A.bf16(2 * 1024), 2)
        WOC = v3(A.bf16(4 * 1024), 4)
        WO = v3(A.bf16(8 * 1024), 8)
        BD = v3(A.bf16(256), 2)
        DG = A.bf16(62 * 128).rearrange("p (j d) -> p j d", d=128)
        htile = v3(A.f32(3 * D), 3)
        U2 = [v3(A.bf16(8 * CH), 8) for _ in range(2)]
        XA = v3(A.f32(800), 2)
        S2 = v3(A.f32(800), 2)
        S4 = v3(A.f32(800), 2)
        YC = v3(A.f32(768), 2)
        YQ = v3(A.f32(768), 2)
        MEAN = A.f32(CH)
        RSTD = A.f32(CH)
        T1 = A.f32(CH)
        T2 = A.f32(CH)
        T3 = A.f32(CH)
        HA = v3(A.f32(64), 2)
        SGB = [A.f32(CH) for _ in range(3)]
        XG = v3(A.bf16(832), 2)
        DF = v3(A.bf16(768), 2)
        YP = v3(A.bf16(768), 2)
        YB = v3(A.bf16(768), 2)
        HB0 = v3(A.bf16(128), 2)
        HB1 = v3(A.bf16(128), 2)
        MG = v3(A.bf16(8 * CH), 8)
        AB = v3(A.bf16(2 * CH), 2)
        OC = v3(A.bf16(4 * CH), 4)
        pbs = small[:, 40:42]
        for g3 in range(3):
            dma("pool", WG[:, :, g3 * 1024:(g3 + 1) * 1024],
                w_in[l, :, 2312 + g3 * 1024:2312 + (g3 + 1) * 1024].rearrange("(k p) c -> p k c", p=128),
                (), [("WG", g3)])
        dma("pool", WOA, w_oa[l, :, :].rearrange("(k p) c -> p k c", p=128), (), ["WOA"])
        dma("pool", WOB, w_ob[l, :, :].rearrange("(k p) c -> p k c", p=128), (), ["WOB"])
        dma("pool", WOC, w_oc[l, :, :].rearrange("(k p) c -> p k c", p=128), (), ["WOC"])
        dma("pool", WO, w_o[l, :, :].rearrange("(k p) c -> p k c", p=128), (), ["WO"])
        memset("dve", BD, 0.0, ["BD0"])
        for g in range(4):
            r0 = (g % 2) * 64
            dma("pool", BD[r0:r0 + 64, g // 2, r0:r0 + 64], pool_w[l, g, :, :], ["BD0"], [("BD", g)])
        bdk = [("BD", g) for g in range(4)]
        for cc in range(2):
            for j in range(31):
                ts("dve", DG[:, cc * 31 + j, :], ident[:, :], chp[:, CP_CW + cc * 31 + j:CP_CW + cc * 31 + j + 1],
                   None, ALU.mult, None, ["ident", ("chp", l)], [("DG", cc)])
        tt("dve", pbs, chp[:, CP_PB:CP_PB + 2], chp[:, CP_PS:CP_PS + 2], ALU.mult, [("chp", l)], ["pbs"])
        wgk = [("WG", g3) for g3 in range(3)]
        YP2 = [YP, v3(A.bf16(768), 2)]
        YB2 = [YB, v3(A.bf16(768), 2)]
        OC2 = [OC, v3(A.bf16(4 * CH), 4)]

        def prep(m, s_):
            YP_, YB_, OC_ = YP2[s_], YB2[s_], OC2[s_]
            osl = slice(m * CH, (m + 1) * CH)
            pp, pm = pc_of(m)
            psl = slice(pm * CH, (pm + 1) * CH)
            dma("sp", XG[:, :, 32:416], ag_loc[pp][256:512, psl].rearrange("(c p) t -> p c t", p=128),
                [("ag_loc", m, 1)], ["XGo"])
            dma("sp", AB, ag_loc[pp][0:256, psl].rearrange("(c p) t -> p c t", p=128),
                [("ag_loc", m, 0)], ["ABo"])
            dma("sp", OC_, ox[:, osl].rearrange("(k p) t -> p k t", p=128),
                [("ox", h, m) for h in range(8)], [("OC", s_)])
            if m > 0:
                qp, qm = pc_of(m - 1)
                e0 = (qm + 1) * CH
                dma("sp", HB0[:, :, 0:32],
                    ag_all[qp][768:1024, e0 - 32:e0].rearrange("(c p) t -> p c t", p=128),
                    [("ag_all", qp)], ["HB0g"])
                dma("sp", HB0[:, :, 32:48],
                    ag_all[qp][512:768, e0 - 16:e0].rearrange("(c p) t -> p c t", p=128),
                    [("ag_all", qp)], ["HB0a"])
            else:
                memset("pool", HB0, 0.0, ["HB0g", "HB0a"])
            e1 = (pm + 1) * CH
            dma("sp", HB1[:, :, 0:32],
                ag_all[pp][256:512, e1 - 32:e1].rearrange("(c p) t -> p c t", p=128),
                [("ag_all", pp)], ["HB1g"])
            dma("sp", HB1[:, :, 32:48],
                ag_all[pp][0:256, e1 - 16:e1].rearrange("(c p) t -> p c t", p=128),
                [("ag_all", pp)], ["HB1a"])
            hk = ["HB0g", "HB0a", "HB1g", "HB1a"]
            ts("dve", HA[:, :, 0:32], HB0[:, :, 0:32], selA, None, ALU.mult, None, hk + ["cst"], ["HAg"])
            stt(XG[:, :, 0:32], HB1[:, :, 0:32], selB, HA[:, :, 0:32], ALU.mult, ALU.add, hk + ["cst", "HAg"], ["XGh"])
            ts("dve", HA[:, :, 0:16], HB0[:, :, 32:48], selA, None, ALU.mult, None, hk + ["cst", "XGh"], ["HAa"])
            stt(XA[:, :, 0:16], HB1[:, :, 32:48], selB, HA[:, :, 0:16], ALU.mult, ALU.add, hk + ["cst", "HAa"], ["XAh"])
            cp("dve", XA[:, :, 16:400], AB, ["ABo"], ["XAo"])
            xak = ["XAh", "XAo"]
            tt("dve", S2[:, :, 1:400], XA[:, :, 1:400], XA[:, :, 0:399], ALU.add, xak, ["S2"])
            tt("dve", S4[:, :, 3:400], S2[:, :, 3:400], S2[:, :, 1:398], ALU.add, ["S2"], ["S4"])
            tt("dve", S2[:, 1, 7:400], S4[:, 1, 7:400], S4[:, 1, 3:396], ALU.add, ["S4", "S2"], ["S8", "S2"])
            tt("dve", S4[64:128, 1, 15:400], S2[64:128, 1, 15:400], S2[64:128, 1, 7:392], ALU.add, ["S8", "S4"],
               ["S16", "S4"])
            ico = C_INVC + (0 if m == 0 else 768)
            IC = cst[:, ico:ico + 768].rearrange("p (c t) -> p c t", c=2)
            wk = ["S2", "S4", "S8", "S16", "cst"]
            PT1 = YC[:, 0, :]
            for cc in range(2):
                tt("dve", PT1[0:64, :], S2[0:64, cc, 16:400], IC[0:64, cc, :], ALU.mult, wk, [("YC", 0)])
                tt("dve", PT1[64:128, :], S4[64:128, cc, 16:400], IC[64:128, cc, :], ALU.mult, wk + [("YC", 0)],
                   [("YC", 0)])
                tt("dve", DF[:, cc, :], PT1, XA[:, cc, 16:400], ALU.subtract, [("YC", 0)] + xak, [("DF", cc)])
            yield
            for cc in range(2):
                b = nbank(0, 7)
                mm(ps[b][:, 0:CH], BD[:, cc, :], DF[:, cc, :], True, True, bdk + [("DF", cc)], [psk(b)])
                act(YP_[:, cc, :], ps[b][:, 0:CH], AF.Identity, [psk(b), ("chp", l), "pbs"], [("YP", s_, cc)],
                    bias=pbs[:, cc:cc + 1], scale=chp[:, CP_PS + cc:CP_PS + cc + 1])
            for cc in range(2):
                b = nbank(0, 7)
                for j in range(31):
                    mm(ps[b][:, 0:CH], DG[:, cc * 31 + j, :], XG[:, cc, 2 + j:2 + j + CH], j == 0, j == 30,
                       [("DG", 0), ("DG", 1), "XGo", "XGh"], [psk(b)])
                act(YC[:, cc, :], ps[b][:, 0:CH], AF.Identity, [psk(b), ("chp", l)], [("YC", cc)],
                    bias=chp[:, CP_CB + cc:CP_CB + cc + 1])
                act(YQ[:, cc, :], YC[:, cc, :], AF.Square, [("YC", cc)], [("YQ", cc)])
            bm = nbank(0, 7)
            bq = nbank(0, 7)
            for cc in range(2):
                mm(ps[bm][:, 0:CH], cst[:, C_ODIV:C_ODIV + 128], YC[:, cc, :], cc == 0, cc == 1, ["cst", ("YC", cc)],
                   [psk(bm)])
            for cc in range(2):
                mm(ps[bq][:, 0:CH], cst[:, C_ODIV:C_ODIV + 128], YQ[:, cc, :], cc == 0, cc == 1, ["cst", ("YQ", cc)],
                   [psk(bq)])
            PT2 = YQ[:, 0, :]
            PT3 = YQ[:, 1, :]
            cp("act", MEAN, ps[bm][:, 0:CH], [psk(bm)], ["MEAN"])
            tt("dve", PT2, MEAN, MEAN, ALU.mult, ["MEAN"], [("YQ", 0)])
            tt("dve", RSTD, ps[bq][:, 0:CH], PT2, ALU.subtract, [psk(bq), ("YQ", 0)], ["RSTD"])
            rsqrt(RSTD, RSTD, 1.0, ["RSTD"], ["RSTD"])
            for cc in range(2):
                tt("dve", PT3, YC[:, cc, :], MEAN, ALU.subtract, [("YC", cc), "MEAN"], [("YQ", 1)])
                tt("dve", PT3, PT3, RSTD, ALU.mult, [("YQ", 1), "RSTD"], [("YQ", 1)])
                act(YB_[:, cc, :], PT3, AF.Silu, [("YQ", 1), ("chp", l)], [("YB", s_, cc)],
                    bias=chp[:, CP_LB + cc:CP_LB + cc + 1], scale=chp[:, CP_LG + cc:CP_LG + cc + 1])

        for _ in prep(0, 0):
            pass
        for m in range(NM):
            s_ = m % 2
            YP_, YB_, OC_ = YP2[s_], YB2[s_], OC2[s_]
            if m == 0:
                dma("sp", U2[0].rearrange("p k t -> p (k t)"), ux[0, :, :], [("ux", 0)], [("U3", 0)])
            if m + 1 < NM:
                dma("sp", U2[(m + 1) % 2].rearrange("p k t -> p (k t)"), ux[m + 1, :, :], [("ux", m + 1)],
                    [("U3", (m + 1) % 2)])
            load_h(hsrc, m, htile, "ht3")
            ukey = ("U3", m % 2)
            U = U2[m % 2]
            pg = prep(m + 1, 1 - s_) if m + 1 < NM else iter(())
            for f in range(8):
                if f == 1 or f == 4:
                    next(pg, None)
                for br in range(3):
                    b = nbank(0, 7)
                    c0 = br * 1024 + f * 128
                    for k in range(8):
                        mm(ps[b][:, 0:CH], WG[:, k, c0:c0 + 128], U[:, k, :], k == 0, k == 7, wgk + [ukey], [psk(b)])
                    act(SGB[br], ps[b][:, 0:CH], AF.Sigmoid, [psk(b)], [("SGB", br)])
                fs = slice(f * 128, (f + 1) * 128)
                ba = nbank(0, 7)
                for cc in range(2):
                    mm(ps[ba][:, 0:CH], WOA[:, cc, fs], YP_[:, cc, :], cc == 0, cc == 1,
                       ["WOA", ("YP", s_, 0), ("YP", s_, 1)], [psk(ba)])
                tt("dve", T1, ps[ba][:, 0:CH], SGB[0], ALU.mult, [psk(ba), ("SGB", 0)], ["T1"])
                bb = nbank(0, 7)
                for cc in range(2):
                    mm(ps[bb][:, 0:CH], WOB[:, cc, fs], YB_[:, cc, :], cc == 0, cc == 1,
                       ["WOB", ("YB", s_, 0), ("YB", s_, 1)], [psk(bb)])
                tt("dve", T2, ps[bb][:, 0:CH], SGB[1], ALU.mult, [psk(bb), ("SGB", 1)], ["T2"])
                bc = nbank(0, 7)
                for k in range(4):
                    mm(ps[bc][:, 0:CH], WOC[:, k, fs], OC_[:, k, :], k == 0, k == 3, ["WOC", ("OC", s_)], [psk(bc)])
                tt("dve", T3, ps[bc][:, 0:CH], SGB[2], ALU.mult, [psk(bc), ("SGB", 2)], ["T3"])
                tt("pool", T1, T1, T2, ALU.add, ["T1", "T2"], ["T1"])
                tt("pool", MG[:, f, :], T1, T3, ALU.add, ["T1", "T3"], [("MG", f)])
            for _ in pg:
                pass
            mgk = [("MG", f) for f in range(8)]
            for i in range(3):
                for half in range(2):
                    b = nbank(0, 7)
                    for k in range(8):
                        mm(ps[b][:, 0:512], MG[:, k, i * 128:(i + 1) * 128], WO[:, k, half * 512:(half + 1) * 512],
                           k == 0, k == 7, mgk + ["WO"], [psk(b)])
                    tt("dve", htile[:, i, half * 512:(half + 1) * 512], htile[:, i, half * 512:(half + 1) * 512],
                       ps[b][:, 0:512], ALU.add, ["ht3", psk(b)], ["ht3"])
            dma("sp", hbuf[m * CH:(m + 1) * CH, :].rearrange("(i p) d -> p i d", p=128), htile,
                ["ht3"], [("hsrc", m)])

    def phase3b(l, last):
        SCS = [list(range(0, 6)), list(range(6, NM))]
        A = Arena()
        ACCT = A.f32(18 * D).rearrange("p (t d) -> p t d", d=D)
        VTT = A.bf16(6 * 8 * CH).rearrange("p (s k t) -> p s k t", s=6, k=8)
        WS = []
        for s in range(2):
            WS.append((v3(A.bf16(2048), 8), v3(A.bf16(2048), 8), v3(A.bf16(2048), 2)))
        RW = v3(A.bf16(160), 8)
        ubf = A.bf16(D)
        LG = A.f32(20)
        GM = A.f32(4)
        OH = A.f32(4)
        EX = A.f32(4)
        ES = v3(A.f32(16), 4)
        EL = A.f32(4)
        M1 = A.f32(4)
        M2 = A.f32(4)
        E2 = A.f32(4)
        SC1 = A.f32(8)
        WGp = A.f32(4)
        CMB = A.f32(18 * 16).rearrange("p (t e) -> p t e", e=16)
        S1 = [A.f32(CH) for _ in range(2)]
        HE = A.bf16(4 * CH).rearrange("p (s c t) -> p s c t", s=2, c=2)
        OT = A.f32(D)
        dma("pool", RW, rw_d[l, :, :].rearrange("(k p) c -> p k c", p=128), (), ["RW"])
        ecount = 0
        for sc in SCS:
            for si, m in enumerate(sc):
                for i in range(3):
                    t = si * 3 + i
                    dma("sp", ACCT[:, t, :], hbuf[m * CH + i * 128:m * CH + (i + 1) * 128, :], [("hsrc", m)],
                        [("ACC", t)])
            for si, m in enumerate(sc):
                for i in range(3):
                    t = si * 3 + i
                    ss = small[:, 48:49]
                    rs = small[:, 49:50]
                    stt(ubf, ACCT[:, t, :], 1.0, ACCT[:, t, :], ALU.mult, ALU.mult, [("ACC", t)], ["ubf", "ss2"],
                        accum=ss)
                    rsqrt(rs, ss, 1.0 / D, ["ss2"], ["rs2"])
                    stt(ubf, ACCT[:, t, :], rs, vecs[:, V_G2:V_G2 + D], ALU.mult, ALU.mult,
                        [("ACC", t), "rs2", ("vecs", l)], ["ubf"])
                    for k in range(8):
                        P.add("pe", lambda e, k=k: e.transpose(psT[:, k * 128:(k + 1) * 128],
                                                               ubf[:, k * 128:(k + 1) * 128], ident[:, :]),
                              ["ubf", "ident"], ["psT"])
                    cp("act", VTT[:, si, :, i * 128:(i + 1) * 128], psT[:, :].rearrange("p (k t) -> p k t", k=8),
                       ["psT"], [("VTT", si)])
                    b = nbank(0, 7)
                    for k in range(8):
                        mm(ps[b][:, 0:20], VTT[:, si, k, i * 128:(i + 1) * 128], RW[:, k, :], k == 0, k == 7,
                           [("VTT", si), "RW"], [psk(b)])
                    tt("dve", LG, ps[b][:, 0:20], vecs[:, V_RB:V_RB + 20], ALU.add, [psk(b), ("vecs", l)], ["LG"])
                    AXX = mybir.AxisListType.X
                    P.add("dve", lambda e: e.reduce_max(GM[:, 0:1], LG[:, 0:4], AXX), ["LG"], ["GM"])
                    ts("dve", OH, LG[:, 0:4], GM[:, 0:1], None, ALU.is_equal, None, ["LG", "GM"], ["OH"])
                    ts("dve", EX, LG[:, 0:4], GM[:, 0:1], None, ALU.subtract, None, ["LG", "GM"], ["EX"])
                    act(EX, EX, AF.Exp, ["EX"], ["EX"])
                    P.add("dve", lambda e: e.reduce_sum(GM[:, 1:2], EX, AXX), ["EX", "GM"], ["GS"])
                    P.add("dve", lambda e: e.reciprocal(GM[:, 2:3], GM[:, 1:2]), ["GS"], ["GW"])
                    LE = LG[:, 4:20].rearrange("p (g e) -> p g e", g=4)
                    for g in range(4):
                        ts("dve", ES[:, g, :], LE[:, g, :], OH[:, g:g + 1], None, ALU.mult, None, ["LG", "OH"],
                           [("ES", g)])
                    esk = [("ES", g) for g in range(4)]
                    tt("dve", ES[:, 0, :], ES[:, 0, :], ES[:, 1, :], ALU.add, esk, [("ES", 0)])
                    tt("dve", ES[:, 2, :], ES[:, 2, :], ES[:, 3, :], ALU.add, esk, [("ES", 2)])
                    tt("dve", EL, ES[:, 0, :], ES[:, 2, :], ALU.add, [("ES", 0), ("ES", 2)], ["EL"])
                    P.add("dve", lambda e: e.reduce_max(SC1[:, 0:1], EL, AXX), ["EL"], ["m1"])
                    ts("dve", M1, EL, SC1[:, 0:1], None, ALU.is_equal, None, ["EL", "m1"], ["M1"])
                    stt(E2, M1, NEG, EL, ALU.mult, ALU.add, ["M1", "EL"], ["E2"])
                    P.add("dve", lambda e: e.reduce_max(SC1[:, 1:2], E2, AXX), ["E2"], ["m2"])
                    ts("dve", M2, E2, SC1[:, 1:2], None, ALU.is_equal, None, ["E2", "m2"], ["M2"])
                    tt("dve", SC1[:, 2:3], SC1[:, 0:1], SC1[:, 1:2], ALU.subtract, ["m1", "m2"], ["dm"])
                    act(SC1[:, 3:4], SC1[:, 2:3], AF.Sigmoid, ["dm"], ["p1"])
                    ts("dve", SC1[:, 4:5], SC1[:, 3:4], -1.0, 1.0, ALU.mult, ALU.add, ["p1"], ["p2"])
                    tt("dve", SC1[:, 3:4], SC1[:, 3:4], GM[:, 2:3], ALU.mult, ["p1", "GW", "p2"], ["p1g"])
                    tt("dve", SC1[:, 4:5], SC1[:, 4:5], GM[:, 2:3], ALU.mult, ["p2", "GW"], ["p2g"])
                    ts("dve", WGp, M1, SC1[:, 3:4], None, ALU.mult, None, ["M1", "p1g"], ["WGp"])
                    stt(WGp, M2, SC1[:, 4:5], WGp, ALU.mult, ALU.add, ["M2", "p2g", "WGp"], ["WGp"])
                    for g in range(4):
                        ts("dve", CMB[:, t, g * 4:(g + 1) * 4], WGp, OH[:, g:g + 1], None, ALU.mult, None,
                           ["WGp", "OH"], [("CMB", t)])

            def load_expert(e, s):
                w1, w3, w2 = WS[s]
                dma("pool", w1, ew1[l, e, :, :].rearrange("(k p) c -> p k c", p=128), (), [("EW1", s)])
                dma("pool", w3, ew3[l, e, :, :].rearrange("(k p) c -> p k c", p=128), (), [("EW3", s)])
                dma("pool", w2, ew2[l, e, :, :].rearrange("(k p) c -> p k c", p=128), (), [("EW2", s)])
            load_expert(0, ecount % 2)
            for e in range(16):
                s = ecount % 2
                ecount += 1
                if e + 1 < 16:
                    load_expert(e + 1, 1 - s)
                w1, w3, w2 = WS[s]
                for si, m in enumerate(sc):
                    hs = si % 2
                    for hc in range(2):
                        b1 = nbank(0, 7)
                        for k in range(8):
                            mm(ps[b1][:, 0:CH], w1[:, k, hc * 128:(hc + 1) * 128], VTT[:, si, k, :], k == 0, k == 7,
                               [("EW1", s), ("VTT", si)], [psk(b1)])
                        b3 = nbank(0, 7)
                        for k in range(8):
                            mm(ps[b3][:, 0:CH], w3[:, k, hc * 128:(hc + 1) * 128], VTT[:, si, k, :], k == 0, k == 7,
                               [("EW3", s), ("VTT", si)], [psk(b3)])
                        act(S1[hc], ps[b1][:, 0:CH], AF.Silu, [psk(b1)], [("S1", hc)])
                        tt("dve", HE[:, hs, hc, :], S1[hc], ps[b3][:, 0:CH], ALU.mult, [("S1", hc), psk(b3)],
                           [("HE", hs, hc)])
                    for i in range(3):
                        t = si * 3 + i
                        for half in range(2):
                            b = nbank(0, 7)
                            for hc in range(2):
                                mm(ps[b][:, 0:512], HE[:, hs, hc, i * 128:(i + 1) * 128],
                                   w2[:, hc, half * 512:(half + 1) * 512], hc == 0, hc == 1,
                                   [("HE", hs, 0), ("HE", hs, 1), ("EW2", s)], [psk(b)])
                            stt(ACCT[:, t, half * 512:(half + 1) * 512], ps[b][:, 0:512], CMB[:, t, e:e + 1],
                                ACCT[:, t, half * 512:(half + 1) * 512], ALU.mult, ALU.add,
                                [psk(b), ("CMB", t), ("ACC", t)], [("ACC", t)])
            for si, m in enumerate(sc):
                for i in range(3):
                    t = si * 3 + i
                    r0 = m * CH + i * 128
                    if not last:
                        dma("sp", hbuf[r0:r0 + 128, :], ACCT[:, t, :], [("ACC", t)], [("hsrc", m), ("hw", m, i)])
                    else:
                        ss = small[:, 52:53]
                        rs = small[:, 53:54]
                        stt(OT, ACCT[:, t, :], 1.0, ACCT[:, t, :], ALU.mult, ALU.mult, [("ACC", t)], ["OT", "ss3"],
                            accum=ss)
                        rsqrt(rs, ss, 1.0 / D, ["ss3"], ["rs3"])
                        stt(OT, ACCT[:, t, :], rs, fing[:, :], ALU.mult, ALU.mult, [("ACC", t), "rs3", "fing"], ["OT"])
                        dma("sp", out_d[r0:r0 + 128, :], OT, ["OT"], [("out", m, i)])

    def phase3b_sparse(l, last):
        A = Arena()
        AXX = mybir.AxisListType.X
        NT = NOWNT
        VBF = A.bf16(NT * D).rearrange("p (t d) -> p t d", d=D)
        INDb = A.bf16(NT * 16)
        M1G = A.f32(NT * 16)
        M2G = A.f32(NT * 16)
        RIN = A.f32(NT * 16)
        TOT = A.f32(NT * 16)
        INC = A.f32(NT * 16)
        RB = A.f32(NT * 16)
        PRD = A.f32(NT * 16)
        PW = A.f32(NT * 2).rearrange("p (t j) -> p t j", j=2)
        SLF = [A.f32(NT) for _ in range(2)]
        SLI = [AR[:, A.off + i * 40:A.off + i * 40 + NT].bitcast(I32) for i in range(2)]
        A.off += 80
        CNT = A.f32(16)
        NTL = A.f32(16)
        PC = A.f32(16)
        BEND = A.f32(16)
        BASE = A.f32(16)
        ONE16 = A.f32(16)
        ONE33 = A.f32(NT)
        TMP33 = A.f32(NT)
        TE = A.f32(NSLT)
        WIF = A.f32(NSLT)
        WII = AR[:, A.off:A.off + NSLT].bitcast(I32)
        A.off += 88
        UST = A.bf16(128)
        ONEb = A.bf16(128)
        RW = v3(A.bf16(160), 8)
        ubfx = A.bf16(D)
        HT = [A.f32(D) for _ in range(3)]
        NRS = 3
        RSET = [dict(VTt=v3(A.bf16(8 * 128), 8), LG=A.f32(20), GM=A.f32(4), OH=A.f32(4), EX=A.f32(4),
                     ES=v3(A.f32(16), 4), EL=A.f32(4), M1=A.f32(4), M2=A.f32(4), E2=A.f32(4), SC1=A.f32(8),
                     ss=A.f32(1), rs=A.f32(1)) for _ in range(NRS)]
        NWB = 3
        WB = [(A.bf16(2048), A.bf16(2048), A.bf16(2048)) for _ in range(NWB)]
        XI = [A.bf16(D) for _ in range(3)]
        XT = [v3(A.bf16(8 * 128), 8) for _ in range(2)]
        S1 = A.f32(256)
        HEb = [A.bf16(256) for _ in range(2)]
        YT = [A.bf16(D) for _ in range(2)]
        OTf = A.f32(D)
        Y1 = [WB[hb_][0][:, 0:D] for hb_ in range(2)]
        Y2 = [WB[hb_][1][:, 0:D] for hb_ in range(2)]
        OT = OTf
        M1G3 = M1G.rearrange("p (t e) -> p t e", e=16)
        M2G3 = M2G.rearrange("p (t e) -> p t e", e=16)
        IND3 = INDb.rearrange("p (t e) -> p t e", e=16)
        dma("pool", RW, rw_d[l, :, :].rearrange("(k p) c -> p k c", p=128), (), ["RW"])
        tt("dve", UST, cst[:, C_U:C_U + 128], cst[:, C_ID:C_ID + 128], ALU.subtract, ["cst"], ["UST"])
        cp("dve", ONEb, cst[:, C_ONES:C_ONES + 128], ["cst"], ["ONEb"])
        memset("dve", ONE16, 1.0, ["ONE16"])
        memset("dve", ONE33, 1.0, ["ONE33"])
        def router_tile(t):
            R_ = RSET[t % NRS]
            q = t % NRS
            VTt, LG, GM, OH, EX, ES, EL, M1, M2, E2, SC1 = (R_[k] for k in
                                                            ("VTt", "LG", "GM", "OH", "EX", "ES", "EL", "M1", "M2", "E2", "SC1"))
            K = lambda nm: (nm, q)
            m, i = t // 3, t % 3
            hb = t % 3
            r0 = m * CH + i * 128
            dma("sp", HT[hb], hbuf[r0:r0 + 128, :], [("hsrc", m)], [("HT", hb)])
            ss = R_["ss"]
            rs = R_["rs"]
            stt(ubfx, HT[hb], 1.0, HT[hb], ALU.mult, ALU.mult, [("HT", hb)], ["ubfx", K("ss2")], accum=ss)
            yield
            rsqrt(rs, ss, 1.0 / D, [K("ss2")], [K("rs2")])
            yield
            stt(VBF[:, t, :], HT[hb], rs, vecs[:, V_G2:V_G2 + D], ALU.mult, ALU.mult,
                [("HT", hb), K("rs2"), ("vecs", l)], [("VBF", t)])
            for k in range(8):
                P.add("pe", lambda e, k=k, t=t: e.transpose(psT[:, k * 128:(k + 1) * 128],
                                                            VBF[:, t, k * 128:(k + 1) * 128], ident[:, :]),
                      [("VBF", t), "ident"], ["psT"])
            cp("act", VTt, psT[:, :].rearrange("p (k t) -> p k t", k=8), ["psT"], [K("VTt")])
            b = nbank(0, 7)
            for k in range(8):
                mm(ps[b][:, 0:20], VTt[:, k, :], RW[:, k, :], k == 0, k == 7, [K("VTt"), "RW"], [psk(b)])
            yield
            tt("dve", LG, ps[b][:, 0:20], vecs[:, V_RB:V_RB + 20], ALU.add, [psk(b), ("vecs", l)], [K("LG")])
            yield
            tt("dve", LG, LG, cst[:, C_TB:C_TB + 20], ALU.add, [K("LG"), "cst"], [K("LG")])
            yield
            P.add("dve", lambda e: e.reduce_max(GM[:, 0:1], LG[:, 0:4], AXX), [K("LG")], [K("GM")])
            yield
            ts("dve", OH, LG[:, 0:4], GM[:, 0:1], None, ALU.is_equal, None, [K("LG"), K("GM")], [K("OH")])
            ts("dve", EX, LG[:, 0:4], GM[:, 0:1], None, ALU.subtract, None, [K("LG"), K("GM")], [K("EX")])
            yield
            act(EX, EX, AF.Exp, [K("EX")], [K("EX")])
            LE = LG[:, 4:20].rearrange("p (g e) -> p g e", g=4)
            for g in range(4):
                ts("dve", ES[:, g, :], LE[:, g, :], OH[:, g:g + 1], None, ALU.mult, None, [K("LG"), K("OH")],
                   [K(("ES", g))])
            yield
            esk = [K(("ES", g)) for g in range(4)]
            tt("dve", ES[:, 0, :], ES[:, 0, :], ES[:, 1, :], ALU.add, esk, [K(("ES", 0))])
            tt("dve", ES[:, 2, :], ES[:, 2, :], ES[:, 3, :], ALU.add, esk, [K(("ES", 2))])
            yield
            tt("dve", EL, ES[:, 0, :], ES[:, 2, :], ALU.add, [K(("ES", 0)), K(("ES", 2))], [K("EL")])
            P.add("dve", lambda e: e.reduce_sum(GM[:, 1:2], EX, AXX), [K("EX"), K("GM")], [K("GS")])
            yield
            P.add("dve", lambda e: e.reduce_max(SC1[:, 0:1], EL, AXX), [K("EL")], [K("m1")])
            P.add("dve", lambda e: e.reciprocal(GM[:, 2:3], GM[:, 1:2]), [K("GS")], [K("GW")])
            yield
            ts("dve", M1, EL, SC1[:, 0:1], None, ALU.is_equal, None, [K("EL"), K("m1")], [K("M1")])
            yield
            stt(E2, M1, NEG, EL, ALU.mult, ALU.add, [K("M1"), K("EL")], [K("E2")])
            yield
            P.add("dve", lambda e: e.reduce_max(SC1[:, 1:2], E2, AXX), [K("E2")], [K("m2")])
            yield
            ts("dve", M2, E2, SC1[:, 1:2], None, ALU.is_equal, None, [K("E2"), K("m2")], [K("M2")])
            tt("dve", SC1[:, 2:3], SC1[:, 0:1], SC1[:, 1:2], ALU.subtract, [K("m1"), K("m2")], [K("dm")])
            yield
            act(SC1[:, 3:4], SC1[:, 2:3], AF.Sigmoid, [K("dm")], [K("p1")])
            for g in range(4):
                ts("dve", M1G3[:, t, g * 4:(g + 1) * 4], M1, OH[:, g:g + 1], None, ALU.mult, None, [K("M1"), K("OH")],
                   [("M1G", t)])
                ts("dve", M2G3[:, t, g * 4:(g + 1) * 4], M2, OH[:, g:g + 1], None, ALU.mult, None, [K("M2"), K("OH")],
                   [("M2G", t)])
            yield
            ts("dve", SC1[:, 4:5], SC1[:, 3:4], -1.0, 1.0, ALU.mult, ALU.add, [K("p1")], [K("p2")])
            tt("dve", IND3[:, t, :], M1G3[:, t, :], M2G3[:, t, :], ALU.add, [("M1G", t), ("M2G", t)], [("IND", t)])
            yield
            tt("dve", PW[:, t, 0:1], SC1[:, 3:4], GM[:, 2:3], ALU.mult, [K("p1"), K("GW"), K("p2")], [("PW", t)])
            tt("dve", PW[:, t, 1:2], SC1[:, 4:5], GM[:, 2:3], ALU.mult, [K("p2"), K("GW"), ("PW", t)], [("PW", t)])
            yield

        for t0 in range(0, NT, NRS):
            gens = [router_tile(t) for t in range(t0, min(NT, t0 + NRS))]
            while gens:
                for g_ in list(gens):
                    try:
                        next(g_)
                    except StopIteration:
                        gens.remove(g_)
        indk = [("IND", t) for t in range(NT)]
        m1k = [("M1G", t) for t in range(NT)]
        m2k = [("M2G", t) for t in range(NT)]
        for half in range(2):
            cs = slice(half * 264, (half + 1) * 264)
            mm(ps[half][:, 0:264], UST, INDb[:, cs], True, True, indk + ["UST"], [psk(half)])
            mm(ps[2 + half][:, 0:264], ONEb, INDb[:, cs], True, True, indk + ["ONEb"], [psk(2 + half)])
            cp("act", RIN[:, cs], ps[half][:, 0:264], [psk(half)], [("RIN", half)])
            cp("dve", TOT[:, cs], ps[2 + half][:, 0:264], [psk(2 + half)], [("TOT", half)])
        TOT3 = TOT.rearrange("p (t e) -> p t e", e=16)
        INC3 = INC.rearrange("p (t e) -> p t e", e=16)
        RB3 = RB.rearrange("p (t e) -> p t e", e=16)
        for e_ in range(16):
            P.add("dve", lambda e, e_=e_: e.tensor_tensor_scan(INC3[:, :, e_], ONE33, TOT3[:, :, e_], 0.0,
                                                              ALU.mult, ALU.add),
                  [("TOT", 0), ("TOT", 1), "ONE33"], [("INC", e_)])
        ik = [("INC", e_) for e_ in range(16)]
        cp("dve", CNT, INC3[:, NT - 1, :], ik, ["CNT"])
        tt("dve", RB, INC, TOT, ALU.subtract, ik + [("TOT", 0), ("TOT", 1)], ["RB"])
        tt("dve", RB, RB, RIN, ALU.add, ["RB", ("RIN", 0), ("RIN", 1)], ["RB"])
        for e_ in range(16):
            ts("dve", TMP33, cst[:, C_TH:C_TH + NT], CNT[:, e_:e_ + 1], None, ALU.is_le, None, ["cst", "CNT"], ["TMP33"])
            P.add("dve", lambda e, e_=e_: e.reduce_sum(NTL[:, e_:e_ + 1], TMP33, AXX), ["TMP33"], [("NTL", e_)])
        ts("dve", PC, NTL, 128.0, None, ALU.mult, None, [("NTL", e_) for e_ in range(16)], ["PC"])
        P.add("dve", lambda e: e.tensor_tensor_scan(BEND, ONE16, PC, 0.0, ALU.mult, ALU.add), ["PC", "ONE16"], ["BEND"])
        tt("dve", BASE, BEND, PC, ALU.subtract, ["BEND", "PC"], ["BASE"])
        for t in range(NT):
            tt("dve", RB3[:, t, :], RB3[:, t, :], BASE, ALU.add, ["RB", "BASE"], ["RB"])
        for j, (MG_, mk_) in enumerate(((M1G, m1k), (M2G, m2k))):
            tt("dve", PRD, MG_, RB, ALU.mult, mk_ + ["RB"], ["PRD"])
            P.add("dve", lambda e, j=j: e.tensor_reduce(SLF[j], PRD.rearrange("p (t e) -> p t e", e=16), AXX, ALU.add),
                  ["PRD"], [("SLF", j)])
            cp("dve", SLI[j], SLF[j], [("SLF", j)], [("SLI", j)])
        memset("dve", TE, 0.0, ["TE"])
        for e_ in range(16):
            stt(TE, cst[:, C_JT:C_JT + NSLT], BEND[:, e_:e_ + 1], TE, ALU.is_ge, ALU.add, ["cst", "BEND", "TE"], ["TE"])
        ts("dve", TE, TE, 15.0, None, ALU.min, None, ["TE"], ["TE"])
        ts("dve", WIF, TE, 128.0, cst[:, C_PIDX:C_PIDX + 1], ALU.mult, ALU.add, ["TE", "cst"], ["WIF"])
        cp("dve", WII, WIF, ["WIF"], ["WII"])
        if "moe" in dbg_out and l == 0:
            dm = dbg_out["moe"]
            for (o, n, ap, k) in ((0, NT, SLF[0], ("SLF", 0)), (40, NT, SLF[1], ("SLF", 1)), (80, 16, CNT, "CNT"),
                                  (96, 16, PC, "PC"), (112, 16, BEND, "BEND"), (128, 16, BASE, "BASE"),
                                  (144, NSLT, TE, "TE"), (232, NSLT, WIF, "WIF"),
                                  (320, 66, PW.rearrange("p t j -> p (t j)"), None)):
                rk = [k] if k is not None else [("PW", t) for t in range(NT)]
                dma("sp", dm[:, o:o + n], ap, rk, [("dbgmoe", o)])
        for t in range(NT):
            for j in range(2):
                P.add("pool", lambda e, t=t, j=j: e.indirect_dma_start(
                    out=XS[:, :], out_offset=bass.IndirectOffsetOnAxis(ap=SLI[j][:, t:t + 1], axis=0),
                    in_=VBF[:, t, :], in_offset=None), [("VBF", t), ("SLI", j)], [("XS", t, j)], kind="d")
        xsk = [("XS", t, j) for t in range(NT) for j in range(2)]
        wsk = [(nm, e_) for nm in ("W1s", "W3s", "W2s") for e_ in range(16)]
        def load_slot(j):
            wb = WB[j % NWB]
            for a, (Wd, nm) in enumerate(((W1s[l], "w1"), (W3s[l], "w3"), (W2s[l], "w2"))):
                P.add("pool", lambda e, a=a, Wd=Wd, wb=wb, j=j: e.indirect_dma_start(
                    out=wb[a], out_offset=None, in_=Wd[:, :],
                    in_offset=bass.IndirectOffsetOnAxis(ap=WII[:, j:j + 1], axis=0)),
                    ["WII"] + wsk, [("WB", j % NWB, a)], kind="d")
            dma("sp", XI[j % 3], XS[j * 128:(j + 1) * 128, :], xsk, [("XI", j % 3)])
        load_slot(0)
        load_slot(1)
        for j in range(NSLT):
            if j + 2 < NSLT:
                load_slot(j + 2)
            w1, w3, w2 = WB[j % NWB]
            w1 = v3(w1, 8)
            w3 = v3(w3, 8)
            w2 = v3(w2, 2)
            wk = [("WB", j % NWB, a) for a in range(3)]
            xi = XI[j % 3]
            xt = XT[j % 2]
            for k in range(8):
                P.add("pe", lambda e, k=k, xi=xi: e.transpose(psT[:, k * 128:(k + 1) * 128],
                                                              xi[:, k * 128:(k + 1) * 128], ident[:, :]),
                      [("XI", j % 3), "ident"], ["psT"])
            cp("act", xt, psT[:, :].rearrange("p (k t) -> p k t", k=8), ["psT"], [("XT", j % 2)])
            b1 = nbank(0, 7)
            b3 = nbank(0, 7)
            for hc in range(2):
                for k in range(8):
                    mm(ps[b1][:, hc * 128:(hc + 1) * 128], w1[:, k, hc * 128:(hc + 1) * 128], xt[:, k, :],
                       k == 0, k == 7, wk + [("XT", j % 2)], [psk(b1)])
                for k in range(8):
                    mm(ps[b3][:, hc * 128:(hc + 1) * 128], w3[:, k, hc * 128:(hc + 1) * 128], xt[:, k, :],
                       k == 0, k == 7, wk + [("XT", j % 2)], [psk(b3)])
            act(S1, ps[b1][:, 0:256], AF.Silu, [psk(b1)], ["S1s"])
            he = HEb[j % 2]
            tt("dve", he, S1, ps[b3][:, 0:256], ALU.mult, ["S1s", psk(b3)], [("HEb", j % 2)])
            yt = YT[j % 2]
            for half in range(2):
                b = nbank(0, 7)
                for hc in range(2):
                    mm(ps[b][:, 0:512], he[:, hc * 128:(hc + 1) * 128], w2[:, hc, half * 512:(half + 1) * 512],
                       hc == 0, hc == 1, wk + [("HEb", j % 2)], [psk(b)])
                cp("act" if half == 0 else "dve", yt[:, half * 512:(half + 1) * 512], ps[b][:, 0:512], [psk(b)],
                   [("YT", j % 2, half)])
            dma("sp", YS[j * 128:(j + 1) * 128, :], yt, [("YT", j % 2, 0), ("YT", j % 2, 1)], [("YS", j)])
        ysk = [("YS", j) for j in range(NSLT)]
        def load_comb(t):
            m, i = t // 3, t % 3
            r0 = m * CH + i * 128
            hb = t % 2
            dma("sp", HT[hb], hbuf[r0:r0 + 128, :], [("hsrc", m)], [("HT", hb)])
            for j, Yb in enumerate((Y1, Y2)):
                P.add("pool", lambda e, t=t, j=j, Yb=Yb, hb=hb: e.indirect_dma_start(
                    out=Yb[hb], out_offset=None, in_=YS[:, :],
                    in_offset=bass.IndirectOffsetOnAxis(ap=SLI[j][:, t:t + 1], axis=0)),
                    [("SLI", j)] + ysk, [("Yg", j, hb), ("WB", hb, j)], kind="d")
        load_comb(0)
        for t in range(NT):
            if t + 1 < NT:
                load_comb(t + 1)
            m, i = t // 3, t % 3
            r0 = m * CH + i * 128
            hb = t % 2
            stt(HT[hb], Y1[hb], PW[:, t, 0:1], HT[hb], ALU.mult, ALU.add, [("Yg", 0, hb), ("PW", t), ("HT", hb)],
                [("HT", hb)])
            stt(HT[hb], Y2[hb], PW[:, t, 1:2], HT[hb], ALU.mult, ALU.add, [("Yg", 1, hb), ("PW", t), ("HT", hb)],
                [("HT", hb)])
            if not last:
                dma("sp", hbuf[r0:r0 + 128, :], HT[hb], [("HT", hb)], [("hsrc", m), ("hw", m, i)])
            else:
                ss = small[:, 52:53]
                rs = small[:, 53:54]
                otk = [("YT", 0, 0), ("YT", 0, 1)]
                stt(OT, HT[hb], 1.0, HT[hb], ALU.mult, ALU.mult, [("HT", hb)], otk + ["ss3"], accum=ss)
                rsqrt(rs, ss, 1.0 / D, ["ss3"], ["rs3"])
                stt(OT, HT[hb], rs, fing[:, :], ALU.mult, ALU.mult, [("HT", hb), "rs3", "fing"], otk)
                dma("sp", out_d[r0:r0 + 128, :], OT, otk, [("out", m, i)])

    phases = debug.get("phases", None)
    n = 0

    def want():
        return phases is None or n < phases
    for l in range(DEPTH):
        src = h0 if l == 0 else hbuf
        for ph in ("p1", "ex", "p2a", "p2b", "p3a", "p3b"):
            if not want():
                break
            if ph == "p1":
                load_layer_vecs(l)
                phase1(l, src)
            elif ph == "ex":
                exchange()
            elif ph == "p2a":
                phase2a()
            elif ph == "p2b":
                phase2b()
            elif ph == "p3a":
                phase3a(l, src)
            else:
                if SPARSE_MOE:
                    phase3b_sparse(l, l == DEPTH - 1)
                else:
                    phase3b(l, l == DEPTH - 1)
            barrier()
            n += 1
    for nm, t in dbg_out.items():
        if nm in ("F", "moe"):
            continue
        src_t = scratch[nm]
        P.add("sp", lambda e, t=t, src_t=src_t: e.dma_start(out=t.ap(), in_=src_t.ap()), (), [("dbg", nm)], kind="d")
    barrier()
    P.emit()
    return nc, stack


def _constants(c):
    cst = np.zeros((128, NCST), np.float32)
    k = np.arange(128)
    cst[:, C_U:C_U + 128] = (k[:, None] <= k[None, :]).astype(np.float32)
    cst[:, C_ONES:C_ONES + 128] = 1.0
    cst[:, C_ODIV:C_ODIV + 128] = 1.0 / 256.0
    cst[:, C_SEL] = 1.0 if c == 0 else 0.0
    cst[:, C_SEL + 1] = 0.0 if c == 0 else 1.0
    q = np.arange(CH)
    for w in range(2):
        for kt in range(3):
            kpos = kt * 128 + k
            causal = np.where(kpos[:, None] <= q[None, :], 0.0, NEG).astype(np.float32)
            if c == 0:
                mk = causal if w == 0 else np.full((128, CH), NEG, np.float32)
            else:
                mk = np.zeros((128, CH), np.float32) if w == 0 else causal
            o = C_MASK + (w * 3 + kt) * CH
            cst[:, o:o + CH] = mk
    wins = np.array([2, 4, 8, 16], np.float32)
    for first in range(2):
        for cc in range(2):
            for half in range(2):
                W = wins[cc * 2 + half]
                t = np.arange(CH, dtype=np.float32)
                if first == 0 and c == 0:
                    cnt = np.minimum(t + 1.0, W)
                else:
                    cnt = np.full(CH, W, np.float32)
                o = C_INVC + (first * 2 + cc) * CH
                cst[half * 64:(half + 1) * 64, o:o + CH] = (1.0 / cnt)[None, :]
    cst[:, C_ID:C_ID + 128] = np.eye(128, dtype=np.float32)
    cst[:, C_JT:C_JT + 82] = (128.0 * np.arange(82, dtype=np.float32))[None, :]
    cst[:, C_TH:C_TH + 33] = (128.0 * np.arange(33, dtype=np.float32) + 1.0)[None, :]
    cst[:, C_PIDX] = np.arange(128, dtype=np.float32)
    cst[:, C_TB:C_TB + 4] = (-1e-7 * np.arange(4, dtype=np.float32))[None, :]
    cst[:, C_TB + 4:C_TB + 20] = np.tile(-1e-7 * np.arange(4, dtype=np.float32), 4)[None, :]
    return cst


_CACHE = {}


def kernel(x, meta, norm1_g, w_in, b_forget, pool_w, pool_b, pool_scale, conv_w, conv_b, conv_ln_g,
           conv_ln_b, w_out_a, w_out_b, w_out_c, w_o, norm2_g, router_g, router_g_b, router_e,
           router_e_b, exp_w1, exp_w3, exp_w2, final_g, _debug=None):
    f = lambda a: np.ascontiguousarray(np.asarray(a, dtype=np.float32))
    x = f(x)
    B = x.shape[0]
    L = 16 + x.shape[1]
    LP = NG * CH
    meta = f(meta)
    vecs = np.zeros((DEPTH, 128, NV), np.float32)
    chp = np.zeros((DEPTH, 128, NCP), np.float32)
    for l in range(DEPTH):
        vecs[l, :, V_G1:V_G1 + D] = f(norm1_g)[l][None, :]
        vecs[l, :, V_G2:V_G2 + D] = f(norm2_g)[l][None, :]
        vecs[l, :, V_BF:V_BF + 8] = f(b_forget)[l][None, :]
        vecs[l, :, V_RB:V_RB + 4] = f(router_g_b)[l][None, :]
        vecs[l, :, V_RB + 4:V_RB + 20] = f(router_e_b)[l][None, :]
        for cc in range(2):
            sl = slice(cc * 128, (cc + 1) * 128)
            chp[l, :, CP_PB + cc] = f(pool_b)[l].reshape(256)[sl]
            chp[l, :, CP_PS + cc] = f(pool_scale)[l][sl]
            chp[l, :, CP_CB + cc] = f(conv_b)[l][sl]
            chp[l, :, CP_LG + cc] = f(conv_ln_g)[l][sl]
            chp[l, :, CP_LB + cc] = f(conv_ln_b)[l][sl]
            chp[l, :, CP_CW + cc * 31:CP_CW + (cc + 1) * 31] = f(conv_w)[l][:, sl].T
    fing = np.ascontiguousarray(np.broadcast_to(f(final_g)[None, :], (128, D)))
    rw = np.ascontiguousarray(np.concatenate([f(router_g), f(router_e)], axis=-1))
    shared = dict(vecs=vecs, fing=fing, chp=chp, w_in=f(w_in), pool_w=f(pool_w), w_out_a=f(w_out_a),
                  w_out_b=f(w_out_b), w_out_c=f(w_out_c), w_o=f(w_o), rw=rw, exp_w1=f(exp_w1),
                  exp_w3=f(exp_w3), exp_w2=f(exp_w2))
    csts = [_constants(0), _constants(1)]
    in_maps = []
    for core in range(NCORES):
        b, c = core // 2, core % 2
        seq = np.zeros((LP, D), np.float32)
        seq[:16] = meta
        seq[16:L] = x[b]
        own = seq.reshape(NM, 2, CH, D)[:, c].reshape(OWN, D)
        d = dict(shared)
        d["h0"] = np.ascontiguousarray(own)
        d["cst"] = csts[c]
        in_maps.append(d)
    key = repr(sorted((_debug or {}).items()))
    if key not in _CACHE:
        _CACHE[key] = build_program(_debug)
    nc, _ = _CACHE[key]
    res = run_bass_kernel_spmd(nc, in_maps, core_ids=list(range(NCORES)))
    out = np.zeros((B, LP, D), np.float32)
    for core in range(NCORES):
        b, c = core // 2, core % 2
        out[b].reshape(NM, 2, CH, D)[:, c] = res.results[core]["out"].reshape(NM, CH, D)
    if _debug:
        kernel.last = res
    return np.ascontiguousarray(out[:, 16:L])
```
